# Optimizing a Trainium2 kernel written in Bass

```python
import math
import jax, jax.numpy as jnp
from jax import lax
import numpy as np

D_MODEL = 1024
BATCH = 2
SEQ = 8192
DEPTH = 1
DEC_BATCH = 32
DEC_SEQ = 4
PAST_LEN = 8192
PAGE_SIZE = 128

HEAD_DIM = 64
N_HEADS = D_MODEL // HEAD_DIM
N_DIFF_HEADS = N_HEADS // 2
N_FOX_HEADS = N_HEADS - N_DIFF_HEADS
DIFF_D = HEAD_DIM // 2
DIFF_W = N_DIFF_HEADS * HEAD_DIM
FOX_W = N_FOX_HEADS * HEAD_DIM
ROT_DIM = DIFF_D // 4
ROPE_THETA = 500000.0
Q_BLOCK = 128
FORGET_BIAS_INIT = 3.0
N_EXPERTS = 64
N_EXPERT_GROUPS = 8
TOPK_GROUPS = 4
TOP_K = 8
D_EXPERT = 256
D_SHARED = 256
ROUTED_SCALE = 2.5
MOE_TOKEN_BLOCK = 128
DEEPNORM_ALPHA = (2.0 * DEPTH) ** 0.25
DEEPNORM_BETA = (8.0 * DEPTH) ** -0.25
LN_EPS = 1e-5
RMS_EPS = 1e-5
NEG_INF = -1e30
IN_SIZES = (DIFF_W, DIFF_W, DIFF_W, FOX_W, FOX_W, FOX_W, N_FOX_HEADS)
D_IN = sum(IN_SIZES)

kernel_name = 'hymba_diffattn_fox_moe_deepnorm_step'


def lambda_init(layer):
    return 0.8 - 0.6 * math.exp(-0.3 * layer)


def layer_norm(x, g, b):
    xf = x.astype(jnp.float32)
    mu = jnp.mean(xf, axis=-1, keepdims=True)
    var = jnp.mean(jnp.square(xf - mu), axis=-1, keepdims=True)
    return ((xf - mu) * lax.rsqrt(var + LN_EPS) * g + b).astype(x.dtype)


def partial_rope(x, pos):
    half = ROT_DIM // 2
    inv_freq = jnp.power(ROPE_THETA, -jnp.arange(half, dtype=jnp.float32) * 2.0 / ROT_DIM)
    ang = pos.astype(jnp.float32)[:, None] * inv_freq[None, :]
    cos = jnp.cos(ang)[None, :, None, None, :].astype(x.dtype)
    sin = jnp.sin(ang)[None, :, None, None, :].astype(x.dtype)
    x1 = x[..., :half]
    x2 = x[..., half:ROT_DIM]
    return jnp.concatenate([x1 * cos - x2 * sin, x2 * cos + x1 * sin, x[..., ROT_DIM:]], axis=-1)


def project(x, w_in, b_forget, pos):
    B, T = x.shape[0], x.shape[1]
    z = jnp.einsum('btd,de->bte', x, w_in)
    splits = np.cumsum(IN_SIZES)[:-1].tolist()
    qd, kd, vd, qf, kf, vf, fl = jnp.split(z, splits, axis=-1)
    qd = partial_rope(qd.reshape(B, T, N_DIFF_HEADS, 2, DIFF_D), pos)
    kd = partial_rope(kd.reshape(B, T, N_DIFF_HEADS, 2, DIFF_D), pos)
    vd = vd.reshape(B, T, N_DIFF_HEADS, HEAD_DIM)
    qf = qf.reshape(B, T, N_FOX_HEADS, HEAD_DIM)
    kf = kf.reshape(B, T, N_FOX_HEADS, HEAD_DIM)
    vf = vf.reshape(B, T, N_FOX_HEADS, HEAD_DIM)
    logf = jax.nn.log_sigmoid((fl + b_forget).astype(jnp.float32))
    return qd, kd, vd, qf, kf, vf, logf


def diff_attention(q, k, v, lam, q_pos, k_pos):
    s = jnp.einsum('bthcd,blhcd->bchtl', q, k).astype(jnp.float32) * (DIFF_D ** -0.5)
    causal = k_pos[None, :] <= q_pos[:, None]
    p = jax.nn.softmax(jnp.where(causal, s, NEG_INF), axis=-1)
    w = p[:, 0] - lam * p[:, 1]
    return jnp.einsum('bhtl,blhe->bthe', w.astype(v.dtype), v)


def forgetting_attention(q, k, v, cq, ck, q_pos, k_pos):
    s = jnp.einsum('bthe,blhe->bhtl', q, k).astype(jnp.float32) * (HEAD_DIM ** -0.5)
    s = s + jnp.transpose(cq, (0, 2, 1))[..., :, None] - jnp.transpose(ck, (0, 2, 1))[..., None, :]
    causal = k_pos[None, :] <= q_pos[:, None]
    p = jax.nn.softmax(jnp.where(causal, s, NEG_INF), axis=-1)
    return jnp.einsum('bhtl,blhe->bthe', p.astype(v.dtype), v)


def merge_heads(od, of, subln_gain, w_o, lam_init):
    B, T = od.shape[0], od.shape[1]
    odf = od.astype(jnp.float32)
    odn = odf * lax.rsqrt(jnp.mean(jnp.square(odf), axis=-1, keepdims=True) + RMS_EPS) * subln_gain * (1.0 - lam_init)
    o = jnp.concatenate([odn.astype(od.dtype).reshape(B, T, DIFF_W), of.reshape(B, T, FOX_W)], axis=-1)
    return jnp.einsum('btd,de->bte', o, w_o)


def moe_ffn(h, w_router, router_bias, w_gate, w_up, w_down, w_sh_gate, w_sh_up, w_sh_down):
    N = h.shape[0]
    scores = jax.nn.sigmoid(jnp.einsum('nd,de->ne', h, w_router).astype(jnp.float32))
    choice = scores + router_bias.astype(jnp.float32)
    grp = choice.reshape(N, N_EXPERT_GROUPS, N_EXPERTS // N_EXPERT_GROUPS)
    grp_score = jnp.sum(lax.top_k(grp, 2)[0], axis=-1)
    _, top_g = lax.top_k(grp_score, TOPK_GROUPS)
    gmask = jnp.sum(jax.nn.one_hot(top_g, N_EXPERT_GROUPS, dtype=jnp.float32), axis=1) > 0
    emask = jnp.repeat(gmask, N_EXPERTS // N_EXPERT_GROUPS, axis=1)
    _, top_e = lax.top_k(jnp.where(emask, choice, NEG_INF), TOP_K)
    w_sel = jnp.take_along_axis(scores, top_e, axis=1)
    w_sel = w_sel / (jnp.sum(w_sel, axis=-1, keepdims=True) + 1e-20) * ROUTED_SCALE
    gates = jnp.sum(jax.nn.one_hot(top_e, N_EXPERTS, dtype=jnp.float32) * w_sel[..., None], axis=1)
    a = jnp.einsum('nd,edf->nef', h, w_gate)
    u = jnp.einsum('nd,edf->nef', h, w_up)
    act = jax.nn.silu(a) * u * gates[..., None].astype(h.dtype)
    y = jnp.einsum('nef,efd->nd', act, w_down)
    sh = jnp.einsum('nf,fd->nd', jax.nn.silu(h @ w_sh_gate) * (h @ w_sh_up), w_sh_down)
    return y + sh


def gather_pages(pool, page_table):
    g = pool[page_table]
    return g.reshape((g.shape[0], g.shape[1] * g.shape[2]) + g.shape[3:])


def setup_inputs(seed: int = 0) -> dict:
    key = jax.random.key(seed)
    ks = jax.random.split(key, 32)
    f32 = jnp.float32
    n_pages = PAST_LEN // PAGE_SIZE
    n_used = DEC_BATCH * n_pages
    n_pool = n_used + max(1, n_used // 4)
    page_table = jax.random.permutation(ks[0], n_pool)[:n_used].reshape(DEC_BATCH, n_pages).astype(jnp.int32)

    def nrm(k, shape, scale=1.0):
        return scale * jax.random.normal(k, shape, f32)

    pool = (DEPTH, n_pool, PAGE_SIZE)
    return {
        'x_prompt': nrm(ks[1], (BATCH, SEQ, D_MODEL)),
        'x_sample': nrm(ks[2], (DEC_BATCH, DEC_SEQ, D_MODEL)),
        'cache_diff_k': nrm(ks[3], pool + (N_DIFF_HEADS, 2, DIFF_D)),
        'cache_diff_v': nrm(ks[4], pool + (N_DIFF_HEADS, HEAD_DIM)),
        'cache_fox_k': nrm(ks[5], pool + (N_FOX_HEADS, HEAD_DIM)),
        'cache_fox_v': nrm(ks[6], pool + (N_FOX_HEADS, HEAD_DIM)),
        'cache_fox_logf': jax.nn.log_sigmoid(FORGET_BIAS_INIT + nrm(ks[7], pool + (N_FOX_HEADS,))),
        'page_table': page_table,
        'w_in': nrm(ks[8], (DEPTH, D_MODEL, D_IN), D_MODEL ** -0.5),
        'b_forget': FORGET_BIAS_INIT + nrm(ks[9], (DEPTH, N_FOX_HEADS), 0.1),
        'lambda_q1': nrm(ks[10], (DEPTH, DIFF_D), 0.1),
        'lambda_k1': nrm(ks[11], (DEPTH, DIFF_D), 0.1),
        'lambda_q2': nrm(ks[12], (DEPTH, DIFF_D), 0.1),
        'lambda_k2': nrm(ks[13], (DEPTH, DIFF_D), 0.1),
        'subln_gain': 1.0 + nrm(ks[14], (DEPTH, HEAD_DIM), 0.02),
        'w_o': nrm(ks[15], (DEPTH, D_MODEL, D_MODEL), D_MODEL ** -0.5 * DEEPNORM_BETA),
        'ln1_g': 1.0 + nrm(ks[16], (DEPTH, D_MODEL), 0.02),
        'ln1_b': nrm(ks[17], (DEPTH, D_MODEL), 0.02),
        'w_router': nrm(ks[18], (DEPTH, D_MODEL, N_EXPERTS), D_MODEL ** -0.5),
        'router_bias': nrm(ks[19], (DEPTH, N_EXPERTS), 0.01),
        'w_exp_gate': nrm(ks[20], (DEPTH, N_EXPERTS, D_MODEL, D_EXPERT), D_MODEL ** -0.5),
        'w_exp_up': nrm(ks[21], (DEPTH, N_EXPERTS, D_MODEL, D_EXPERT), D_MODEL ** -0.5),
        'w_exp_down': nrm(ks[22], (DEPTH, N_EXPERTS, D_EXPERT, D_MODEL), D_EXPERT ** -0.5 * DEEPNORM_BETA),
        'w_sh_gate': nrm(ks[23], (DEPTH, D_MODEL, D_SHARED), D_MODEL ** -0.5),
        'w_sh_up': nrm(ks[24], (DEPTH, D_MODEL, D_SHARED), D_MODEL ** -0.5),
        'w_sh_down': nrm(ks[25], (DEPTH, D_SHARED, D_MODEL), D_SHARED ** -0.5 * DEEPNORM_BETA),
        'ln2_g': 1.0 + nrm(ks[26], (DEPTH, D_MODEL), 0.02),
        'ln2_b': nrm(ks[27], (DEPTH, D_MODEL), 0.02),
    }


def reference(x_prompt, x_sample, cache_diff_k, cache_diff_v, cache_fox_k, cache_fox_v, cache_fox_logf,
              page_table, w_in, b_forget, lambda_q1, lambda_k1, lambda_q2, lambda_k2, subln_gain, w_o,
              ln1_g, ln1_b, w_router, router_bias, w_exp_gate, w_exp_up, w_exp_down,
              w_sh_gate, w_sh_up, w_sh_down, ln2_g, ln2_b):
    f32 = jnp.float32
    n_blocks = SEQ // Q_BLOCK
    pos_p = jnp.arange(SEQ, dtype=jnp.int32)
    pos_s = PAST_LEN + jnp.arange(DEC_SEQ, dtype=jnp.int32)
    kpos_s = jnp.arange(PAST_LEN + DEC_SEQ, dtype=jnp.int32)
    xp, xs = x_prompt, x_sample
    p_dk, p_dv, p_fk, p_fv, p_fl = [], [], [], [], []
    s_dk, s_dv, s_fk, s_fv, s_fl = [], [], [], [], []
    for l in range(DEPTH):
        lam_init = lambda_init(l)
        lam = (jnp.exp(jnp.sum((lambda_q1[l] * lambda_k1[l]).astype(f32)))
               - jnp.exp(jnp.sum((lambda_q2[l] * lambda_k2[l]).astype(f32))) + lam_init)
        moe_w = (w_router[l], router_bias[l], w_exp_gate[l], w_exp_up[l], w_exp_down[l],
                 w_sh_gate[l], w_sh_up[l], w_sh_down[l])

        qd, kd, vd, qf, kf, vf, logf = project(xp, w_in[l], b_forget[l], pos_p)
        c = jnp.cumsum(logf, axis=1)

        def prompt_block(i):
            start = i * Q_BLOCK
            q_pos = start + jnp.arange(Q_BLOCK, dtype=jnp.int32)
            od_b = diff_attention(lax.dynamic_slice_in_dim(qd, start, Q_BLOCK, axis=1), kd, vd, lam, q_pos, pos_p)
            of_b = forgetting_attention(lax.dynamic_slice_in_dim(qf, start, Q_BLOCK, axis=1), kf, vf,
                                        lax.dynamic_slice_in_dim(c, start, Q_BLOCK, axis=1), c, q_pos, pos_p)
            return od_b, of_b

        od, of = lax.map(prompt_block, jnp.arange(n_blocks, dtype=jnp.int32))
        bp = xp.shape[0]
        od = jnp.moveaxis(od, 0, 1).reshape(bp, SEQ, N_DIFF_HEADS, HEAD_DIM)
        of = jnp.moveaxis(of, 0, 1).reshape(bp, SEQ, N_FOX_HEADS, HEAD_DIM)
        hp = layer_norm(DEEPNORM_ALPHA * xp + merge_heads(od, of, subln_gain[l], w_o[l], lam_init), ln1_g[l], ln1_b[l])
        moe_p = lax.map(lambda blk: moe_ffn(blk, *moe_w), hp.reshape(-1, MOE_TOKEN_BLOCK, D_MODEL)).reshape(hp.shape)
        xp = layer_norm(DEEPNORM_ALPHA * hp + moe_p, ln2_g[l], ln2_b[l])
        p_dk.append(kd); p_dv.append(vd); p_fk.append(kf); p_fv.append(vf); p_fl.append(logf)

        sqd, skd, svd, sqf, skf, svf, slogf = project(xs, w_in[l], b_forget[l], pos_s)
        kd_all = jnp.concatenate([gather_pages(cache_diff_k[l], page_table), skd], axis=1)
        vd_all = jnp.concatenate([gather_pages(cache_diff_v[l], page_table), svd], axis=1)
        kf_all = jnp.concatenate([gather_pages(cache_fox_k[l], page_table), skf], axis=1)
        vf_all = jnp.concatenate([gather_pages(cache_fox_v[l], page_table), svf], axis=1)
        c_all = jnp.cumsum(jnp.concatenate([gather_pages(cache_fox_logf[l], page_table).astype(f32), slogf], axis=1), axis=1)
        od_s = diff_attention(sqd, kd_all, vd_all, lam, pos_s, kpos_s)
        of_s = forgetting_attention(sqf, kf_all, vf_all, c_all[:, PAST_LEN:], c_all, pos_s, kpos_s)
        hs = layer_norm(DEEPNORM_ALPHA * xs + merge_heads(od_s, of_s, subln_gain[l], w_o[l], lam_init), ln1_g[l], ln1_b[l])
        moe_s = moe_ffn(hs.reshape(-1, D_MODEL), *moe_w).reshape(hs.shape)
        xs = layer_norm(DEEPNORM_ALPHA * hs + moe_s, ln2_g[l], ln2_b[l])
        s_dk.append(skd); s_dv.append(svd); s_fk.append(skf); s_fv.append(svf); s_fl.append(slogf)

    return (xp, xs,
            jnp.stack(p_dk), jnp.stack(p_dv), jnp.stack(p_fk), jnp.stack(p_fv), jnp.stack(p_fl),
            jnp.stack(s_dk), jnp.stack(s_dv), jnp.stack(s_fk), jnp.stack(s_fv), jnp.stack(s_fl))
```

```python
import math
import numpy as np
from contextlib import ExitStack
import concourse.bass as bass
import concourse.mybir as mybir
from concourse.bass_utils import run_bass_kernel_spmd

F32 = mybir.dt.float32
BF16 = mybir.dt.bfloat16
I32 = mybir.dt.int32
U32 = mybir.dt.uint32
AF = mybir.ActivationFunctionType
ALU = mybir.AluOpType
AX = mybir.AxisListType

D = 1024
SEQ = 8192
NT = 64
NCH = 16
NOWN = 16
NE = 64
DEEP_ALPHA = 2.0 ** 0.25
LAM_INIT = 0.8 - 0.6 * math.exp(0.0)
LN_EPS = 1e-5
RMS_EPS = 1e-5
ROPE_THETA = 500000.0
C_QD, C_KD, C_VD, C_QF, C_KF, C_VF, C_FL = 0, 512, 1024, 1536, 2048, 2560, 3072
D_IN = 3080
NEGBIG = -30000.0
PAST = 8192


class Buf:
    __slots__ = ("name", "lw", "rd", "sem", "semval")

    def __init__(self, name):
        self.name = name
        self.lw = None
        self.rd = {}
        self.sem = None
        self.semval = 0


class Eng:
    def __init__(self, name, handle, sem):
        self.name = name
        self.h = handle
        self.sem = sem
        self.count = 0
        self.waited = {}
        self.same_engine_sync = name in ("act", "dve", "pool")


class T:
    def __init__(self, t, name):
        self.t = t
        self.b = Buf(name)

    def __getitem__(self, k):
        return self.t[k]


class FW:
    def __init__(self, nc, stack):
        self.nc = nc
        self.stack = stack
        self.root = stack
        self.nsem = 0
        self.dma_bufs = []
        self.pe = Eng("pe", nc.tensor, self.new_sem("pe"))
        self.act = Eng("act", nc.scalar, self.new_sem("act"))
        self.dve = Eng("dve", nc.vector, self.new_sem("dve"))
        self.pool = Eng("pool", nc.gpsimd, self.new_sem("pool"))
        self.sp = Eng("sp", nc.sync, self.new_sem("sp"))
        self.ninst = 0
        self.out_events = []

    def new_sem(self, name):
        s = self.root.enter_context(self.nc.semaphore(name))
        self.nsem += 1
        return s

    def sb(self, name, shape, dt):
        return T(self.stack.enter_context(self.nc.sbuf_tensor(name, list(shape), dt)), name)

    def ps(self, name, shape, dt):
        return T(self.stack.enter_context(self.nc.psum_tensor(name, list(shape), dt)), name)

    def _need(self, eng, reads, writes):
        deps = {}

        def add(ev):
            if ev is None:
                return
            k, v = ev
            if deps.get(id(k), (None, 0))[1] < v:
                deps[id(k)] = (k, v)

        for b in reads:
            add(b.lw)
        for b in writes:
            add(b.lw)
            for kv in b.rd.values():
                add(kv)
        for k, v in deps.values():
            if k is eng.sem:
                if not eng.same_engine_sync or v > eng.count:
                    continue
            if eng.waited.get(id(k), 0) >= v:
                continue
            eng.h.wait_ge(k, v)
            eng.waited[id(k)] = v

    def op(self, eng, fn, reads=(), writes=(), inc=True):
        reads = [r.b if isinstance(r, T) else r for r in reads]
        writes = [w.b if isinstance(w, T) else w for w in writes]
        self._need(eng, reads, writes)
        ins = fn()
        self.ninst += 1
        if inc:
            eng.count += 1
            ins.then_inc(eng.sem, 1)
            ev = (eng.sem, eng.count)
        else:
            ev = (eng.sem, eng.count + 1)
        for b in writes:
            b.lw = ev
            b.rd = {}
        for b in reads:
            if b.rd.get(id(ev[0]), (None, 0))[1] < ev[1]:
                b.rd[id(ev[0])] = ev
        return ins

    def dma(self, q, out, in_, reads=(), writes=(), sembuf=None, indirect=None, final=False):
        reads = [r.b if isinstance(r, T) else r for r in reads]
        writes = [w.b if isinstance(w, T) else w for w in writes]
        if sembuf is None:
            sembuf = writes[0] if writes else reads[0]
        if isinstance(sembuf, T):
            sembuf = sembuf.b
        if sembuf.sem is None:
            sembuf.sem = self.new_sem("d_" + sembuf.name)
            self.dma_bufs.append(sembuf)
        self._need(q, reads, writes)
        if sembuf.semval > 0 and q.waited.get(id(sembuf.sem), 0) < sembuf.semval:
            q.h.wait_ge(sembuf.sem, sembuf.semval)
            q.waited[id(sembuf.sem)] = sembuf.semval
        if indirect is not None:
            ins = q.h.indirect_dma_start(out=out, out_offset=None, in_=in_,
                                         in_offset=bass.IndirectOffsetOnAxis(ap=indirect, axis=0))
        else:
            ins = q.h.dma_start(out=out, in_=in_)
        self.ninst += 1
        sembuf.semval += 16
        ins.then_inc(sembuf.sem, 16)
        ev = (sembuf.sem, sembuf.semval)
        for b in writes:
            b.lw = ev
            b.rd = {}
        for b in reads:
            b.rd[id(ev[0])] = ev
        if final:
            self.out_events = [e for e in self.out_events if e[0] is not ev[0]] + [ev]
        return ev

    def barrier(self):
        sp = self.sp
        engs = [self.pe, self.act, self.dve, self.pool]
        for b in self.dma_bufs:
            if b.semval > 0 and sp.waited.get(id(b.sem), 0) < b.semval:
                sp.h.wait_ge(b.sem, b.semval)
                sp.waited[id(b.sem)] = b.semval
        for f in engs:
            if f.count > sp.waited.get(id(f.sem), 0):
                sp.h.wait_ge(f.sem, f.count)
                sp.waited[id(f.sem)] = f.count
        sp.count += 1
        sp.h.nop().then_inc(sp.sem, 1)
        for e in engs:
            e.h.wait_ge(sp.sem, sp.count)
            e.waited[id(sp.sem)] = sp.count

    def push(self):
        es = ExitStack()
        es.__enter__()
        prev = self.stack
        self.stack = es
        return (es, prev)

    def pop(self, ph):
        self.barrier()
        ph[0].__exit__(None, None, None)
        self.stack = ph[1]

    def finish(self):
        for k, v in self.out_events:
            self.sp.h.wait_ge(k, v)


def host_consts():
    p = np.arange(128)
    ident = np.eye(128, dtype=np.float32)
    U = (p[:, None] <= p[None, :]).astype(np.float32)
    q = np.arange(512)
    dm = np.stack([(128 * j + p[:, None] <= q[None, :]) for j in range(4)], axis=1).astype(np.float32)
    mk = np.zeros((128, 4), np.float32)
    mk[:, 0] = ((p // 32) % 2 == 0)
    mk[:, 1] = ((p // 32) % 2 == 1)
    mk[:, 2] = (p == 0)
    mk[:, 3] = 1.0
    half = 4
    inv_freq = np.power(np.float32(ROPE_THETA), -np.arange(half, dtype=np.float32) * np.float32(2.0) / np.float32(8)).astype(np.float32)
    invf = np.tile(inv_freq[None, :], (128, 1)).astype(np.float32)
    return ident, U, dm.reshape(128, 2048), mk, invf


def build_main():
    nc = bass.Bass("TRN2", target_bir_lowering=False)

    def din(name, shape, dt=F32):
        return nc.dram_tensor(name, list(shape), dt, kind="ExternalInput").ap()

    def dout(name, shape, dt=F32):
        return nc.dram_tensor(name, list(shape), dt, kind="ExternalOutput").ap()

    x_perm = din("x_perm", [SEQ, D])
    kpos = din("kpos", [128, NT])
    tpos_in = din("tpos", [NT, 1])
    qfirst_in = din("qfirst", [1, 4])
    x_s = din("x_s", [16, D])
    attn_s_in = din("attn_s", [16, D])
    w_in = din("w_in", [D, D_IN])
    b_forget = din("b_forget", [1, 8])
    lam4 = din("lam4", [4, 32])
    subln = din("subln", [64, 1])
    w_o = din("w_o", [D, D])
    ln1 = din("ln1", [2, D])
    ln2 = din("ln2", [2, D])
    w_router = din("w_router", [D, NE])
    r_bias = din("r_bias", [1, NE])
    w_g = din("w_g", [NE + 1, D, 256])
    w_u = din("w_u", [NE + 1, D, 256])
    w_d = din("w_d", [NE + 1, 256, D])
    c_ident = din("c_ident", [128, 128])
    c_U = din("c_U", [128, 128])
    c_dm = din("c_dm", [128, 2048])
    c_mk = din("c_mk", [128, 4])
    c_invf = din("c_invf", [128, 4])

    y_own = dout("y_own", [2048, D])
    y_s = dout("y_s", [16, D])
    kd_own = dout("kd_own", [2048, 512])
    vd_own = dout("vd_own", [2048, 512])
    kf_own = dout("kf_own", [2048, 512])
    vf_own = dout("vf_own", [2048, 512])
    lf_own = dout("lf_own", [2048, 8])

    kT_scr = nc.dram_tensor("kT_scr", [8, 128, SEQ], BF16, kind="Internal").ap()
    v_scr = nc.dram_tensor("v_scr", [8, 128, NT, 130], BF16, kind="Internal").ap()
    attn_scr = nc.dram_tensor("attn_scr", [2048, D], BF16, kind="Internal").ap()

    with ExitStack() as st:
        fw = FW(nc, st)
        pe, act, dve, pool, sp = fw.pe, fw.act, fw.dve, fw.pool, fw.sp
        V = nc.vector
        A = nc.scalar
        G = nc.gpsimd
        PE = nc.tensor

        PS = [fw.ps(f"ps{i}", [128, 512], F32) for i in range(8)]

        ident = fw.sb("ident", [128, 128], F32)
        identb = fw.sb("identb", [128, 128], BF16)
        Umat = fw.sb("Umat", [128, 128], F32)
        mk = fw.sb("mk", [128, 4], F32)
        invf = fw.sb("invf", [128, 4], F32)
        kpos_sb = fw.sb("kpos_sb", [128, NT], F32)
        cosT = fw.sb("cosT", [128, NT, 4], F32)
        sinT = fw.sb("sinT", [128, NT, 4], F32)
        bf_bc = fw.sb("bf_bc", [128, 1, 8], F32)
        logf = fw.sb("logf", [128, NT, 8], F32)
        ones32 = fw.sb("ones32", [128, 128], F32)
        onesb = fw.sb("onesb", [128, 128], BF16)
        lamt = fw.sb("lamt", [128, 1], F32)
        gainc = fw.sb("gainc", [64, 1], F32)

        ld = lambda t, src, q=sp: fw.dma(q, t[:], src, writes=[t])
        ld(ident, c_ident[:, :])
        ld(Umat, c_U[:, :])
        ld(mk, c_mk[:, :])
        ld(invf, c_invf[:, :])
        ld(kpos_sb, kpos[:, :])
        fw.dma(sp, bf_bc[:], b_forget[0:1, :].partition_broadcast(128), writes=[bf_bc])
        fw.op(dve, lambda: V.tensor_copy(out=identb[:], in_=ident[:]), reads=[ident], writes=[identb])
        fw.op(dve, lambda: V.memset(ones32[:], 1.0), writes=[ones32])
        fw.op(dve, lambda: V.memset(onesb[:], 1.0), writes=[onesb])

        lamv = fw.sb("lamv", [128, 4, 32], F32)
        fw.dma(sp, lamv[:].rearrange("p a b -> p (a b)"),
               lam4.rearrange("a b -> (a b)").unsqueeze(0).partition_broadcast(128).squeeze(1)
               if False else lam4.rearrange("(o a) b -> o (a b)", o=1).partition_broadcast(128).squeeze(1),
               writes=[lamv])
        lprod = fw.sb("lprod", [128, 2, 32], F32)
        lsum = fw.sb("lsum", [128, 2], F32)
        lexp = fw.sb("lexp", [128, 2], F32)
        fw.op(dve, lambda: V.tensor_tensor(out=lprod[:, 0, :], in0=lamv[:, 0, :], in1=lamv[:, 1, :], op=ALU.mult), reads=[lamv], writes=[lprod])
        fw.op(dve, lambda: V.tensor_tensor(out=lprod[:, 1, :], in0=lamv[:, 2, :], in1=lamv[:, 3, :], op=ALU.mult), reads=[lamv], writes=[lprod])
        fw.op(dve, lambda: V.tensor_reduce(out=lsum[:], in_=lprod[:], axis=AX.X, op=ALU.add), reads=[lprod], writes=[lsum])
        fw.op(act, lambda: A.activation(out=lexp[:], in_=lsum[:], func=AF.Exp), reads=[lsum], writes=[lexp])
        fw.op(dve, lambda: V.tensor_tensor(out=lamt[:], in0=lexp[:, 1:2], in1=lexp[:, 0:1], op=ALU.subtract), reads=[lexp], writes=[lamt])
        fw.op(dve, lambda: V.tensor_scalar(out=lamt[:], in0=lamt[:], scalar1=-LAM_INIT, scalar2=None, op0=ALU.add), reads=[lamt], writes=[lamt])
        ld(gainc, subln[:, :])
        fw.op(dve, lambda: V.tensor_scalar(out=gainc[:], in0=gainc[:], scalar1=1.0 - LAM_INIT, scalar2=None, op0=ALU.mult), reads=[gainc], writes=[gainc])

        ang = fw.sb("ang", [128, NT, 4], F32)
        angi = fw.sb("angi", [128, NT, 4], I32)
        angk = fw.sb("angk", [128, NT, 4], F32)
        angr = fw.sb("angr", [128, NT, 4], F32)
        angc = fw.sb("angc", [128, NT, 4], F32)
        TWO_PI = 2.0 * math.pi

        def reduce_sin(dst, shift):
            fw.op(dve, lambda: V.tensor_scalar(out=angk[:], in0=ang[:], scalar1=shift, scalar2=1.0 / TWO_PI, op0=ALU.add, op1=ALU.mult), reads=[ang], writes=[angk])
            fw.op(dve, lambda: V.tensor_copy(out=angi[:], in_=angk[:]), reads=[angk], writes=[angi])
            fw.op(dve, lambda: V.tensor_copy(out=angk[:], in_=angi[:]), reads=[angi], writes=[angk])
            fw.op(dve, lambda: V.tensor_scalar(out=angr[:], in0=ang[:], scalar1=shift, scalar2=None, op0=ALU.add), reads=[ang], writes=[angr])
            fw.op(dve, lambda: V.scalar_tensor_tensor(out=angr[:], in0=angk[:], scalar=-TWO_PI, in1=angr[:], op0=ALU.mult, op1=ALU.add), reads=[angk, angr], writes=[angr])
            fw.op(dve, lambda: V.tensor_scalar(out=angc[:], in0=angr[:], scalar1=math.pi, scalar2=-TWO_PI, op0=ALU.is_gt, op1=ALU.mult), reads=[angr], writes=[angc])
            fw.op(dve, lambda: V.tensor_tensor(out=angr[:], in0=angr[:], in1=angc[:], op=ALU.add), reads=[angr, angc], writes=[angr])
            fw.op(dve, lambda: V.tensor_scalar(out=angc[:], in0=angr[:], scalar1=-math.pi, scalar2=TWO_PI, op0=ALU.is_lt, op1=ALU.mult), reads=[angr], writes=[angc])
            fw.op(dve, lambda: V.tensor_tensor(out=angr[:], in0=angr[:], in1=angc[:], op=ALU.add), reads=[angr, angc], writes=[angr])
            fw.op(dve, lambda: V.tensor_scalar(out=angr[:], in0=angr[:], scalar1=-3.14159, scalar2=3.14159, op0=ALU.max, op1=ALU.min), reads=[angr], writes=[angr])
            fw.op(act, lambda: A.activation(out=dst[:], in_=angr[:], func=AF.Sin), reads=[angr], writes=[dst])

        fw.op(dve, lambda: V.tensor_tensor(out=ang[:], in0=kpos_sb[:].unsqueeze(2).broadcast_to([128, NT, 4]),
                                           in1=invf[:].unsqueeze(1).broadcast_to([128, NT, 4]), op=ALU.mult),
              reads=[kpos_sb, invf], writes=[ang])
        reduce_sin(sinT, 0.0)
        reduce_sin(cosT, math.pi / 2.0)

        S12 = fw.push()
        qT = fw.sb("qT", [128, 8, 2048], BF16)
        S1 = fw.push()
        win = fw.sb("win", [128, 8, D_IN], BF16)
        for kc in range(8):
            for hlf in range(2):
                c0 = hlf * 1540
                fw.dma(pool, win[:, kc, c0:c0 + 1540], w_in[kc * 128:(kc + 1) * 128, c0:c0 + 1540], writes=[win])
        xb = [fw.sb(f"xb{i}", [128, D], BF16) for i in range(3)]
        xf = [fw.sb(f"xf{i}", [128, D], F32) for i in range(3)]
        xT = [fw.sb(f"xT{i}", [128, 8, 128], BF16) for i in range(2)]
        NST = 2
        stg = {nm: [fw.sb(f"s_{nm}{i}", [128, 512], F32) for i in range(NST)] for nm in ("kd", "vd", "kf", "vf", "qd", "qf")}
        stb = {nm: [fw.sb(f"b_{nm}{i}", [128, 512], BF16) for i in range(NST)] for nm in ("kd", "kf", "qd", "qf")}
        vaug = [fw.sb(f"vaug{i}", [128, 16, 65], BF16) for i in range(2)]
        for vv in vaug:
            fw.op(pool, lambda vv=vv: G.memset(vv[:], 1.0), writes=[vv])
        kTst = [fw.sb(f"kTst{i}", [128, 8, 512], BF16) for i in range(2)]
        rt = [fw.sb(f"rt{i}", [128, 64], F32) for i in range(4)]
        zt = fw.sb("zt", [128, 32], F32)
        et = fw.sb("et", [128, 32], F32)
        PX = [PS[0], PS[1]]
        PJ = [PS[2], PS[3], PS[4]]
        PL = PS[5]
        PK = [PS[6], PS[7]]
        colof = {"qd": C_QD, "kd": C_KD, "vd": C_VD, "qf": C_QF, "kf": C_KF, "vf": C_VF}
        outd = {"kd": kd_own, "vd": vd_own, "kf": kf_own, "vf": vf_own}
        kT_b = [Buf(f"kTscr{c}") for c in range(NCH)]
        v_b = [Buf(f"vscr{t}") for t in range(NT)]

        def rope(s_t, t):
            v3 = s_t[:].rearrange("p (g d) -> p g d", d=32)
            x1 = v3[:, :, 0:4]
            x2 = v3[:, :, 4:8]
            cb = cosT[:, t, :].unsqueeze(1).broadcast_to([128, 16, 4])
            sbb = sinT[:, t, :].unsqueeze(1).broadcast_to([128, 16, 4])
            r = [x[:].rearrange("p (g d) -> p g d", d=4) for x in rt]
            fw.op(dve, lambda: V.tensor_tensor(out=r[0], in0=x1, in1=cb, op=ALU.mult), reads=[s_t, cosT], writes=[rt[0]])
            fw.op(dve, lambda: V.tensor_tensor(out=r[1], in0=x2, in1=sbb, op=ALU.mult), reads=[s_t, sinT], writes=[rt[1]])
            fw.op(dve, lambda: V.tensor_tensor(out=r[2], in0=x2, in1=cb, op=ALU.mult), reads=[s_t, cosT], writes=[rt[2]])
            fw.op(dve, lambda: V.tensor_tensor(out=r[3], in0=x1, in1=sbb, op=ALU.mult), reads=[s_t, sinT], writes=[rt[3]])
            fw.op(dve, lambda: V.tensor_tensor(out=x1, in0=r[0], in1=r[1], op=ALU.subtract), reads=[rt[0], rt[1]], writes=[s_t])
            fw.op(dve, lambda: V.tensor_tensor(out=x2, in0=r[2], in1=r[3], op=ALU.add), reads=[rt[2], rt[3]], writes=[s_t])

        pjc = [0]

        def x_load(t):
            xft = xf[t % 3]
            fw.dma(sp, xft[:], x_perm[t * 128:(t + 1) * 128, :], writes=[xft])

        def P1(t):
            xbt = xb[t % 3]
            xTt = xT[t % 2]
            px = PX[t % 2]
            xft = xf[t % 3]
            fw.op(pool, lambda: G.tensor_copy(out=xbt[:], in_=xft[:]), reads=[xft], writes=[xbt])
            pxb = px[:].bitcast(BF16)
            for kc in range(8):
                fw.op(pe, lambda kc=kc: PE.transpose(out=pxb[:, kc * 128:(kc + 1) * 128], in_=xbt[:, kc * 128:(kc + 1) * 128], identity=identb[:]),
                      reads=[xbt, identb], writes=[px], inc=(kc == 7))
            fw.op(dve, lambda: V.tensor_copy(out=xTt[:].rearrange("p a b -> p (a b)"), in_=pxb[:, 0:1024]), reads=[px], writes=[xTt])

        def P2(t):
            ch, j = divmod(t, 4)
            own = (ch % 4 == 0)
            so = ch // 4
            xTt = xT[t % 2]
            groups = ["kd", "vd", "kf", "vf"] + (["qd", "qf"] if own else [])
            si = t % NST
            for nm in groups:
                pj = PJ[pjc[0] % 3]
                pjc[0] += 1
                c0 = colof[nm]
                for kc in range(8):
                    fw.op(pe, lambda kc=kc, pj=pj, c0=c0: PE.matmul(pj[:, :], lhsT=xTt[:, kc, :], rhs=win[:, kc, c0:c0 + 512], start=(kc == 0), stop=(kc == 7)),
                          reads=[xTt, win], writes=[pj], inc=(kc == 7))
                s_t = stg[nm][si]
                va = vaug[t % 2]
                h0 = 0 if nm == "vd" else 8
                need32 = own or nm in ("kd", "qd")
                if need32:
                    fw.op(act, lambda pj=pj, s_t=s_t: A.activation(out=s_t[:], in_=pj[:, :], func=AF.Copy), reads=[pj], writes=[s_t])
                    if nm in ("kd", "qd"):
                        rope(s_t, t)
                    if nm in ("kd", "kf", "qd", "qf"):
                        b_t = stb[nm][si]
                        fw.op(dve, lambda s_t=s_t, b_t=b_t: V.tensor_copy(out=b_t[:], in_=s_t[:]), reads=[s_t], writes=[b_t])
                    else:
                        fw.op(dve, lambda s_t=s_t, va=va, h0=h0: V.tensor_copy(out=va[:, h0:h0 + 8, 0:64], in_=s_t[:].rearrange("p (h e) -> p h e", e=64)),
                              reads=[s_t], writes=[va])
                elif nm == "kf":
                    b_t = stb[nm][si]
                    fw.op(act, lambda pj=pj, b_t=b_t: A.activation(out=b_t[:], in_=pj[:, :], func=AF.Copy), reads=[pj], writes=[b_t])
                else:
                    fw.op(act, lambda pj=pj, va=va, h0=h0: A.activation(out=va[:, h0:h0 + 8, 0:64], in_=pj[:, :].rearrange("p (h e) -> p h e", e=64), func=AF.Copy),
                          reads=[pj], writes=[va])
                if own and nm in outd:
                    r0 = so * 512 + j * 128
                    fw.dma(sp, outd[nm][r0:r0 + 128, :], s_t[:], reads=[s_t], writes=[Buf("o")], sembuf=s_t, final=True)
            for kc in range(8):
                fw.op(pe, lambda kc=kc: PE.matmul(PL[:, j * 8:(j + 1) * 8], lhsT=xTt[:, kc, :], rhs=win[:, kc, C_FL:C_FL + 8], start=(kc == 0), stop=(kc == 7)),
                      reads=[xTt, win], writes=[PL], inc=(kc == 7))
            if j == 3:
                fw.op(dve, lambda: V.tensor_tensor(out=zt[:].rearrange("p (a h) -> p a h", h=8), in0=PL[:, 0:32].rearrange("p (a h) -> p a h", h=8),
                                                   in1=bf_bc[:].broadcast_to([128, 4, 8]), op=ALU.add), reads=[PL, bf_bc], writes=[zt])
                fw.op(act, lambda: A.activation(out=et[:], in_=zt[:], func=AF.Exp, scale=-1.0), reads=[zt], writes=[et])
                fw.op(act, lambda: A.activation(out=zt[:], in_=et[:], func=AF.Ln, bias=1.0), reads=[et], writes=[zt])
                fw.op(dve, lambda: V.tensor_scalar(out=logf[:, ch * 4:ch * 4 + 4, :], in0=zt[:].rearrange("p (a h) -> p a h", h=8), scalar1=-1.0, scalar2=None, op0=ALU.mult),
                      reads=[zt], writes=[logf])
                if own:
                    fw.dma(sp, lf_own[so * 512:(so + 1) * 512, :].rearrange("(a p) h -> p a h", p=128), logf[:, ch * 4:ch * 4 + 4, :],
                           reads=[logf], writes=[Buf("o")], sembuf=logf, final=True)

        def P3(t):
            ch, j = divmod(t, 4)
            own = (ch % 4 == 0)
            so = ch // 4
            si = t % NST
            pk = PK[t % 2]
            pkb = pk[:].bitcast(BF16)
            kst = kTst[ch % 2]
            for gi in range(8):
                src = stb["kd"][si] if gi < 4 else stb["kf"][si]
                g4 = gi % 4
                fw.op(pe, lambda gi=gi, src=src, g4=g4: PE.transpose(out=pkb[:, gi * 128:(gi + 1) * 128], in_=src[:, g4 * 128:(g4 + 1) * 128], identity=identb[:]),
                      reads=[src, identb], writes=[pk], inc=(gi == 7))
            fw.op(dve, lambda: V.tensor_copy(out=kst[:, :, j * 128:(j + 1) * 128], in_=pkb[:, 0:1024].rearrange("p (g n) -> p g n", n=128)),
                  reads=[pk], writes=[kst])
            if own:
                pq = PK[(t + 1) % 2]
                pqb = pq[:].bitcast(BF16)
                for gi in range(8):
                    src = stb["qd"][si] if gi < 4 else stb["qf"][si]
                    g4 = gi % 4
                    fw.op(pe, lambda gi=gi, src=src, g4=g4: PE.transpose(out=pqb[:, gi * 128:(gi + 1) * 128], in_=src[:, g4 * 128:(g4 + 1) * 128], identity=identb[:]),
                          reads=[src, identb], writes=[pq], inc=(gi == 7))
                q0 = so * 512 + j * 128
                pq3 = pqb[:, 0:1024].rearrange("p (g n) -> p g n", n=128)
                fw.op(dve, lambda: V.tensor_copy(out=qT[:, :, q0:q0 + 128], in_=pq3), reads=[pq], writes=[qT])
            va = vaug[t % 2]
            fw.dma(sp, v_scr[:, :, t, :].rearrange("g p c -> p g c"), va[:].rearrange("p (g a) c -> p g (a c)", a=2), reads=[va], writes=[v_b[t]], sembuf=va)
            if j == 3:
                fw.dma(sp, kT_scr[:, :, ch * 512:(ch + 1) * 512].rearrange("g p n -> p g n"), kst[:], reads=[kst], writes=[kT_b[ch]], sembuf=kst)

        x_load(0)
        x_load(1)
        P1(0)
        for t in range(NT):
            if t + 2 < NT:
                x_load(t + 2)
            if t + 1 < NT:
                P1(t + 1)
            P2(t)
            if t >= 1:
                P3(t - 1)
        P3(NT - 1)

        fw.pop(S1)
        S2 = fw.push()
        tposc = fw.sb("tposc", [NT, 1], F32)
        tposr = fw.sb("tposr", [NT, 1, NT], F32)
        Bm = fw.sb("Bm", [NT, NT], F32)
        Tt = fw.sb("Tt", [NT, 8], F32)
        Rm = fw.sb("Rm", [NT, NT, 8], F32)
        cc_ = fw.sb("cc", [128, NT, 8], F32)
        rbc = fw.sb("rbc", [128, 4, 8], F32)
        qfirst = fw.sb("qfirst_sb", [128, 1, 4], F32)
        visb = fw.sb("visb", [128, 4, 12], F32)
        bfox = fw.sb("bfox", [128, 4, NT, 8], F32)
        sel0 = fw.sb("sel0", [128, 128], F32)
        ld(tposc, tpos_in[:, :])
        fw.dma(sp, tposr[:], kpos[0:1, :].partition_broadcast(NT), writes=[tposr])
        fw.op(dve, lambda: V.tensor_scalar(out=Bm[:], in0=tposr[:, 0, :], scalar1=tposc[:, 0:1], scalar2=None, op0=ALU.is_gt), reads=[tposr, tposc], writes=[Bm])
        for h in range(8):
            fw.op(pe, lambda h=h: PE.matmul(PS[0][0:NT, h:h + 1], lhsT=logf[:, :, h], rhs=ones32[:, 0:1], start=True, stop=True), reads=[logf, ones32], writes=[PS[0]], inc=(h == 7))
        fw.op(dve, lambda: V.tensor_copy(out=Tt[:], in_=PS[0][0:NT, 0:8]), reads=[PS[0]], writes=[Tt])
        fw.op(dve, lambda: V.tensor_tensor(out=Rm[:], in0=Bm[:].unsqueeze(2).broadcast_to([NT, NT, 8]), in1=Tt[:].unsqueeze(1).broadcast_to([NT, NT, 8]), op=ALU.mult),
              reads=[Bm, Tt], writes=[Rm])
        fw.op(pe, lambda: PE.matmul(PS[1][:, :], lhsT=Umat[:], rhs=logf[:].rearrange("p t h -> p (t h)"), start=True, stop=False), reads=[Umat, logf], writes=[PS[1]], inc=False)
        fw.op(pe, lambda: PE.matmul(PS[1][:, :], lhsT=ones32[0:NT, :], rhs=Rm[:].rearrange("p t h -> p (t h)"), start=False, stop=True), reads=[ones32, Rm], writes=[PS[1]])
        fw.op(dve, lambda: V.tensor_copy(out=cc_[:].rearrange("p t h -> p (t h)"), in_=PS[1][:, :]), reads=[PS[1]], writes=[cc_])
        fw.op(dve, lambda: V.tensor_copy(out=sel0[:], in_=mk[:, 2:3].broadcast_to([128, 128])), reads=[mk], writes=[sel0])
        for s in range(4):
            fw.op(pe, lambda s=s: PE.matmul(PS[2][:, s * 8:(s + 1) * 8], lhsT=sel0[:], rhs=cc_[:, 16 * s + 2, :], start=True, stop=True), reads=[sel0, cc_], writes=[PS[2]], inc=(s == 3))
        fw.op(dve, lambda: V.tensor_copy(out=rbc[:].rearrange("p s h -> p (s h)"), in_=PS[2][:, 0:32]), reads=[PS[2]], writes=[rbc])
        fw.dma(sp, qfirst[:], qfirst_in[0:1, :].partition_broadcast(128), writes=[qfirst])
        for s in range(4):
            fw.op(dve, lambda s=s: V.tensor_scalar(out=visb[:, s, :], in0=kpos_sb[:, 16 * s + 4:16 * s + 16], scalar1=qfirst[:, 0, s:s + 1], scalar2=NEGBIG, op0=ALU.is_gt, op1=ALU.mult),
                  reads=[kpos_sb, qfirst], writes=[visb])
            nk = 16 * (s + 1)
            fw.op(dve, lambda s=s, nk=nk: V.tensor_tensor(out=bfox[:, s, 0:nk, :], in0=rbc[:, s, :].unsqueeze(1).broadcast_to([128, nk, 8]), in1=cc_[:, 0:nk, :], op=ALU.subtract),
                  reads=[rbc, cc_], writes=[bfox])
            fw.op(dve, lambda s=s: V.tensor_tensor(out=bfox[:, s, 16 * s + 4:16 * s + 16, :], in0=bfox[:, s, 16 * s + 4:16 * s + 16, :],
                                                   in1=visb[:, s, :].unsqueeze(2).broadcast_to([128, 12, 8]), op=ALU.add), reads=[bfox, visb], writes=[bfox])

        dmask = fw.sb("dmask", [128, 4, 512], BF16)
        fw.dma(pool, dmask[:].rearrange("p a b -> p (a b)"), c_dm[:, :], writes=[dmask])
        KTg = [fw.sb(f"KTg{i}", [128, SEQ], BF16) for i in range(2)]
        Vg = [fw.sb(f"Vg{i}", [128, NT, 130], BF16) for i in range(2)]
        PT = [fw.sb(f"PT{i}", [128, 512], BF16) for i in range(6)]
        qm = [fw.sb(f"qm{i}", [128, 2, 512], BF16) for i in range(2)]
        Ast = [fw.sb(f"Ast{i}", [128, 4, 128], BF16) for i in range(2)]
        od1 = [fw.sb(f"od1_{i}", [128, 4, 64], F32) for i in range(2)]
        od2 = fw.sb("od2", [128, 4, 64], F32)
        sqt = fw.sb("sqt", [128, 4, 64], F32)
        rl4 = [fw.sb(f"rl4_{i}", [128, 4], F32) for i in range(2)]
        ssq4 = fw.sb("ssq4", [128, 4], F32)
        gain_bc = fw.sb("gain_bc", [128, 1, 64], F32)
        fw.dma(sp, gain_bc[:], subln.rearrange("e o -> o e").partition_broadcast(128), writes=[gain_bc])
        fw.op(dve, lambda: V.tensor_scalar(out=gain_bc[:], in0=gain_bc[:], scalar1=1.0 - LAM_INIT, scalar2=None, op0=ALU.mult), reads=[gain_bc], writes=[gain_bc])
        attn_b = Buf("attn_scr")
        PSS = [PS[0], PS[1], PS[2], PS[3]]
        PSO = [PS[4], PS[5], PS[6], PS[7]]
        DSCALE = 32.0 ** -0.5
        FSCALE = 64.0 ** -0.5
        it_i = 0
        pair_i = 0
        qm_i = 0
        ast_i = 0

        def load_group(g):
            fw.dma(sp, KTg[g % 2][:], kT_scr[g, :, :], reads=kT_b, writes=[KTg[g % 2]])
            fw.dma(sp, Vg[g % 2][:].rearrange("p t c -> p (t c)"), v_scr[g, :, :, :].rearrange("p t c -> p (t c)"), reads=v_b, writes=[Vg[g % 2]])

        load_group(0)
        for g in range(8):
            if g + 1 < 8:
                load_group(g + 1)
            kt = KTg[g % 2]
            vg = Vg[g % 2]
            isdiff = g < 4
            for s in range(4):
                nkb = 16 * (s + 1)
                a_t = Ast[ast_i % 2]
                ast_i += 1
                if isdiff:
                    qmt = qm[qm_i % 2]
                    qm_i += 1
                    for m_ in range(2):
                        fw.op(dve, lambda m_=m_, qmt=qmt: V.tensor_scalar(out=qmt[:, m_, :], in0=qT[:, g, s * 512:(s + 1) * 512], scalar1=mk[:, m_:m_ + 1], scalar2=None, op0=ALU.mult),
                              reads=[qT, mk], writes=[qmt])
                for m in ((0, 1) if isdiff else (None,)):
                    pos_ = [PSO[(2 * pair_i) % 4], PSO[(2 * pair_i + 1) % 4]]
                    pair_i += 1
                    if isdiff:
                        qsl = [qmt[0:64, m, :], qmt[64:128, m, :]]
                        qsrc = qmt
                    else:
                        qsl = [qT[0:64, g, s * 512:(s + 1) * 512], qT[64:128, g, s * 512:(s + 1) * 512]]
                        qsrc = qT
                    pend = None

                    def pv(kb, pts, pos_=pos_):
                        for hh in range(2):
                            for i4 in range(4):
                                fw.op(pe, lambda hh=hh, i4=i4: PE.matmul(pos_[hh][:, i4 * 65:(i4 + 1) * 65], lhsT=pts[hh][:, i4 * 128:(i4 + 1) * 128], rhs=vg[:, kb, hh * 65:(hh + 1) * 65],
                                                                       start=(kb == 0 and i4 == 0), stop=(kb == nkb - 1 and i4 == 3)),
                                      reads=[vg, pts[hh]], writes=[pos_[hh]], inc=(i4 == 3))

                    for kb in range(nkb):
                        pss = [PSS[(2 * it_i) % 4], PSS[(2 * it_i + 1) % 4]]
                        pts = [PT[(2 * it_i) % 6], PT[(2 * it_i + 1) % 6]]
                        it_i += 1
                        for hh in range(2):
                            fw.op(pe, lambda hh=hh: PE.matmul(pss[hh][:, :], lhsT=kt[hh * 64:(hh + 1) * 64, kb * 128:(kb + 1) * 128], rhs=qsl[hh], start=True, stop=True),
                                  reads=[kt, qsrc], writes=[pss[hh]])
                        if pend is not None:
                            pv(*pend)
                        band = kb - 16 * s
                        for hh in range(2):
                            if isdiff:
                                if band >= 4:
                                    fw.op(act, lambda hh=hh: A.activation(out=pts[hh][:], in_=pss[hh][:, :], func=AF.Exp, scale=DSCALE, bias=visb[:, s, band - 4:band - 3]),
                                          reads=[pss[hh], visb], writes=[pts[hh]])
                                else:
                                    fw.op(act, lambda hh=hh: A.activation(out=pts[hh][:], in_=pss[hh][:, :], func=AF.Exp, scale=DSCALE), reads=[pss[hh]], writes=[pts[hh]])
                            else:
                                fh = 2 * (g - 4) + hh
                                fw.op(act, lambda hh=hh, fh=fh: A.activation(out=pts[hh][:], in_=pss[hh][:, :], func=AF.Exp, scale=FSCALE, bias=bfox[:, s, kb, fh:fh + 1]),
                                      reads=[pss[hh], bfox], writes=[pts[hh]])
                            if 0 <= band < 4:
                                fw.op(pool, lambda hh=hh: G.tensor_tensor(out=pts[hh][:], in0=pts[hh][:], in1=dmask[:, band, :], op=ALU.mult), reads=[pts[hh], dmask], writes=[pts[hh]])
                        pend = (kb, pts)
                    pv(*pend)
                    for hh in range(2):
                        o3 = pos_[hh][:, 0:260].rearrange("p (i c) -> p i c", c=65)
                        rlt = rl4[hh]
                        fw.op(dve, lambda o3=o3, rlt=rlt: V.reciprocal(out=rlt[:], in_=o3[:, :, 64]), reads=[pos_[hh]], writes=[rlt])
                        rb = rlt[:].unsqueeze(2).broadcast_to([128, 4, 64])
                        if not isdiff:
                            fw.op(dve, lambda o3=o3, rb=rb, hh=hh: V.tensor_tensor(out=a_t[:, :, hh * 64:(hh + 1) * 64], in0=o3[:, :, 0:64], in1=rb, op=ALU.mult), reads=[pos_[hh], rlt], writes=[a_t])
                        elif m == 0:
                            fw.op(dve, lambda o3=o3, rb=rb, hh=hh: V.tensor_tensor(out=od1[hh][:], in0=o3[:, :, 0:64], in1=rb, op=ALU.mult), reads=[pos_[hh], rlt], writes=[od1[hh]])
                        else:
                            fw.op(dve, lambda o3=o3, rb=rb: V.tensor_tensor(out=od2[:], in0=o3[:, :, 0:64], in1=rb, op=ALU.mult), reads=[pos_[hh], rlt], writes=[od2])
                            fw.op(dve, lambda hh=hh: V.scalar_tensor_tensor(out=od2[:].rearrange("p a b -> p (a b)"), in0=od2[:].rearrange("p a b -> p (a b)"), scalar=lamt[:, 0:1],
                                                                         in1=od1[hh][:].rearrange("p a b -> p (a b)"), op0=ALU.mult, op1=ALU.add), reads=[od2, od1[hh], lamt], writes=[od2])
                            fw.op(dve, lambda: V.tensor_tensor(out=sqt[:], in0=od2[:], in1=od2[:], op=ALU.mult), reads=[od2], writes=[sqt])
                            fw.op(dve, lambda: V.tensor_reduce(out=ssq4[:], in_=sqt[:], axis=AX.X, op=ALU.add), reads=[sqt], writes=[ssq4])
                            fw.op(act, lambda: A.activation(out=ssq4[:], in_=ssq4[:], func=AF.Sqrt, scale=1.0 / 64.0, bias=RMS_EPS), reads=[ssq4], writes=[ssq4])
                            fw.op(dve, lambda: V.reciprocal(out=ssq4[:], in_=ssq4[:]), reads=[ssq4], writes=[ssq4])
                            fw.op(dve, lambda: V.tensor_tensor(out=od2[:], in0=od2[:], in1=ssq4[:].unsqueeze(2).broadcast_to([128, 4, 64]), op=ALU.mult), reads=[od2, ssq4], writes=[od2])
                            fw.op(dve, lambda hh=hh: V.tensor_tensor(out=a_t[:, :, hh * 64:(hh + 1) * 64], in0=od2[:], in1=gain_bc[:].broadcast_to([128, 4, 64]), op=ALU.mult), reads=[od2, gain_bc], writes=[a_t])
                c0 = g * 128
                fw.dma(sp, attn_scr[s * 512:(s + 1) * 512, c0:c0 + 128].rearrange("(i p) c -> p i c", p=128), a_t[:], reads=[a_t], writes=[attn_b], sembuf=a_t)

        fw.pop(S2)
        fw.pop(S12)
        S345 = fw.push()
        NTOK = 2048 + 16
        hT = fw.sb("hT", [128, 8, NTOK], BF16)
        yacc = fw.sb("yacc", [128, 17, D], F32)
        gates = fw.sb("gates", [128, 17, NE + 1], F32)
        fw.op(dve, lambda: V.memset(gates[:], 1.0), writes=[gates])
        stats = fw.sb("stats", [128, 2, 6], F32)
        mv = fw.sb("mv", [128, 2], F32)
        rs = fw.sb("rs", [128, 1], F32)
        S3 = fw.push()
        wob = fw.sb("wob", [128, 8, D], BF16)
        fw.dma(pool, wob[:], w_o.rearrange("(k p) d -> p k d", p=128), writes=[wob])
        AT = [fw.sb(f"AT{i}", [128, 8, 128], BF16) for i in range(2)]
        g1 = fw.sb("g1", [128, 1, D], F32)
        b1 = fw.sb("b1", [128, 1, D], F32)
        fw.dma(sp, g1[:], ln1[0:1, :].partition_broadcast(128), writes=[g1])
        fw.dma(sp, b1[:], ln1[1:2, :].partition_broadcast(128), writes=[b1])
        wr32 = fw.sb("wr32", [128, 8, NE], F32)
        fw.dma(sp, wr32[:], w_router.rearrange("(k p) e -> p k e", p=128), writes=[wr32])
        rb_bc = fw.sb("rb_bc", [128, 1, NE], F32)
        fw.dma(sp, rb_bc[:], r_bias[0:1, :].partition_broadcast(128), writes=[rb_bc])
        att = [fw.sb(f"att{i}", [128, D], BF16) for i in range(2)]
        xres = [fw.sb(f"xres{i}", [128, D], F32) for i in range(2)]
        pre = fw.sb("pre", [128, D], F32)
        hT32 = fw.sb("hT32", [128, 8, 128], F32)
        sc = fw.sb("sc", [128, NE], F32)
        chs = fw.sb("chs", [128, NE], F32)
        ch2 = fw.sb("ch2", [128, NE], F32)
        eqm = fw.sb("eqm", [128, NE], F32)
        m1 = fw.sb("m1", [128, 8], F32)
        m2 = fw.sb("m2", [128, 8], F32)
        gs = fw.sb("gs", [128, 8], F32)
        top8 = fw.sb("top8", [128, 8], F32)
        gmask = fw.sb("gmask", [128, 8], F32)
        den = fw.sb("den", [128, 1], F32)

        def layer_norm(src, gam, bet, dst, n):
            for hf in range(2):
                fw.op(dve, lambda hf=hf: V.bn_stats(out=stats[0:n, hf, :], in_=src[0:n, hf * 512:(hf + 1) * 512]), reads=[src], writes=[stats])
            fw.op(dve, lambda: V.bn_aggr(out=mv[0:n, :], in_=stats[0:n, :, :].rearrange("p a b -> p (a b)")), reads=[stats], writes=[mv])
            fw.op(act, lambda: A.activation(out=rs[0:n, :], in_=mv[0:n, 1:2], func=AF.Sqrt, bias=LN_EPS), reads=[mv], writes=[rs])
            fw.op(dve, lambda: V.reciprocal(out=rs[0:n, :], in_=rs[0:n, :]), reads=[rs], writes=[rs])
            fw.op(dve, lambda: V.tensor_scalar(out=dst[0:n, :], in0=src[0:n, :], scalar1=mv[0:n, 0:1], scalar2=rs[0:n, 0:1], op0=ALU.subtract, op1=ALU.mult),
                  reads=[src, mv, rs], writes=[dst])
            fw.op(dve, lambda: V.tensor_tensor(out=dst[0:n, :], in0=dst[0:n, :], in1=gam[0:n, 0, :], op=ALU.mult), reads=[dst, gam], writes=[dst])
            fw.op(dve, lambda: V.tensor_tensor(out=dst[0:n, :], in0=dst[0:n, :], in1=bet[0:n, 0, :], op=ALU.add), reads=[dst, bet], writes=[dst])

        hbuf = fw.sb("hbuf", [128, D], F32)
        pre2 = [pre, fw.sb("pre_b", [128, D], F32)]

        def stageA(ti):
            n = 128 if ti < 16 else 16
            t0 = ti * 128
            pre = pre2[ti % 2]
            xr = xres[ti % 2]
            at_t = att[ti % 2]
            if ti < 16:
                fw.dma(sp, at_t[:], attn_scr[t0:t0 + 128, :], reads=[attn_b], writes=[at_t])
            else:
                fw.dma(pool, at_t[0:16, :], attn_s_in[:, :], writes=[at_t])
            att_T = AT[ti % 2]
            pab = PS[6 + ti % 2][:].bitcast(BF16)
            for kc in range(8):
                fw.op(pe, lambda kc=kc: PE.transpose(out=pab[:, kc * 128:kc * 128 + n], in_=at_t[0:n, kc * 128:(kc + 1) * 128], identity=identb[0:n, 0:n]),
                      reads=[at_t, identb], writes=[PS[6 + ti % 2]], inc=(kc == 7))
            fw.op(act, lambda: A.activation(out=att_T[:, :, 0:n], in_=pab[:, 0:1024].rearrange("p (a b) -> p a b", b=128)[:, :, 0:n], func=AF.Copy), reads=[PS[6 + ti % 2]], writes=[att_T])
            if ti < 16:
                so, j = divmod(ti, 4)
                gt = (so * 16 + j)
                fw.dma(sp, xr[:], x_perm[gt * 128:(gt + 1) * 128, :], writes=[xr])
            else:
                fw.dma(sp, xr[0:16, :], x_s[:, :], writes=[xr])
            for hf in range(2):
                py = PS[hf]
                for h in range(8):
                    fw.op(pe, lambda h=h, py=py, hf=hf: PE.matmul(py[0:n, :], lhsT=att_T[:, h, 0:n], rhs=wob[:, h, hf * 512:(hf + 1) * 512], start=(h == 0), stop=(h == 7)),
                          reads=[att_T, wob], writes=[py], inc=(h == 7))
                fw.op(dve, lambda py=py, hf=hf: V.scalar_tensor_tensor(out=pre[0:n, hf * 512:(hf + 1) * 512], in0=xr[0:n, hf * 512:(hf + 1) * 512], scalar=DEEP_ALPHA,
                                                                       in1=py[0:n, :], op0=ALU.mult, op1=ALU.add), reads=[xr, py], writes=[pre])
        def stageB(ti):
            n = 128 if ti < 16 else 16
            t0 = ti * 128
            pre = pre2[ti % 2]
            layer_norm(pre, g1, b1, hbuf, n)
            fw.op(act, lambda: A.mul(out=yacc[0:n, ti, :], in_=hbuf[0:n, :], mul=DEEP_ALPHA), reads=[hbuf], writes=[yacc])
            for kc in range(8):
                pt_ = PS[2 + kc // 4]
                fw.op(pe, lambda kc=kc, pt_=pt_: PE.transpose(out=pt_[:, (kc % 4) * 128:(kc % 4) * 128 + n], in_=hbuf[0:n, kc * 128:(kc + 1) * 128], identity=ident[0:n, 0:n]),
                      reads=[hbuf, ident], writes=[pt_], inc=(kc % 4 == 3))
            for hb in range(2):
                pt_ = PS[2 + hb]
                fw.op(act, lambda pt_=pt_, hb=hb: A.activation(out=hT32[:, hb * 4:hb * 4 + 4, 0:n], in_=pt_[:, :].rearrange("p (a b) -> p a b", b=128)[:, :, 0:n], func=AF.Copy),
                      reads=[pt_], writes=[hT32])
            fw.op(pool, lambda: G.tensor_copy(out=hT[:, :, t0:t0 + n], in_=hT32[:, :, 0:n]), reads=[hT32], writes=[hT])
            pr = PS[4]
            for kc in range(8):
                fw.op(pe, lambda kc=kc: PE.matmul(pr[0:n, 0:NE], lhsT=hT32[:, kc, 0:n], rhs=wr32[:, kc, :], start=(kc == 0), stop=(kc == 7)), reads=[hT32, wr32], writes=[pr], inc=(kc == 7))
            fw.op(act, lambda: A.activation(out=sc[0:n, :], in_=pr[0:n, 0:NE], func=AF.Sigmoid), reads=[pr], writes=[sc])
            fw.op(dve, lambda: V.tensor_tensor(out=chs[0:n, :], in0=sc[0:n, :], in1=rb_bc[0:n, 0, :], op=ALU.add), reads=[sc, rb_bc], writes=[chs])
            c3 = chs[0:n, :].rearrange("p (g k) -> p g k", k=8)
            fw.op(dve, lambda: V.tensor_reduce(out=m1[0:n, :], in_=c3, axis=AX.X, op=ALU.max), reads=[chs], writes=[m1])
            fw.op(dve, lambda: V.tensor_tensor(out=eqm[0:n, :].rearrange("p (g k) -> p g k", k=8), in0=c3, in1=m1[0:n, :].unsqueeze(2).broadcast_to([n, 8, 8]), op=ALU.is_ge), reads=[chs, m1], writes=[eqm])
            fw.op(dve, lambda: V.scalar_tensor_tensor(out=ch2[0:n, :], in0=eqm[0:n, :], scalar=-1e30, in1=chs[0:n, :], op0=ALU.mult, op1=ALU.add), reads=[eqm, chs], writes=[ch2])
            fw.op(dve, lambda: V.tensor_reduce(out=m2[0:n, :], in_=ch2[0:n, :].rearrange("p (g k) -> p g k", k=8), axis=AX.X, op=ALU.max), reads=[ch2], writes=[m2])
            fw.op(dve, lambda: V.tensor_tensor(out=gs[0:n, :], in0=m1[0:n, :], in1=m2[0:n, :], op=ALU.add), reads=[m1, m2], writes=[gs])
            fw.op(dve, lambda: V.max(out=top8[0:n, :], in_=gs[0:n, :]), reads=[gs], writes=[top8])
            fw.op(dve, lambda: V.tensor_scalar(out=gmask[0:n, :], in0=gs[0:n, :], scalar1=top8[0:n, 3:4], scalar2=None, op0=ALU.is_ge), reads=[gs, top8], writes=[gmask])
            fw.op(dve, lambda: V.tensor_tensor(out=ch2[0:n, :].rearrange("p (g k) -> p g k", k=8), in0=c3, in1=gmask[0:n, :].unsqueeze(2).broadcast_to([n, 8, 8]), op=ALU.mult), reads=[chs, gmask], writes=[ch2])
            fw.op(dve, lambda: V.tensor_scalar(out=eqm[0:n, 0:8], in0=gmask[0:n, :], scalar1=-1.0, scalar2=1e30, op0=ALU.add, op1=ALU.mult), reads=[gmask], writes=[eqm])
            fw.op(dve, lambda: V.tensor_tensor(out=ch2[0:n, :].rearrange("p (g k) -> p g k", k=8), in0=ch2[0:n, :].rearrange("p (g k) -> p g k", k=8),
                                               in1=eqm[0:n, 0:8].unsqueeze(2).broadcast_to([n, 8, 8]), op=ALU.add), reads=[ch2, eqm], writes=[ch2])
            fw.op(dve, lambda: V.max(out=top8[0:n, :], in_=ch2[0:n, :]), reads=[ch2], writes=[top8])
            fw.op(dve, lambda: V.tensor_scalar(out=eqm[0:n, :], in0=ch2[0:n, :], scalar1=top8[0:n, 7:8], scalar2=None, op0=ALU.is_ge), reads=[ch2, top8], writes=[eqm])
            fw.op(dve, lambda: V.tensor_tensor(out=ch2[0:n, :], in0=eqm[0:n, :], in1=sc[0:n, :], op=ALU.mult), reads=[eqm, sc], writes=[ch2])
            fw.op(dve, lambda: V.tensor_reduce(out=den[0:n, :], in_=ch2[0:n, :], axis=AX.X, op=ALU.add), reads=[ch2], writes=[den])
            fw.op(dve, lambda: V.tensor_scalar(out=den[0:n, :], in0=den[0:n, :], scalar1=1e-20, scalar2=None, op0=ALU.add), reads=[den], writes=[den])
            fw.op(dve, lambda: V.reciprocal(out=den[0:n, :], in_=den[0:n, :]), reads=[den], writes=[den])
            fw.op(dve, lambda: V.tensor_scalar(out=gates[0:n, ti, 0:NE], in0=ch2[0:n, :], scalar1=den[0:n, 0:1], scalar2=2.5, op0=ALU.mult, op1=ALU.mult), reads=[ch2, den], writes=[gates])

        stageA(0)
        for ti in range(17):
            if ti + 1 < 17:
                stageA(ti + 1)
            stageB(ti)
        fw.pop(S3)
        S4 = fw.push()
        wgb = [fw.sb(f"wgb{i}", [128, 8, 256], BF16) for i in range(2)]
        wub = [fw.sb(f"wub{i}", [128, 8, 256], BF16) for i in range(2)]
        wdb = [fw.sb(f"wdb{i}", [128, 2, D], BF16) for i in range(2)]
        sa = [fw.sb(f"sa{i}", [128, 512], F32) for i in range(2)]
        actb = [fw.sb(f"actb{i}", [128, 2, 512], BF16) for i in range(2)]
        PA = [PS[0], PS[1]]
        PU = [PS[2], PS[3]]
        PY = [[PS[4], PS[5]], [PS[6], PS[7]]]

        wg32 = [fw.sb(f"wg32_{i}", [128, 8, 256], F32) for i in range(2)]
        wu32 = [fw.sb(f"wu32_{i}", [128, 8, 256], F32) for i in range(2)]
        wd32 = [fw.sb(f"wd32_{i}", [128, 2, D], F32) for i in range(2)]

        def load_expert(e):
            i = e % 2
            fw.dma(sp, wg32[i][:], w_g[e].rearrange("(k p) f -> p k f", p=128), writes=[wg32[i]])
            fw.dma(sp, wu32[i][:], w_u[e].rearrange("(k p) f -> p k f", p=128), writes=[wu32[i]])
            fw.dma(sp, wd32[i][:], w_d[e].rearrange("(k p) d -> p k d", p=128), writes=[wd32[i]])

        def cast_expert(e):
            i = e % 2
            fw.op(pool, lambda: G.tensor_copy(out=wgb[i][:].rearrange("p a b -> p (a b)"), in_=wg32[i][:].rearrange("p a b -> p (a b)")), reads=[wg32[i]], writes=[wgb[i]])
            fw.op(act, lambda: A.activation(out=wub[i][:].rearrange("p a b -> p (a b)"), in_=wu32[i][:].rearrange("p a b -> p (a b)"), func=AF.Copy), reads=[wu32[i]], writes=[wub[i]])
            fw.op(act, lambda: A.activation(out=wdb[i][:].rearrange("p a b -> p (a b)"), in_=wd32[i][:].rearrange("p a b -> p (a b)"), func=AF.Copy), reads=[wd32[i]], writes=[wdb[i]])

        load_expert(0)
        cast_expert(0)
        load_expert(1)
        chunks = [(c * 512, 512) for c in range(4)] + [(2048, 16)]
        cnt = {"au": 0, "y": 0, "ck": 0}

        def gate_up(e, c0, cn):
            i = e % 2
            ab = actb[cnt["ck"] % 2]
            cnt["ck"] += 1
            for fc in range(2):
                pa = PA[cnt["au"] % 2]
                pu = PU[cnt["au"] % 2]
                sat = sa[cnt["au"] % 2]
                cnt["au"] += 1
                for kc in range(8):
                    fw.op(pe, lambda kc=kc: PE.matmul(pa[:, 0:cn], lhsT=wgb[i][:, kc, fc * 128:(fc + 1) * 128], rhs=hT[:, kc, c0:c0 + cn], start=(kc == 0), stop=(kc == 7)),
                          reads=[wgb[i], hT], writes=[pa], inc=(kc == 7))
                for kc in range(8):
                    fw.op(pe, lambda kc=kc: PE.matmul(pu[:, 0:cn], lhsT=wub[i][:, kc, fc * 128:(fc + 1) * 128], rhs=hT[:, kc, c0:c0 + cn], start=(kc == 0), stop=(kc == 7)),
                          reads=[wub[i], hT], writes=[pu], inc=(kc == 7))
                fw.op(act, lambda: A.activation(out=sat[:, 0:cn], in_=pa[:, 0:cn], func=AF.Silu), reads=[pa], writes=[sat])
                fw.op(dve, lambda: V.tensor_tensor(out=ab[:, fc, 0:cn], in0=sat[:, 0:cn], in1=pu[:, 0:cn], op=ALU.mult), reads=[sat, pu], writes=[ab])
            return ab

        def down(e, c0, cn, ab):
            i = e % 2
            nsub = max(1, cn // 128)
            for sb_ in range(nsub):
                n = min(128, cn)
                ti = c0 // 128 + sb_
                py = PY[cnt["y"] % 2]
                cnt["y"] += 1
                for hf in range(2):
                    for fc in range(2):
                        fw.op(pe, lambda fc=fc: PE.matmul(py[hf][0:n, :], lhsT=ab[:, fc, sb_ * 128:sb_ * 128 + n], rhs=wdb[i][:, fc, hf * 512:(hf + 1) * 512], start=(fc == 0), stop=(fc == 1)),
                              reads=[ab, wdb[i]], writes=[py[hf]], inc=(fc == 1))
                    fw.op(dve, lambda: V.scalar_tensor_tensor(out=yacc[0:n, ti, hf * 512:(hf + 1) * 512], in0=py[hf][0:n, :], scalar=gates[0:n, ti, e:e + 1],
                                                              in1=yacc[0:n, ti, hf * 512:(hf + 1) * 512], op0=ALU.mult, op1=ALU.add),
                          reads=[py[hf], gates, yacc], writes=[yacc])

        prev = None
        for e in range(NE + 1):
            for (c0, cn) in chunks:
                ab = gate_up(e, c0, cn)
                if prev is not None:
                    down(*prev)
                prev = (e, c0, cn, ab)
                if c0 == 0 and e + 1 <= NE:
                    cast_expert(e + 1)
                    if e + 2 <= NE:
                        load_expert(e + 2)
        down(*prev)


        fw.pop(S4)
        S5 = fw.push()
        g2 = fw.sb("g2", [128, 1, D], F32)
        b2 = fw.sb("b2", [128, 1, D], F32)
        fw.dma(sp, g2[:], ln2[0:1, :].partition_broadcast(128), writes=[g2])
        fw.dma(sp, b2[:], ln2[1:2, :].partition_broadcast(128), writes=[b2])
        yo = [fw.sb(f"yo{i}", [128, D], F32) for i in range(2)]
        ysrc = [fw.sb(f"ysrc{i}", [128, D], F32) for i in range(2)]
        for ti in range(17):
            n = 128 if ti < 16 else 16
            o_t = yo[ti % 2]
            s_t = ysrc[ti % 2]
            fw.op(act, lambda s_t=s_t, ti=ti, n=n: A.activation(out=s_t[0:n, :], in_=yacc[0:n, ti, :], func=AF.Copy), reads=[yacc], writes=[s_t])
            layer_norm(s_t, g2, b2, o_t, n)
            if ti < 16:
                fw.dma(sp, y_own[ti * 128:(ti + 1) * 128, :], o_t[:], reads=[o_t], writes=[Buf("o")], sembuf=o_t, final=True)
            else:
                fw.dma(sp, y_s[:, :], o_t[0:16, :], reads=[o_t], writes=[Buf("o")], sembuf=o_t, final=True)
        fw.finish()
        fw.pop(S5)
        fw.pop(S345)
        print("main program: instructions", fw.ninst, "semaphores", fw.nsem)
    return nc


NPOOL = 2560
_NC_CACHE = {}


def host_consts_A():
    p = np.arange(128)
    SU = (p[:, None] > p[None, :]).astype(np.float32)
    j = np.arange(64)
    SUP = (j[:, None] > j[None, :]).astype(np.float32)
    rm = np.zeros((128, 4), np.float32)
    rm[:, 3] = p
    rm[:, 0] = (p < 32)
    rm[:, 1] = (p >= 32) & (p < 64)
    rm[:, 2] = (p >= 64)
    mN = np.zeros((128, 32, 12), np.float32)
    for sq in range(32):
        for i in range(4):
            for ip in range(i + 1):
                mN[sq * 4 + ip, sq, [i, 4 + i, 8 + i]] = 1.0
    E = np.zeros((3, 12, 32, 128), np.float32)
    for sq in range(32):
        for i in range(4):
            E[0, i, sq, sq * 4 + i] = 1.0
            E[1, 4 + i, sq, sq * 4 + i] = 1.0
            E[2, 8 + i, sq, sq * 4 + i] = 1.0
    t = np.arange(128)
    PN = ((t[:, None] // 4 == t[None, :] // 4) & (t[:, None] <= t[None, :])).astype(np.float32)
    return SU, SUP, rm, mN.reshape(128, 384), E.reshape(3, 12, 4096), PN


def build_sample():
    nc = bass.Bass("TRN2", target_bir_lowering=False)

    def din(name, shape, dt=F32):
        return nc.dram_tensor(name, list(shape), dt, kind="ExternalInput").ap()

    def dout(name, shape, dt=F32):
        return nc.dram_tensor(name, list(shape), dt, kind="ExternalOutput").ap()

    xs = din("xs", [128, D])
    w_s = din("w_s", [D, 385])
    bf1 = din("bf1", [1, 1])
    lam4 = din("lam4", [4, 32])
    subln = din("subln", [1, 64])
    spos = din("spos", [128, 1])
    ptb_in = din("ptb", [1, 2048], I32)
    ptP_in = din("ptP", [128, 16], I32)
    kv_pool = din("kv_pool", [NPOOL * 128, 256])
    lf_pool = din("lf_pool", [NPOOL, 128])
    c_ident = din("c_ident", [128, 128])
    c_SU = din("c_SU", [128, 128])
    c_SUP = din("c_SUP", [64, 64])
    c_rm = din("c_rm", [128, 4])
    c_mN = din("c_mN", [128, 384])
    c_E = din("c_E", [3, 12, 4096])
    c_PN = din("c_PN", [128, 128])
    c_invf = din("c_invf", [128, 4])

    attn_c = dout("attn_c", [128, 128])
    nkd = dout("nkd", [128, 64])
    nvd = dout("nvd", [128, 64])
    nkf = dout("nkf", [128, 64])
    nvf = dout("nvf", [128, 64])
    nlf = dout("nlf", [128, 1])

    with ExitStack() as st:
        fw = FW(nc, st)
        pe, act, dve, pool, sp = fw.pe, fw.act, fw.dve, fw.pool, fw.sp
        V = nc.vector
        A = nc.scalar
        G = nc.gpsimd
        PE = nc.tensor
        PS = [fw.ps(f"ps{i}", [128, 512], F32) for i in range(8)]
        ld = lambda t, src, q=sp: fw.dma(q, t[:], src, writes=[t])

        ident = fw.sb("ident", [128, 128], F32)
        identb = fw.sb("identb", [128, 128], BF16)
        SU = fw.sb("SU", [128, 128], F32)
        SUP = fw.sb("SUP", [64, 64], F32)
        rm = fw.sb("rm", [128, 4], F32)
        mN = fw.sb("mN", [128, 32, 12], F32)
        PN = fw.sb("PN", [128, 128], F32)
        invf = fw.sb("invf", [128, 4], F32)
        ones32 = fw.sb("ones32", [128, 128], F32)
        ld(ident, c_ident[:, :])
        ld(SU, c_SU[:, :])
        ld(SUP, c_SUP[:, :])
        ld(rm, c_rm[:, :])
        ld(PN, c_PN[:, :])
        ld(invf, c_invf[:, :])
        fw.dma(sp, mN[:].rearrange("p a b -> p (a b)"), c_mN[:, :], writes=[mN])
        fw.op(dve, lambda: V.tensor_copy(out=identb[:], in_=ident[:]), reads=[ident], writes=[identb])
        fw.op(dve, lambda: V.memset(ones32[:], 1.0), writes=[ones32])

        lamv = fw.sb("lamv", [128, 4, 32], F32)
        fw.dma(sp, lamv[:].rearrange("p a b -> p (a b)"), lam4.rearrange("(o a) b -> o (a b)", o=1).partition_broadcast(128).squeeze(1), writes=[lamv])
        lprod = fw.sb("lprod", [128, 2, 32], F32)
        lsum = fw.sb("lsum", [128, 2], F32)
        lexp = fw.sb("lexp", [128, 2], F32)
        lamt = fw.sb("lamt", [128, 1], F32)
        fw.op(dve, lambda: V.tensor_tensor(out=lprod[:, 0, :], in0=lamv[:, 0, :], in1=lamv[:, 1, :], op=ALU.mult), reads=[lamv], writes=[lprod])
        fw.op(dve, lambda: V.tensor_tensor(out=lprod[:, 1, :], in0=lamv[:, 2, :], in1=lamv[:, 3, :], op=ALU.mult), reads=[lamv], writes=[lprod])
        fw.op(dve, lambda: V.tensor_reduce(out=lsum[:], in_=lprod[:], axis=AX.X, op=ALU.add), reads=[lprod], writes=[lsum])
        fw.op(act, lambda: A.activation(out=lexp[:], in_=lsum[:], func=AF.Exp), reads=[lsum], writes=[lexp])
        fw.op(dve, lambda: V.tensor_tensor(out=lamt[:], in0=lexp[:, 1:2], in1=lexp[:, 0:1], op=ALU.subtract), reads=[lexp], writes=[lamt])
        fw.op(dve, lambda: V.tensor_scalar(out=lamt[:], in0=lamt[:], scalar1=-LAM_INIT, scalar2=None, op0=ALU.add), reads=[lamt], writes=[lamt])
        E0 = fw.sb("E0", [12, 4096], F32)
        E1 = fw.sb("E1", [12, 4096], F32)
        SelF = fw.sb("SelF", [12, 4096], F32)
        SelD = fw.sb("SelD", [12, 4096], F32)
        ld(E0, c_E[0, :, :])
        ld(E1, c_E[1, :, :])
        ld(SelF, c_E[2, :, :])
        fw.op(dve, lambda: V.scalar_tensor_tensor(out=SelD[:], in0=E1[:], scalar=lamt[0:12, 0:1], in1=E0[:], op0=ALU.mult, op1=ALU.add), reads=[E1, E0, lamt], writes=[SelD])

        xsb = fw.sb("xsb", [128, D], BF16)
        xsT = fw.sb("xsT", [128, 8, 128], BF16)
        wsb = fw.sb("wsb", [128, 8, 385], BF16)
        fw.dma(pool, xsb[:], xs[:, :], writes=[xsb])
        fw.dma(pool, wsb[:], w_s.rearrange("(k p) c -> p k c", p=128), writes=[wsb])
        pxb = PS[0][:].bitcast(BF16)
        for kc in range(8):
            fw.op(pe, lambda kc=kc: PE.transpose(out=pxb[:, kc * 128:(kc + 1) * 128], in_=xsb[:, kc * 128:(kc + 1) * 128], identity=identb[:]), reads=[xsb, identb], writes=[PS[0]], inc=(kc == 7))
        fw.op(dve, lambda: V.tensor_copy(out=xsT[:].rearrange("p a b -> p (a b)"), in_=pxb[:, 0:1024]), reads=[PS[0]], writes=[xsT])
        for kc in range(8):
            fw.op(pe, lambda kc=kc: PE.matmul(PS[1][:, 0:385], lhsT=xsT[:, kc, :], rhs=wsb[:, kc, :], start=(kc == 0), stop=(kc == 7)), reads=[xsT, wsb], writes=[PS[1]], inc=(kc == 7))
        z = fw.sb("z", [128, 385], F32)
        fw.op(act, lambda: A.activation(out=z[:], in_=PS[1][:, 0:385], func=AF.Copy), reads=[PS[1]], writes=[z])
        pos = fw.sb("pos", [128, 1], F32)
        ld(pos, spos[:, :])
        ang = fw.sb("ang", [128, 4], F32)
        angk = fw.sb("angk", [128, 4], F32)
        angi = fw.sb("angi", [128, 4], I32)
        angr = fw.sb("angr", [128, 4], F32)
        angc = fw.sb("angc", [128, 4], F32)
        cosS = fw.sb("cosS", [128, 4], F32)
        sinS = fw.sb("sinS", [128, 4], F32)
        TWO_PI = 2.0 * math.pi
        fw.op(dve, lambda: V.tensor_scalar(out=ang[:], in0=invf[:], scalar1=pos[:, 0:1], scalar2=None, op0=ALU.mult), reads=[invf, pos], writes=[ang])

        def reduce_sin(dst, shift):
            fw.op(dve, lambda: V.tensor_scalar(out=angk[:], in0=ang[:], scalar1=shift, scalar2=1.0 / TWO_PI, op0=ALU.add, op1=ALU.mult), reads=[ang], writes=[angk])
            fw.op(dve, lambda: V.tensor_copy(out=angi[:], in_=angk[:]), reads=[angk], writes=[angi])
            fw.op(dve, lambda: V.tensor_copy(out=angk[:], in_=angi[:]), reads=[angi], writes=[angk])
            fw.op(dve, lambda: V.tensor_scalar(out=angr[:], in0=ang[:], scalar1=shift, scalar2=None, op0=ALU.add), reads=[ang], writes=[angr])
            fw.op(dve, lambda: V.scalar_tensor_tensor(out=angr[:], in0=angk[:], scalar=-TWO_PI, in1=angr[:], op0=ALU.mult, op1=ALU.add), reads=[angk, angr], writes=[angr])
            fw.op(dve, lambda: V.tensor_scalar(out=angc[:], in0=angr[:], scalar1=math.pi, scalar2=-TWO_PI, op0=ALU.is_gt, op1=ALU.mult), reads=[angr], writes=[angc])
            fw.op(dve, lambda: V.tensor_tensor(out=angr[:], in0=angr[:], in1=angc[:], op=ALU.add), reads=[angr, angc], writes=[angr])
            fw.op(dve, lambda: V.tensor_scalar(out=angc[:], in0=angr[:], scalar1=-math.pi, scalar2=TWO_PI, op0=ALU.is_lt, op1=ALU.mult), reads=[angr], writes=[angc])
            fw.op(dve, lambda: V.tensor_tensor(out=angr[:], in0=angr[:], in1=angc[:], op=ALU.add), reads=[angr, angc], writes=[angr])
            fw.op(dve, lambda: V.tensor_scalar(out=angr[:], in0=angr[:], scalar1=-3.14159, scalar2=3.14159, op0=ALU.max, op1=ALU.min), reads=[angr], writes=[angr])
            fw.op(act, lambda: A.activation(out=dst[:], in_=angr[:], func=AF.Sin), reads=[angr], writes=[dst])

        reduce_sin(sinS, 0.0)
        reduce_sin(cosS, math.pi / 2.0)
        rt = [fw.sb(f"rt{i}", [128, 8], F32) for i in range(4)]

        def rope64(c0):
            v3 = z[:, c0:c0 + 64].rearrange("p (g d) -> p g d", d=32)
            x1 = v3[:, :, 0:4]
            x2 = v3[:, :, 4:8]
            cb = cosS[:].unsqueeze(1).broadcast_to([128, 2, 4])
            sbb = sinS[:].unsqueeze(1).broadcast_to([128, 2, 4])
            r = [x[:].rearrange("p (g d) -> p g d", d=4) for x in rt]
            fw.op(dve, lambda: V.tensor_tensor(out=r[0], in0=x1, in1=cb, op=ALU.mult), reads=[z, cosS], writes=[rt[0]])
            fw.op(dve, lambda: V.tensor_tensor(out=r[1], in0=x2, in1=sbb, op=ALU.mult), reads=[z, sinS], writes=[rt[1]])
            fw.op(dve, lambda: V.tensor_tensor(out=r[2], in0=x2, in1=cb, op=ALU.mult), reads=[z, cosS], writes=[rt[2]])
            fw.op(dve, lambda: V.tensor_tensor(out=r[3], in0=x1, in1=sbb, op=ALU.mult), reads=[z, sinS], writes=[rt[3]])
            fw.op(dve, lambda: V.tensor_tensor(out=x1, in0=r[0], in1=r[1], op=ALU.subtract), reads=[rt[0], rt[1]], writes=[z])
            fw.op(dve, lambda: V.tensor_tensor(out=x2, in0=r[2], in1=r[3], op=ALU.add), reads=[rt[2], rt[3]], writes=[z])

        rope64(0)
        rope64(64)
        bfc = fw.sb("bfc", [128, 1, 1], F32)
        fw.dma(sp, bfc[:], bf1[0:1, :].partition_broadcast(128), writes=[bfc])
        slf = fw.sb("slf", [128, 1], F32)
        e1 = fw.sb("e1", [128, 1], F32)
        fw.op(dve, lambda: V.tensor_tensor(out=slf[:], in0=z[:, 384:385], in1=bfc[:, 0, :], op=ALU.add), reads=[z, bfc], writes=[slf])
        fw.op(act, lambda: A.activation(out=e1[:], in_=slf[:], func=AF.Exp, scale=-1.0), reads=[slf], writes=[e1])
        fw.op(act, lambda: A.activation(out=slf[:], in_=e1[:], func=AF.Ln, bias=1.0), reads=[e1], writes=[slf])
        fw.op(dve, lambda: V.tensor_scalar(out=slf[:], in0=slf[:], scalar1=-1.0, scalar2=None, op0=ALU.mult), reads=[slf], writes=[slf])
        fw.dma(sp, nkd[:, :], z[:, 64:128], reads=[z], writes=[Buf("o")], sembuf=z, final=True)
        fw.dma(sp, nvd[:, :], z[:, 128:192], reads=[z], writes=[Buf("o")], sembuf=z, final=True)
        fw.dma(sp, nkf[:, :], z[:, 256:320], reads=[z], writes=[Buf("o")], sembuf=z, final=True)
        fw.dma(sp, nvf[:, :], z[:, 320:384], reads=[z], writes=[Buf("o")], sembuf=z, final=True)
        fw.dma(sp, nlf[:, :], slf[:], reads=[slf], writes=[Buf("o")], sembuf=slf, final=True)

        qk = fw.sb("qk", [128, 2, 128], F32)
        fw.op(dve, lambda: V.tensor_scalar(out=qk[:, 0, 0:64], in0=z[:, 0:64], scalar1=32.0 ** -0.5, scalar2=None, op0=ALU.mult), reads=[z], writes=[qk])
        fw.op(dve, lambda: V.tensor_scalar(out=qk[:, 0, 64:128], in0=z[:, 192:256], scalar1=0.125, scalar2=None, op0=ALU.mult), reads=[z], writes=[qk])
        fw.op(dve, lambda: V.tensor_copy(out=qk[:, 1, 0:64], in_=z[:, 64:128]), reads=[z], writes=[qk])
        fw.op(dve, lambda: V.tensor_copy(out=qk[:, 1, 64:128], in_=z[:, 256:320]), reads=[z], writes=[qk])
        pqb = PS[2][:]
        for a in range(2):
            fw.op(pe, lambda a=a: PE.transpose(out=pqb[:, a * 128:(a + 1) * 128], in_=qk[:, a, :], identity=ident[:]), reads=[qk, ident], writes=[PS[2]], inc=(a == 1))
        Qblk = fw.sb("Qblk", [128, 32, 12], F32)
        KTn = fw.sb("KTn", [128, 128], F32)
        for jb in range(3):
            fw.op(dve, lambda jb=jb: V.tensor_scalar(out=Qblk[:, :, jb * 4:(jb + 1) * 4], in0=pqb[:, 0:128].rearrange("p (s i) -> p s i", i=4), scalar1=rm[:, jb:jb + 1], scalar2=None, op0=ALU.mult),
                  reads=[PS[2], rm], writes=[Qblk])
        fw.op(dve, lambda: V.tensor_copy(out=KTn[:], in_=pqb[:, 128:256]), reads=[PS[2]], writes=[KTn])
        Vn = fw.sb("Vn", [128, 129], F32)
        fw.op(dve, lambda: V.memset(Vn[:], 1.0), writes=[Vn])
        fw.op(dve, lambda: V.tensor_copy(out=Vn[:, 0:64], in_=z[:, 128:192]), reads=[z], writes=[Vn])
        fw.op(dve, lambda: V.tensor_copy(out=Vn[:, 64:128], in_=z[:, 320:384]), reads=[z], writes=[Vn])
        biasN = fw.sb("biasN", [128, 1], F32)
        fw.op(pe, lambda: PE.matmul(PS[3][:, 0:1], lhsT=PN[:], rhs=slf[:], start=True, stop=True), reads=[PN, slf], writes=[PS[3]])
        fw.op(dve, lambda: V.tensor_scalar(out=biasN[:], in0=PS[3][:, 0:1], scalar1=-1.0, scalar2=None, op0=ALU.mult), reads=[PS[3]], writes=[biasN])

        ptb = fw.sb("ptb_sb", [128, 1, 2048], I32)
        fw.dma(sp, ptb[:], ptb_in[0:1, :].partition_broadcast(128), writes=[ptb])
        idxf = fw.sb("idxf", [128, 2048], F32)
        idx = fw.sb("idx", [128, 2048], I32)
        fw.op(dve, lambda: V.tensor_copy(out=idxf[:], in_=ptb[:, 0, :]), reads=[ptb], writes=[idxf])
        fw.op(dve, lambda: V.tensor_scalar(out=idxf[:], in0=idxf[:], scalar1=128.0, scalar2=rm[:, 3:4], op0=ALU.mult, op1=ALU.add), reads=[idxf, rm], writes=[idxf])
        fw.op(dve, lambda: V.tensor_copy(out=idx[:], in_=idxf[:]), reads=[idxf], writes=[idx])
        ptP = fw.sb("ptP_sb", [128, 16], I32)
        ld(ptP, ptP_in[:, :])
        LT = [fw.sb(f"LT{i}", [128, 128], F32) for i in range(2)]
        Lall = fw.sb("Lall", [128, 32, 64], F32)
        for i in range(16):
            lt = LT[i % 2]
            fw.dma(pool, lt[:], lf_pool[:, :], reads=[ptP], writes=[lt], indirect=ptP[:, i:i + 1].bitcast(U32))
            pt_ = PS[4 + (i % 2)]
            fw.op(pe, lambda lt=lt, pt_=pt_: PE.transpose(out=pt_[:, 0:128], in_=lt[:], identity=ident[:]), reads=[lt, ident], writes=[pt_])
            fw.op(dve, lambda i=i, pt_=pt_: V.tensor_copy(out=Lall[:, 2 * i:2 * i + 2, :].rearrange("p a b -> p (a b)"), in_=pt_[:, 0:128]), reads=[pt_], writes=[Lall])
        Tcol = fw.sb("Tcol", [64, 32], F32)
        Rm = fw.sb("Rm", [64, 32, 64], F32)
        biasP = fw.sb("biasP", [128, 32, 64], F32)
        for sq in range(32):
            fw.op(pe, lambda sq=sq: PE.matmul(PS[6][0:64, sq:sq + 1], lhsT=Lall[:, sq, :], rhs=ones32[:, 0:1], start=True, stop=True), reads=[Lall, ones32], writes=[PS[6]], inc=(sq == 31))
        fw.op(dve, lambda: V.tensor_copy(out=Tcol[:], in_=PS[6][0:64, 0:32]), reads=[PS[6]], writes=[Tcol])
        fw.op(dve, lambda: V.tensor_tensor(out=Rm[:], in0=Tcol[:].unsqueeze(2).broadcast_to([64, 32, 64]), in1=SUP[:].unsqueeze(1).broadcast_to([64, 32, 64]), op=ALU.mult), reads=[Tcol, SUP], writes=[Rm])
        for q4 in range(4):
            pb = PS[q4 % 2]
            fw.op(pe, lambda q4=q4, pb=pb: PE.matmul(pb[:, :], lhsT=SU[:], rhs=Lall[:, q4 * 8:(q4 + 1) * 8, :].rearrange("p a b -> p (a b)"), start=True, stop=False), reads=[SU, Lall], writes=[pb], inc=False)
            fw.op(pe, lambda q4=q4, pb=pb: PE.matmul(pb[:, :], lhsT=ones32[0:64, :], rhs=Rm[:, q4 * 8:(q4 + 1) * 8, :].rearrange("p a b -> p (a b)"), start=False, stop=True), reads=[ones32, Rm], writes=[pb])
            fw.op(dve, lambda q4=q4, pb=pb: V.tensor_copy(out=biasP[:, q4 * 8:(q4 + 1) * 8, :].rearrange("p a b -> p (a b)"), in_=pb[:, :]), reads=[pb], writes=[biasP])

        NKV = 48
        kv = [fw.sb(f"kv{i}", [128, 257], F32) for i in range(NKV)]
        for t_ in kv:
            fw.op(dve, lambda t_=t_: V.memset(t_[:, 256:257], 1.0), writes=[t_])
        PTt = [fw.sb(f"PTt{i}", [128, 4, 12], F32) for i in range(2)]
        sx = [fw.sb(f"sx{i}", [128, 4, 4], F32) for i in range(2)]
        PTn = fw.sb("PTn", [128, 12], F32)
        sxn = fw.sb("sxn", [128, 4], F32)
        On = [fw.sb(f"On{i}", [12, 129], F32) for i in range(2)]
        rl = [fw.sb(f"rl{i}", [12, 1], F32) for i in range(2)]
        PSS = [PS[0], PS[1]]
        PSO = [PS[2], PS[3]]
        PFD = PS[4]
        PFF = PS[5]
        kv_i = 0
        grp_i = 0
        for sq in range(32):
            po = PSO[sq % 2]
            pend = []

            def flush(pend=pend, po=po):
                for (first, last, lhs, rhs_t, rd) in pend:
                    fw.op(pe, lambda: PE.matmul(po[0:12, 0:129], lhsT=lhs, rhs=rhs_t[:, 128:257] if rhs_t is not Vn else rhs_t[:, :], start=first, stop=last), reads=rd, writes=[po])
                del pend[:]

            for g4 in range(16):
                pss = PSS[grp_i % 2]
                ptt = PTt[grp_i % 2]
                sxt = sx[grp_i % 2]
                grp_i += 1
                tiles = []
                for jj in range(4):
                    j = g4 * 4 + jj
                    t_ = kv[kv_i % NKV]
                    kv_i += 1
                    fw.dma(pool, t_[:, 0:256], kv_pool[:, :], reads=[idx], writes=[t_], indirect=idx[:, sq * 64 + j:sq * 64 + j + 1].bitcast(U32))
                    fw.op(pe, lambda jj=jj, t_=t_, pss=pss: PE.matmul(pss[:, jj * 12:(jj + 1) * 12], lhsT=t_[:, 0:128], rhs=Qblk[:, sq, :], start=True, stop=True), reads=[t_, Qblk], writes=[pss], inc=(jj == 3))
                    tiles.append(t_)
                flush()
                p3 = pss[:, 0:48].rearrange("p (a b) -> p a b", b=12)
                fw.op(dve, lambda: V.tensor_tensor(out=sxt[:], in0=p3[:, :, 8:12], in1=biasP[:, sq, g4 * 4:g4 * 4 + 4].unsqueeze(2).broadcast_to([128, 4, 4]), op=ALU.add), reads=[pss, biasP], writes=[sxt])
                fw.op(act, lambda: A.activation(out=ptt[:, :, 0:8], in_=p3[:, :, 0:8], func=AF.Exp), reads=[pss], writes=[ptt])
                fw.op(act, lambda: A.activation(out=ptt[:, :, 8:12], in_=sxt[:], func=AF.Exp), reads=[sxt], writes=[ptt])
                for jj in range(4):
                    pend.append((g4 == 0 and jj == 0, False, ptt[:, jj, :], tiles[jj], [ptt, tiles[jj]]))
            pss = PSS[grp_i % 2]
            grp_i += 1
            fw.op(pe, lambda: PE.matmul(pss[:, 0:12], lhsT=KTn[:], rhs=Qblk[:, sq, :], start=True, stop=True), reads=[KTn, Qblk], writes=[pss])
            flush()
            fw.op(dve, lambda: V.tensor_scalar(out=sxn[:], in0=pss[:, 8:12], scalar1=biasN[:, 0:1], scalar2=None, op0=ALU.add), reads=[pss, biasN], writes=[sxn])
            fw.op(act, lambda: A.activation(out=PTn[:, 0:8], in_=pss[:, 0:8], func=AF.Exp), reads=[pss], writes=[PTn])
            fw.op(act, lambda: A.activation(out=PTn[:, 8:12], in_=sxn[:], func=AF.Exp), reads=[sxn], writes=[PTn])
            fw.op(dve, lambda: V.tensor_tensor(out=PTn[:], in0=PTn[:], in1=mN[:, sq, :], op=ALU.mult), reads=[PTn, mN], writes=[PTn])
            pend.append((False, True, PTn[:, :], Vn, [PTn, Vn]))
            flush()
            on = On[sq % 2]
            rlt = rl[sq % 2]
            fw.op(dve, lambda: V.reciprocal(out=rlt[:], in_=po[0:12, 128:129]), reads=[po], writes=[rlt])
            fw.op(dve, lambda: V.tensor_scalar(out=on[:], in0=po[0:12, 0:129], scalar1=rlt[:, 0:1], scalar2=None, op0=ALU.mult), reads=[po, rlt], writes=[on])
            fw.op(pe, lambda: PE.matmul(PFD[:, 0:64], lhsT=SelD[:, sq * 128:(sq + 1) * 128], rhs=on[:, 0:64], start=(sq == 0), stop=(sq == 31)), reads=[SelD, on], writes=[PFD])
            fw.op(pe, lambda: PE.matmul(PFF[:, 0:64], lhsT=SelF[:, sq * 128:(sq + 1) * 128], rhs=on[:, 64:128], start=(sq == 0), stop=(sq == 31)), reads=[SelF, on], writes=[PFF])

        res = fw.sb("res", [128, 128], F32)
        od = fw.sb("od", [128, 64], F32)
        sqv = fw.sb("sqv", [128, 64], F32)
        ssq = fw.sb("ssq", [128, 1], F32)
        gb = fw.sb("gb", [128, 1, 64], F32)
        fw.dma(sp, gb[:], subln[0:1, :].partition_broadcast(128), writes=[gb])
        fw.op(dve, lambda: V.tensor_copy(out=od[:], in_=PFD[:, 0:64]), reads=[PFD], writes=[od])
        fw.op(dve, lambda: V.tensor_tensor(out=sqv[:], in0=od[:], in1=od[:], op=ALU.mult), reads=[od], writes=[sqv])
        fw.op(dve, lambda: V.tensor_reduce(out=ssq[:], in_=sqv[:], axis=AX.X, op=ALU.add), reads=[sqv], writes=[ssq])
        fw.op(act, lambda: A.activation(out=ssq[:], in_=ssq[:], func=AF.Sqrt, scale=1.0 / 64.0, bias=RMS_EPS), reads=[ssq], writes=[ssq])
        fw.op(dve, lambda: V.reciprocal(out=ssq[:], in_=ssq[:]), reads=[ssq], writes=[ssq])
        fw.op(dve, lambda: V.tensor_scalar(out=od[:], in0=od[:], scalar1=ssq[:, 0:1], scalar2=1.0 - LAM_INIT, op0=ALU.mult, op1=ALU.mult), reads=[od, ssq], writes=[od])
        fw.op(dve, lambda: V.tensor_tensor(out=res[:, 0:64], in0=od[:], in1=gb[:, 0, :], op=ALU.mult), reads=[od, gb], writes=[res])
        fw.op(dve, lambda: V.tensor_copy(out=res[:, 64:128], in_=PFF[:, 0:64]), reads=[PFF], writes=[res])
        fw.dma(sp, attn_c[:, :], res[:], reads=[res], writes=[Buf("o")], sembuf=res, final=True)
        fw.finish()
        print("sample program: instructions", fw.ninst, "semaphores", fw.nsem)
    return nc


def run_sample(inputs):
    f32 = np.float32
    if "sample" not in _NC_CACHE:
        _NC_CACHE["sample"] = build_sample()
    nc = _NC_CACHE["sample"]
    ident, U, dm, mk, invf = host_consts()
    SU, SUP, rm, mN, E, PN = host_consts_A()
    xs = np.ascontiguousarray(np.asarray(inputs["x_sample"], f32).reshape(128, D))
    w_in = np.asarray(inputs["w_in"][0], f32)
    pt = np.asarray(inputs["page_table"]).astype(np.int32)
    lam4 = np.stack([inputs["lambda_q1"][0], inputs["lambda_k1"][0], inputs["lambda_q2"][0], inputs["lambda_k2"][0]]).astype(f32)
    spos = (PAST + (np.arange(128) % 4)).astype(f32).reshape(128, 1)
    ptb = np.ascontiguousarray(pt.reshape(1, 2048))
    ptP = np.ascontiguousarray(pt.reshape(16, 128).T)
    ck = np.asarray(inputs["cache_diff_k"][0], f32).reshape(NPOOL, 128, 8, 64)
    cv = np.asarray(inputs["cache_diff_v"][0], f32)
    fk = np.asarray(inputs["cache_fox_k"][0], f32)
    fv = np.asarray(inputs["cache_fox_v"][0], f32)
    fl = np.asarray(inputs["cache_fox_logf"][0], f32)
    shared = {"xs": xs, "lam4": lam4, "subln": np.asarray(inputs["subln_gain"], f32).reshape(1, 64), "spos": spos, "ptb": ptb, "ptP": ptP,
              "c_ident": ident, "c_SU": SU, "c_SUP": SUP, "c_rm": rm, "c_mN": mN, "c_E": E, "c_PN": PN, "c_invf": invf}
    in_maps = []
    for c in range(8):
        m = dict(shared)
        cols = np.concatenate([C_QD + c * 64 + np.arange(64), C_KD + c * 64 + np.arange(64), C_VD + c * 64 + np.arange(64),
                               C_QF + c * 64 + np.arange(64), C_KF + c * 64 + np.arange(64), C_VF + c * 64 + np.arange(64), [C_FL + c]])
        m["w_s"] = np.ascontiguousarray(w_in[:, cols])
        m["bf1"] = np.asarray(inputs["b_forget"], f32).reshape(8)[c].reshape(1, 1)
        kvp = np.empty((NPOOL, 128, 256), f32)
        kvp[:, 0:64, 0:128] = ck[:, :, c, :].transpose(0, 2, 1)
        kvp[:, 64:128, 0:128] = fk[:, :, c, :].transpose(0, 2, 1)
        kvp[:, :, 128:192] = cv[:, :, c, :]
        kvp[:, :, 192:256] = fv[:, :, c, :]
        m["kv_pool"] = kvp.reshape(NPOOL * 128, 256)
        m["lf_pool"] = np.ascontiguousarray(fl[:, :, c])
        in_maps.append(m)
    res = run_bass_kernel_spmd(nc, in_maps, core_ids=list(range(8)))
    attn = np.zeros((128, 16, 64), f32)
    nkd = np.zeros((128, 8, 64), f32)
    nvd = np.zeros((128, 8, 64), f32)
    nkf = np.zeros((128, 8, 64), f32)
    nvf = np.zeros((128, 8, 64), f32)
    nlf = np.zeros((128, 8), f32)
    for c in range(8):
        r = res.results[c]
        attn[:, c, :] = r["attn_c"][:, 0:64]
        attn[:, 8 + c, :] = r["attn_c"][:, 64:128]
        nkd[:, c] = r["nkd"]
        nvd[:, c] = r["nvd"]
        nkf[:, c] = r["nkf"]
        nvf[:, c] = r["nvf"]
        nlf[:, c] = r["nlf"][:, 0]
    return (attn.reshape(128, 1024), nkd.reshape(1, 32, 4, 8, 2, 32), nvd.reshape(1, 32, 4, 8, 64), nkf.reshape(1, 32, 4, 8, 64),
            nvf.reshape(1, 32, 4, 8, 64), nlf.reshape(1, 32, 4, 8))


def kernel(**inputs):
    attn_s, nkd, nvd, nkf, nvf, nlf = run_sample(inputs)
    y_p, y_s, kd, vd, kf, vf, lf = run_main(inputs, attn_s)
    return (y_p, y_s, kd, vd, kf, vf, lf, nkd, nvd, nkf, nvf, nlf)


def chunk_order(cc):
    order = []
    for s in range(4):
        order += [4 * s + cc] + [4 * s + j for j in range(4) if j != cc]
    return order


def run_main(inputs, attn_s):
    f32 = np.float32
    if "main" not in _NC_CACHE:
        _NC_CACHE["main"] = build_main()
    nc = _NC_CACHE["main"]
    ident, U, dm, mk, invf = host_consts()
    xp = np.asarray(inputs["x_prompt"], f32)
    xs = np.asarray(inputs["x_sample"], f32).reshape(128, D)
    w_g = np.concatenate([np.asarray(inputs["w_exp_gate"][0], f32), np.asarray(inputs["w_sh_gate"][0], f32)[None]], axis=0)
    w_u = np.concatenate([np.asarray(inputs["w_exp_up"][0], f32), np.asarray(inputs["w_sh_up"][0], f32)[None]], axis=0)
    w_d = np.concatenate([np.asarray(inputs["w_exp_down"][0], f32), np.asarray(inputs["w_sh_down"][0], f32)[None]], axis=0)
    lam4 = np.stack([inputs["lambda_q1"][0], inputs["lambda_k1"][0], inputs["lambda_q2"][0], inputs["lambda_k2"][0]]).astype(f32)
    shared = {
        "w_in": np.ascontiguousarray(inputs["w_in"][0], f32), "b_forget": np.asarray(inputs["b_forget"], f32).reshape(1, 8),
        "lam4": lam4, "subln": np.asarray(inputs["subln_gain"], f32).reshape(64, 1), "w_o": np.ascontiguousarray(inputs["w_o"][0], f32),
        "ln1": np.stack([inputs["ln1_g"][0], inputs["ln1_b"][0]]).astype(f32), "ln2": np.stack([inputs["ln2_g"][0], inputs["ln2_b"][0]]).astype(f32),
        "w_router": np.ascontiguousarray(inputs["w_router"][0], f32), "r_bias": np.asarray(inputs["router_bias"], f32).reshape(1, NE),
        "w_g": w_g, "w_u": w_u, "w_d": w_d,
        "c_ident": ident, "c_U": U, "c_dm": dm, "c_mk": mk, "c_invf": invf,
    }
    in_maps = []
    toks = []
    for c in range(8):
        b, cc = divmod(c, 4)
        tok = np.concatenate([np.arange(ch * 512, (ch + 1) * 512) for ch in chunk_order(cc)])
        toks.append(tok)
        tt = tok.reshape(NT, 128)
        m = dict(shared)
        m["x_perm"] = np.ascontiguousarray(xp[b][tok])
        m["kpos"] = np.ascontiguousarray(tt.T.astype(f32))
        m["tpos"] = np.ascontiguousarray(tt[:, 0:1].astype(f32))
        m["qfirst"] = np.array([[tok[s * 2048] for s in range(4)]], f32)
        m["x_s"] = np.ascontiguousarray(xs[16 * c:16 * c + 16])
        m["attn_s"] = np.ascontiguousarray(attn_s[16 * c:16 * c + 16])
        in_maps.append(m)
    res = run_bass_kernel_spmd(nc, in_maps, core_ids=list(range(8)))
    y_p = np.zeros((2, SEQ, D), f32)
    y_s = np.zeros((128, D), f32)
    kd = np.zeros((2, SEQ, 512), f32)
    vd = np.zeros((2, SEQ, 512), f32)
    kf = np.zeros((2, SEQ, 512), f32)
    vf = np.zeros((2, SEQ, 512), f32)
    lf = np.zeros((2, SEQ, 8), f32)
    for c in range(8):
        b, cc = divmod(c, 4)
        r = res.results[c]
        own = np.concatenate([toks[c][s * 2048:s * 2048 + 512] for s in range(4)])
        y_p[b, own] = r["y_own"]
        kd[b, own] = r["kd_own"]
        vd[b, own] = r["vd_own"]
        kf[b, own] = r["kf_own"]
        vf[b, own] = r["vf_own"]
        lf[b, own] = r["lf_own"]
        y_s[16 * c:16 * c + 16] = r["y_s"]
    return (y_p, y_s.reshape(32, 4, D), kd.reshape(1, 2, SEQ, 8, 2, 32), vd.reshape(1, 2, SEQ, 8, 64),
            kf.reshape(1, 2, SEQ, 8, 64), vf.reshape(1, 2, SEQ, 8, 64), lf.reshape(1, 2, SEQ, 8))


if __name__ == "__main__":
    build_sample()
    build_main()
```

```python
import math
import numpy as np
from contextlib import ExitStack
import concourse.bass as bass
import concourse.mybir as mybir
from concourse.bass_utils import run_bass_kernel_spmd

F32 = mybir.dt.float32
BF16 = mybir.dt.bfloat16
I32 = mybir.dt.int32
U32 = mybir.dt.uint32
AF = mybir.ActivationFunctionType
ALU = mybir.AluOpType
AX = mybir.AxisListType

D = 1024
SEQ = 8192
NT = 64
NCH = 16
NOWN = 16
NE = 64
DEEP_ALPHA = 2.0 ** 0.25
LAM_INIT = 0.8 - 0.6 * math.exp(0.0)
LN_EPS = 1e-5
RMS_EPS = 1e-5
ROPE_THETA = 500000.0
C_QD, C_KD, C_VD, C_QF, C_KF, C_VF, C_FL = 0, 512, 1024, 1536, 2048, 2560, 3072
D_IN = 3080
NEGBIG = -30000.0
PAST = 8192


class Buf:
    __slots__ = ("name", "lw", "rd", "sem", "semval")

    def __init__(self, name):
        self.name = name
        self.lw = None
        self.rd = {}
        self.sem = None
        self.semval = 0


class Eng:
    def __init__(self, name, handle, sem):
        self.name = name
        self.h = handle
        self.sem = sem
        self.count = 0
        self.waited = {}
        self.same_engine_sync = name in ("act", "dve", "pool")


class T:
    def __init__(self, t, name):
        self.t = t
        self.b = Buf(name)

    def __getitem__(self, k):
        return self.t[k]


class FW:
    def __init__(self, nc, stack):
        self.nc = nc
        self.stack = stack
        self.root = stack
        self.nsem = 0
        self.dma_bufs = []
        self.pe = Eng("pe", nc.tensor, self.new_sem("pe"))
        self.act = Eng("act", nc.scalar, self.new_sem("act"))
        self.dve = Eng("dve", nc.vector, self.new_sem("dve"))
        self.pool = Eng("pool", nc.gpsimd, self.new_sem("pool"))
        self.sp = Eng("sp", nc.sync, self.new_sem("sp"))
        self.ninst = 0
        self.out_events = []

    def new_sem(self, name):
        s = self.root.enter_context(self.nc.semaphore(name))
        self.nsem += 1
        return s

    def sb(self, name, shape, dt):
        return T(self.stack.enter_context(self.nc.sbuf_tensor(name, list(shape), dt)), name)

    def ps(self, name, shape, dt):
        return T(self.stack.enter_context(self.nc.psum_tensor(name, list(shape), dt)), name)

    def _need(self, eng, reads, writes):
        deps = {}

        def add(ev):
            if ev is None:
                return
            k, v = ev
            if deps.get(id(k), (None, 0))[1] < v:
                deps[id(k)] = (k, v)

        for b in reads:
            add(b.lw)
        for b in writes:
            add(b.lw)
            for kv in b.rd.values():
                add(kv)
        for k, v in deps.values():
            if k is eng.sem:
                if not eng.same_engine_sync or v > eng.count:
                    continue
            if eng.waited.get(id(k), 0) >= v:
                continue
            eng.h.wait_ge(k, v)
            eng.waited[id(k)] = v

    def op(self, eng, fn, reads=(), writes=(), inc=True):
        reads = [r.b if isinstance(r, T) else r for r in reads]
        writes = [w.b if isinstance(w, T) else w for w in writes]
        self._need(eng, reads, writes)
        ins = fn()
        self.ninst += 1
        if inc:
            eng.count += 1
            ins.then_inc(eng.sem, 1)
            ev = (eng.sem, eng.count)
        else:
            ev = (eng.sem, eng.count + 1)
        for b in writes:
            b.lw = ev
            b.rd = {}
        for b in reads:
            if b.rd.get(id(ev[0]), (None, 0))[1] < ev[1]:
                b.rd[id(ev[0])] = ev
        return ins

    def dma(self, q, out, in_, reads=(), writes=(), sembuf=None, indirect=None, final=False):
        reads = [r.b if isinstance(r, T) else r for r in reads]
        writes = [w.b if isinstance(w, T) else w for w in writes]
        if sembuf is None:
            sembuf = writes[0] if writes else reads[0]
        if isinstance(sembuf, T):
            sembuf = sembuf.b
        if sembuf.sem is None:
            sembuf.sem = self.new_sem("d_" + sembuf.name)
            self.dma_bufs.append(sembuf)
        implied = (sembuf in writes and sembuf.lw is not None and sembuf.lw[0] is sembuf.sem
                   and sembuf.lw[1] == sembuf.semval and len(sembuf.rd) > 0
                   and all(ev[0] is not sembuf.sem for ev in sembuf.rd.values()))
        self._need(q, reads, writes)
        if not implied and sembuf.semval > 0 and q.waited.get(id(sembuf.sem), 0) < sembuf.semval:
            q.h.wait_ge(sembuf.sem, sembuf.semval)
            q.waited[id(sembuf.sem)] = sembuf.semval
        if indirect is not None:
            ins = q.h.indirect_dma_start(out=out, out_offset=None, in_=in_,
                                         in_offset=bass.IndirectOffsetOnAxis(ap=indirect, axis=0))
        else:
            ins = q.h.dma_start(out=out, in_=in_)
        self.ninst += 1
        sembuf.semval += 16
        ins.then_inc(sembuf.sem, 16)
        ev = (sembuf.sem, sembuf.semval)
        for b in writes:
            b.lw = ev
            b.rd = {}
        for b in reads:
            b.rd[id(ev[0])] = ev
        if final:
            self.out_events = [e for e in self.out_events if e[0] is not ev[0]] + [ev]
        return ev

    def barrier(self):
        sp = self.sp
        engs = [self.pe, self.act, self.dve, self.pool]
        for b in self.dma_bufs:
            if b.semval > 0 and sp.waited.get(id(b.sem), 0) < b.semval:
                sp.h.wait_ge(b.sem, b.semval)
                sp.waited[id(b.sem)] = b.semval
        for f in engs:
            if f.count > sp.waited.get(id(f.sem), 0):
                sp.h.wait_ge(f.sem, f.count)
                sp.waited[id(f.sem)] = f.count
        sp.count += 1
        sp.h.nop().then_inc(sp.sem, 1)
        for e in engs:
            e.h.wait_ge(sp.sem, sp.count)
            e.waited[id(sp.sem)] = sp.count

    def push(self):
        es = ExitStack()
        es.__enter__()
        prev = self.stack
        self.stack = es
        return (es, prev)

    def pop(self, ph):
        self.barrier()
        ph[0].__exit__(None, None, None)
        self.stack = ph[1]

    def finish(self):
        for k, v in self.out_events:
            self.sp.h.wait_ge(k, v)


def host_consts():
    p = np.arange(128)
    ident = np.eye(128, dtype=np.float32)
    U = (p[:, None] <= p[None, :]).astype(np.float32)
    q = np.arange(512)
    dm = np.stack([(128 * j + p[:, None] <= q[None, :]) for j in range(4)], axis=1).astype(np.float32)
    mk = np.zeros((128, 4), np.float32)
    mk[:, 0] = ((p // 32) % 2 == 0)
    mk[:, 1] = ((p // 32) % 2 == 1)
    mk[:, 2] = (p == 0)
    mk[:, 3] = 1.0
    half = 4
    inv_freq = np.power(np.float32(ROPE_THETA), -np.arange(half, dtype=np.float32) * np.float32(2.0) / np.float32(8)).astype(np.float32)
    invf = np.tile(inv_freq[None, :], (128, 1)).astype(np.float32)
    return ident, U, dm.reshape(128, 2048), mk, invf


def build_main():
    nc = bass.Bass("TRN2", target_bir_lowering=False)

    def din(name, shape, dt=F32):
        return nc.dram_tensor(name, list(shape), dt, kind="ExternalInput").ap()

    def dout(name, shape, dt=F32):
        return nc.dram_tensor(name, list(shape), dt, kind="ExternalOutput").ap()

    x_perm = din("x_perm", [SEQ, D])
    kpos = din("kpos", [128, NT])
    tpos_in = din("tpos", [NT, 1])
    qfirst_in = din("qfirst", [1, 4])
    x_s = din("x_s", [16, D])
    attn_s_in = din("attn_s", [16, D])
    w_in = din("w_in", [D, D_IN])
    b_forget = din("b_forget", [1, 8])
    lam4 = din("lam4", [4, 32])
    subln = din("subln", [64, 1])
    w_o = din("w_o", [D, D])
    ln1 = din("ln1", [2, D])
    ln2 = din("ln2", [2, D])
    w_router = din("w_router", [D, NE])
    r_bias = din("r_bias", [1, NE])
    w_g = din("w_g", [NE + 1, D, 256])
    w_u = din("w_u", [NE + 1, D, 256])
    w_d = din("w_d", [NE + 1, 256, D])
    c_ident = din("c_ident", [128, 128])
    c_U = din("c_U", [128, 128])
    c_dm = din("c_dm", [128, 2048])
    c_mk = din("c_mk", [128, 4])
    c_invf = din("c_invf", [128, 4])

    y_own = dout("y_own", [2048, D])
    y_s = dout("y_s", [16, D])
    kd_own = dout("kd_own", [2048, 512])
    vd_own = dout("vd_own", [2048, 512])
    kf_own = dout("kf_own", [2048, 512])
    vf_own = dout("vf_own", [2048, 512])
    lf_own = dout("lf_own", [2048, 8])

    kT_scr = nc.dram_tensor("kT_scr", [8, 128, SEQ], BF16, kind="Internal").ap()
    v_scr = nc.dram_tensor("v_scr", [8, 128, NT, 130], BF16, kind="Internal").ap()
    attn_scr = nc.dram_tensor("attn_scr", [2048, D], BF16, kind="Internal").ap()

    with ExitStack() as st:
        fw = FW(nc, st)
        pe, act, dve, pool, sp = fw.pe, fw.act, fw.dve, fw.pool, fw.sp
        V = nc.vector
        A = nc.scalar
        G = nc.gpsimd
        PE = nc.tensor

        PS = [fw.ps(f"ps{i}", [128, 512], F32) for i in range(8)]

        ident = fw.sb("ident", [128, 128], F32)
        identb = fw.sb("identb", [128, 128], BF16)
        Umat = fw.sb("Umat", [128, 128], F32)
        mk = fw.sb("mk", [128, 4], F32)
        invf = fw.sb("invf", [128, 4], F32)
        kpos_sb = fw.sb("kpos_sb", [128, NT], F32)
        cosT = fw.sb("cosT", [128, NT, 4], F32)
        sinT = fw.sb("sinT", [128, NT, 4], F32)
        bf_bc = fw.sb("bf_bc", [128, 1, 8], F32)
        logf = fw.sb("logf", [128, NT, 8], F32)
        ones32 = fw.sb("ones32", [128, 128], F32)
        onesb = fw.sb("onesb", [128, 128], BF16)
        lamt = fw.sb("lamt", [128, 1], F32)
        gainc = fw.sb("gainc", [64, 1], F32)

        ld = lambda t, src, q=sp: fw.dma(q, t[:], src, writes=[t])
        ld(ident, c_ident[:, :])
        ld(Umat, c_U[:, :])
        ld(mk, c_mk[:, :])
        ld(invf, c_invf[:, :])
        ld(kpos_sb, kpos[:, :])
        fw.dma(sp, bf_bc[:], b_forget[0:1, :].partition_broadcast(128), writes=[bf_bc])
        fw.op(dve, lambda: V.tensor_copy(out=identb[:], in_=ident[:]), reads=[ident], writes=[identb])
        fw.op(dve, lambda: V.memset(ones32[:], 1.0), writes=[ones32])
        fw.op(dve, lambda: V.memset(onesb[:], 1.0), writes=[onesb])

        lamv = fw.sb("lamv", [128, 4, 32], F32)
        fw.dma(sp, lamv[:].rearrange("p a b -> p (a b)"),
               lam4.rearrange("a b -> (a b)").unsqueeze(0).partition_broadcast(128).squeeze(1)
               if False else lam4.rearrange("(o a) b -> o (a b)", o=1).partition_broadcast(128).squeeze(1),
               writes=[lamv])
        lprod = fw.sb("lprod", [128, 2, 32], F32)
        lsum = fw.sb("lsum", [128, 2], F32)
        lexp = fw.sb("lexp", [128, 2], F32)
        fw.op(dve, lambda: V.tensor_tensor(out=lprod[:, 0, :], in0=lamv[:, 0, :], in1=lamv[:, 1, :], op=ALU.mult), reads=[lamv], writes=[lprod])
        fw.op(dve, lambda: V.tensor_tensor(out=lprod[:, 1, :], in0=lamv[:, 2, :], in1=lamv[:, 3, :], op=ALU.mult), reads=[lamv], writes=[lprod])
        fw.op(dve, lambda: V.tensor_reduce(out=lsum[:], in_=lprod[:], axis=AX.X, op=ALU.add), reads=[lprod], writes=[lsum])
        fw.op(act, lambda: A.activation(out=lexp[:], in_=lsum[:], func=AF.Exp), reads=[lsum], writes=[lexp])
        fw.op(dve, lambda: V.tensor_tensor(out=lamt[:], in0=lexp[:, 1:2], in1=lexp[:, 0:1], op=ALU.subtract), reads=[lexp], writes=[lamt])
        fw.op(dve, lambda: V.tensor_scalar(out=lamt[:], in0=lamt[:], scalar1=-LAM_INIT, scalar2=None, op0=ALU.add), reads=[lamt], writes=[lamt])
        ld(gainc, subln[:, :])
        fw.op(dve, lambda: V.tensor_scalar(out=gainc[:], in0=gainc[:], scalar1=1.0 - LAM_INIT, scalar2=None, op0=ALU.mult), reads=[gainc], writes=[gainc])

        ang = fw.sb("ang", [128, NT, 4], F32)
        angi = fw.sb("angi", [128, NT, 4], I32)
        angk = fw.sb("angk", [128, NT, 4], F32)
        angr = fw.sb("angr", [128, NT, 4], F32)
        angc = fw.sb("angc", [128, NT, 4], F32)
        TWO_PI = 2.0 * math.pi

        def reduce_sin(dst, shift):
            fw.op(dve, lambda: V.tensor_scalar(out=angk[:], in0=ang[:], scalar1=shift, scalar2=1.0 / TWO_PI, op0=ALU.add, op1=ALU.mult), reads=[ang], writes=[angk])
            fw.op(dve, lambda: V.tensor_copy(out=angi[:], in_=angk[:]), reads=[angk], writes=[angi])
            fw.op(dve, lambda: V.tensor_copy(out=angk[:], in_=angi[:]), reads=[angi], writes=[angk])
            fw.op(dve, lambda: V.tensor_scalar(out=angr[:], in0=ang[:], scalar1=shift, scalar2=None, op0=ALU.add), reads=[ang], writes=[angr])
            fw.op(dve, lambda: V.scalar_tensor_tensor(out=angr[:], in0=angk[:], scalar=-TWO_PI, in1=angr[:], op0=ALU.mult, op1=ALU.add), reads=[angk, angr], writes=[angr])
            fw.op(dve, lambda: V.tensor_scalar(out=angc[:], in0=angr[:], scalar1=math.pi, scalar2=-TWO_PI, op0=ALU.is_gt, op1=ALU.mult), reads=[angr], writes=[angc])
            fw.op(dve, lambda: V.tensor_tensor(out=angr[:], in0=angr[:], in1=angc[:], op=ALU.add), reads=[angr, angc], writes=[angr])
            fw.op(dve, lambda: V.tensor_scalar(out=angc[:], in0=angr[:], scalar1=-math.pi, scalar2=TWO_PI, op0=ALU.is_lt, op1=ALU.mult), reads=[angr], writes=[angc])
            fw.op(dve, lambda: V.tensor_tensor(out=angr[:], in0=angr[:], in1=angc[:], op=ALU.add), reads=[angr, angc], writes=[angr])
            fw.op(dve, lambda: V.tensor_scalar(out=angr[:], in0=angr[:], scalar1=-3.14159, scalar2=3.14159, op0=ALU.max, op1=ALU.min), reads=[angr], writes=[angr])
            fw.op(act, lambda: A.activation(out=dst[:], in_=angr[:], func=AF.Sin), reads=[angr], writes=[dst])

        fw.op(dve, lambda: V.tensor_tensor(out=ang[:], in0=kpos_sb[:].unsqueeze(2).broadcast_to([128, NT, 4]),
                                           in1=invf[:].unsqueeze(1).broadcast_to([128, NT, 4]), op=ALU.mult),
              reads=[kpos_sb, invf], writes=[ang])
        reduce_sin(sinT, 0.0)
        reduce_sin(cosT, math.pi / 2.0)

        S12 = fw.push()
        qT = fw.sb("qT", [128, 8, 2048], BF16)
        S1 = fw.push()
        win = fw.sb("win", [128, 8, D_IN], BF16)
        for kc in range(8):
            for hlf in range(2):
                c0 = hlf * 1540
                fw.dma(pool, win[:, kc, c0:c0 + 1540], w_in[kc * 128:(kc + 1) * 128, c0:c0 + 1540], writes=[win])
        xb = [fw.sb(f"xb{i}", [128, D], BF16) for i in range(3)]
        xf = [fw.sb(f"xf{i}", [128, D], F32) for i in range(3)]
        xT = [fw.sb(f"xT{i}", [128, 8, 128], BF16) for i in range(2)]
        NST = 2
        stg = {nm: [fw.sb(f"s_{nm}{i}", [128, 512], F32) for i in range(NST)] for nm in ("kd", "vd", "kf", "vf", "qd", "qf")}
        stb = {nm: [fw.sb(f"b_{nm}{i}", [128, 512], BF16) for i in range(NST)] for nm in ("kd", "kf", "qd", "qf")}
        vaug = [fw.sb(f"vaug{i}", [128, 16, 65], BF16) for i in range(2)]
        for vv in vaug:
            fw.op(pool, lambda vv=vv: G.memset(vv[:], 1.0), writes=[vv])
        kTst = [fw.sb(f"kTst{i}", [128, 8, 512], BF16) for i in range(2)]
        rt = [fw.sb(f"rt{i}", [128, 64], F32) for i in range(4)]
        zt = fw.sb("zt", [128, 32], F32)
        et = fw.sb("et", [128, 32], F32)
        PX = [PS[0], PS[1]]
        PJ = [PS[2], PS[3], PS[4]]
        PL = PS[5]
        PK = [PS[6], PS[7]]
        colof = {"qd": C_QD, "kd": C_KD, "vd": C_VD, "qf": C_QF, "kf": C_KF, "vf": C_VF}
        outd = {"kd": kd_own, "vd": vd_own, "kf": kf_own, "vf": vf_own}
        kT_b = [Buf(f"kTscr{c}") for c in range(NCH)]
        v_b = [Buf(f"vscr{t}") for t in range(NT)]

        def rope(s_t, t):
            v3 = s_t[:].rearrange("p (g d) -> p g d", d=32)
            x1 = v3[:, :, 0:4]
            x2 = v3[:, :, 4:8]
            cb = cosT[:, t, :].unsqueeze(1).broadcast_to([128, 16, 4])
            sbb = sinT[:, t, :].unsqueeze(1).broadcast_to([128, 16, 4])
            r = [x[:].rearrange("p (g d) -> p g d", d=4) for x in rt]
            fw.op(dve, lambda: V.tensor_tensor(out=r[0], in0=x1, in1=cb, op=ALU.mult), reads=[s_t, cosT], writes=[rt[0]])
            fw.op(dve, lambda: V.tensor_tensor(out=r[1], in0=x2, in1=sbb, op=ALU.mult), reads=[s_t, sinT], writes=[rt[1]])
            fw.op(dve, lambda: V.tensor_tensor(out=r[2], in0=x2, in1=cb, op=ALU.mult), reads=[s_t, cosT], writes=[rt[2]])
            fw.op(dve, lambda: V.tensor_tensor(out=r[3], in0=x1, in1=sbb, op=ALU.mult), reads=[s_t, sinT], writes=[rt[3]])
            fw.op(dve, lambda: V.tensor_tensor(out=x1, in0=r[0], in1=r[1], op=ALU.subtract), reads=[rt[0], rt[1]], writes=[s_t])
            fw.op(dve, lambda: V.tensor_tensor(out=x2, in0=r[2], in1=r[3], op=ALU.add), reads=[rt[2], rt[3]], writes=[s_t])

        pjc = [0]

        def x_load(t):
            xft = xf[t % 3]
            fw.dma(sp, xft[:], x_perm[t * 128:(t + 1) * 128, :], writes=[xft])

        def P1(t):
            xbt = xb[t % 3]
            xTt = xT[t % 2]
            px = PX[t % 2]
            xft = xf[t % 3]
            fw.op(pool, lambda: G.tensor_copy(out=xbt[:], in_=xft[:]), reads=[xft], writes=[xbt])
            pxb = px[:].bitcast(BF16)
            for kc in range(8):
                fw.op(pe, lambda kc=kc: PE.transpose(out=pxb[:, kc * 128:(kc + 1) * 128], in_=xbt[:, kc * 128:(kc + 1) * 128], identity=identb[:]),
                      reads=[xbt, identb], writes=[px], inc=(kc == 7))
            fw.op(dve, lambda: V.tensor_copy(out=xTt[:].rearrange("p a b -> p (a b)"), in_=pxb[:, 0:1024]), reads=[px], writes=[xTt])

        def P2(t):
            ch, j = divmod(t, 4)
            own = (ch % 4 == 0)
            so = ch // 4
            xTt = xT[t % 2]
            groups = ["kd", "vd", "kf", "vf"] + (["qd", "qf"] if own else [])
            si = t % NST
            for nm in groups:
                pj = PJ[pjc[0] % 3]
                pjc[0] += 1
                c0 = colof[nm]
                for kc in range(8):
                    fw.op(pe, lambda kc=kc, pj=pj, c0=c0: PE.matmul(pj[:, :], lhsT=xTt[:, kc, :], rhs=win[:, kc, c0:c0 + 512], start=(kc == 0), stop=(kc == 7)),
                          reads=[xTt, win], writes=[pj], inc=(kc == 7))
                s_t = stg[nm][si]
                va = vaug[t % 2]
                h0 = 0 if nm == "vd" else 8
                need32 = own or nm in ("kd", "qd")
                if need32:
                    fw.op(act, lambda pj=pj, s_t=s_t: A.activation(out=s_t[:], in_=pj[:, :], func=AF.Copy), reads=[pj], writes=[s_t])
                    if nm in ("kd", "qd"):
                        rope(s_t, t)
                    if nm in ("kd", "kf", "qd", "qf"):
                        b_t = stb[nm][si]
                        fw.op(dve, lambda s_t=s_t, b_t=b_t: V.tensor_copy(out=b_t[:], in_=s_t[:]), reads=[s_t], writes=[b_t])
                    else:
                        fw.op(dve, lambda s_t=s_t, va=va, h0=h0: V.tensor_copy(out=va[:, h0:h0 + 8, 0:64], in_=s_t[:].rearrange("p (h e) -> p h e", e=64)),
                              reads=[s_t], writes=[va])
                elif nm == "kf":
                    b_t = stb[nm][si]
                    fw.op(act, lambda pj=pj, b_t=b_t: A.activation(out=b_t[:], in_=pj[:, :], func=AF.Copy), reads=[pj], writes=[b_t])
                else:
                    fw.op(act, lambda pj=pj, va=va, h0=h0: A.activation(out=va[:, h0:h0 + 8, 0:64], in_=pj[:, :].rearrange("p (h e) -> p h e", e=64), func=AF.Copy),
                          reads=[pj], writes=[va])
                if own and nm in outd:
                    r0 = so * 512 + j * 128
                    fw.dma(sp, outd[nm][r0:r0 + 128, :], s_t[:], reads=[s_t], writes=[Buf("o")], sembuf=s_t, final=True)
            for kc in range(8):
                fw.op(pe, lambda kc=kc: PE.matmul(PL[:, j * 8:(j + 1) * 8], lhsT=xTt[:, kc, :], rhs=win[:, kc, C_FL:C_FL + 8], start=(kc == 0), stop=(kc == 7)),
                      reads=[xTt, win], writes=[PL], inc=(kc == 7))
            if j == 3:
                fw.op(dve, lambda: V.tensor_tensor(out=zt[:].rearrange("p (a h) -> p a h", h=8), in0=PL[:, 0:32].rearrange("p (a h) -> p a h", h=8),
                                                   in1=bf_bc[:].broadcast_to([128, 4, 8]), op=ALU.add), reads=[PL, bf_bc], writes=[zt])
                fw.op(act, lambda: A.activation(out=et[:], in_=zt[:], func=AF.Exp, scale=-1.0), reads=[zt], writes=[et])
                fw.op(act, lambda: A.activation(out=zt[:], in_=et[:], func=AF.Ln, bias=1.0), reads=[et], writes=[zt])
                fw.op(dve, lambda: V.tensor_scalar(out=logf[:, ch * 4:ch * 4 + 4, :], in0=zt[:].rearrange("p (a h) -> p a h", h=8), scalar1=-1.0, scalar2=None, op0=ALU.mult),
                      reads=[zt], writes=[logf])
                if own:
                    fw.dma(sp, lf_own[so * 512:(so + 1) * 512, :].rearrange("(a p) h -> p a h", p=128), logf[:, ch * 4:ch * 4 + 4, :],
                           reads=[logf], writes=[Buf("o")], sembuf=logf, final=True)

        def P3(t):
            ch, j = divmod(t, 4)
            own = (ch % 4 == 0)
            so = ch // 4
            si = t % NST
            pk = PK[t % 2]
            pkb = pk[:].bitcast(BF16)
            kst = kTst[ch % 2]
            for gi in range(8):
                src = stb["kd"][si] if gi < 4 else stb["kf"][si]
                g4 = gi % 4
                fw.op(pe, lambda gi=gi, src=src, g4=g4: PE.transpose(out=pkb[:, gi * 128:(gi + 1) * 128], in_=src[:, g4 * 128:(g4 + 1) * 128], identity=identb[:]),
                      reads=[src, identb], writes=[pk], inc=(gi == 7))
            fw.op(dve, lambda: V.tensor_copy(out=kst[:, :, j * 128:(j + 1) * 128], in_=pkb[:, 0:1024].rearrange("p (g n) -> p g n", n=128)),
                  reads=[pk], writes=[kst])
            if own:
                pq = PK[(t + 1) % 2]
                pqb = pq[:].bitcast(BF16)
                for gi in range(8):
                    src = stb["qd"][si] if gi < 4 else stb["qf"][si]
                    g4 = gi % 4
                    fw.op(pe, lambda gi=gi, src=src, g4=g4: PE.transpose(out=pqb[:, gi * 128:(gi + 1) * 128], in_=src[:, g4 * 128:(g4 + 1) * 128], identity=identb[:]),
                          reads=[src, identb], writes=[pq], inc=(gi == 7))
                q0 = so * 512 + j * 128
                pq3 = pqb[:, 0:1024].rearrange("p (g n) -> p g n", n=128)
                fw.op(dve, lambda: V.tensor_copy(out=qT[:, :, q0:q0 + 128], in_=pq3), reads=[pq], writes=[qT])
            va = vaug[t % 2]
            fw.dma(sp, v_scr[:, :, t, :].rearrange("g p c -> p g c"), va[:].rearrange("p (g a) c -> p g (a c)", a=2), reads=[va], writes=[v_b[t]], sembuf=va)
            if j == 3:
                fw.dma(sp, kT_scr[:, :, ch * 512:(ch + 1) * 512].rearrange("g p n -> p g n"), kst[:], reads=[kst], writes=[kT_b[ch]], sembuf=kst)

        x_load(0)
        x_load(1)
        P1(0)
        for t in range(NT):
            if t + 2 < NT:
                x_load(t + 2)
            if t + 1 < NT:
                P1(t + 1)
            P2(t)
            if t >= 1:
                P3(t - 1)
        P3(NT - 1)

        fw.pop(S1)
        S2 = fw.push()
        tposc = fw.sb("tposc", [NT, 1], F32)
        tposr = fw.sb("tposr", [NT, 1, NT], F32)
        Bm = fw.sb("Bm", [NT, NT], F32)
        Tt = fw.sb("Tt", [NT, 8], F32)
        Rm = fw.sb("Rm", [NT, NT, 8], F32)
        cc_ = fw.sb("cc", [128, NT, 8], F32)
        rbc = fw.sb("rbc", [128, 4, 8], F32)
        qfirst = fw.sb("qfirst_sb", [128, 1, 4], F32)
        visb = fw.sb("visb", [128, 4, 12], F32)
        bfox = fw.sb("bfox", [128, 4, NT, 8], F32)
        sel0 = fw.sb("sel0", [128, 128], F32)
        ld(tposc, tpos_in[:, :])
        fw.dma(sp, tposr[:], kpos[0:1, :].partition_broadcast(NT), writes=[tposr])
        fw.op(dve, lambda: V.tensor_scalar(out=Bm[:], in0=tposr[:, 0, :], scalar1=tposc[:, 0:1], scalar2=None, op0=ALU.is_gt), reads=[tposr, tposc], writes=[Bm])
        for h in range(8):
            fw.op(pe, lambda h=h: PE.matmul(PS[0][0:NT, h:h + 1], lhsT=logf[:, :, h], rhs=ones32[:, 0:1], start=True, stop=True), reads=[logf, ones32], writes=[PS[0]], inc=(h == 7))
        fw.op(dve, lambda: V.tensor_copy(out=Tt[:], in_=PS[0][0:NT, 0:8]), reads=[PS[0]], writes=[Tt])
        fw.op(dve, lambda: V.tensor_tensor(out=Rm[:], in0=Bm[:].unsqueeze(2).broadcast_to([NT, NT, 8]), in1=Tt[:].unsqueeze(1).broadcast_to([NT, NT, 8]), op=ALU.mult),
              reads=[Bm, Tt], writes=[Rm])
        fw.op(pe, lambda: PE.matmul(PS[1][:, :], lhsT=Umat[:], rhs=logf[:].rearrange("p t h -> p (t h)"), start=True, stop=False), reads=[Umat, logf], writes=[PS[1]], inc=False)
        fw.op(pe, lambda: PE.matmul(PS[1][:, :], lhsT=ones32[0:NT, :], rhs=Rm[:].rearrange("p t h -> p (t h)"), start=False, stop=True), reads=[ones32, Rm], writes=[PS[1]])
        fw.op(dve, lambda: V.tensor_copy(out=cc_[:].rearrange("p t h -> p (t h)"), in_=PS[1][:, :]), reads=[PS[1]], writes=[cc_])
        fw.op(dve, lambda: V.tensor_copy(out=sel0[:], in_=mk[:, 2:3].broadcast_to([128, 128])), reads=[mk], writes=[sel0])
        for s in range(4):
            fw.op(pe, lambda s=s: PE.matmul(PS[2][:, s * 8:(s + 1) * 8], lhsT=sel0[:], rhs=cc_[:, 16 * s + 2, :], start=True, stop=True), reads=[sel0, cc_], writes=[PS[2]], inc=(s == 3))
        fw.op(dve, lambda: V.tensor_copy(out=rbc[:].rearrange("p s h -> p (s h)"), in_=PS[2][:, 0:32]), reads=[PS[2]], writes=[rbc])
        fw.dma(sp, qfirst[:], qfirst_in[0:1, :].partition_broadcast(128), writes=[qfirst])
        for s in range(4):
            fw.op(dve, lambda s=s: V.tensor_scalar(out=visb[:, s, :], in0=kpos_sb[:, 16 * s + 4:16 * s + 16], scalar1=qfirst[:, 0, s:s + 1], scalar2=NEGBIG, op0=ALU.is_gt, op1=ALU.mult),
                  reads=[kpos_sb, qfirst], writes=[visb])
            nk = 16 * (s + 1)
            fw.op(dve, lambda s=s, nk=nk: V.tensor_tensor(out=bfox[:, s, 0:nk, :], in0=rbc[:, s, :].unsqueeze(1).broadcast_to([128, nk, 8]), in1=cc_[:, 0:nk, :], op=ALU.subtract),
                  reads=[rbc, cc_], writes=[bfox])
            fw.op(dve, lambda s=s: V.tensor_tensor(out=bfox[:, s, 16 * s + 4:16 * s + 16, :], in0=bfox[:, s, 16 * s + 4:16 * s + 16, :],
                                                   in1=visb[:, s, :].unsqueeze(2).broadcast_to([128, 12, 8]), op=ALU.add), reads=[bfox, visb], writes=[bfox])

        dmask = fw.sb("dmask", [128, 4, 512], BF16)
        fw.dma(pool, dmask[:].rearrange("p a b -> p (a b)"), c_dm[:, :], writes=[dmask])
        KTg = [fw.sb(f"KTg{i}", [128, SEQ], BF16) for i in range(2)]
        Vg = [fw.sb(f"Vg{i}", [128, NT, 130], BF16) for i in range(2)]
        PT = [fw.sb(f"PT{i}", [128, 512], BF16) for i in range(6)]
        qm = [fw.sb(f"qm{i}", [128, 2, 512], BF16) for i in range(2)]
        Ast = [fw.sb(f"Ast{i}", [128, 4, 128], BF16) for i in range(2)]
        od1 = [fw.sb(f"od1_{i}", [128, 4, 64], F32) for i in range(2)]
        od2 = fw.sb("od2", [128, 4, 64], F32)
        sqt = fw.sb("sqt", [128, 4, 64], F32)
        rl4 = [fw.sb(f"rl4_{i}", [128, 4], F32) for i in range(2)]
        ssq4 = fw.sb("ssq4", [128, 4], F32)
        gain_bc = fw.sb("gain_bc", [128, 1, 64], F32)
        fw.dma(sp, gain_bc[:], subln.rearrange("e o -> o e").partition_broadcast(128), writes=[gain_bc])
        fw.op(dve, lambda: V.tensor_scalar(out=gain_bc[:], in0=gain_bc[:], scalar1=1.0 - LAM_INIT, scalar2=None, op0=ALU.mult), reads=[gain_bc], writes=[gain_bc])
        attn_b = Buf("attn_scr")
        PSS = [PS[0], PS[1], PS[2], PS[3]]
        PSO = [PS[4], PS[5], PS[6], PS[7]]
        DSCALE = 32.0 ** -0.5
        FSCALE = 64.0 ** -0.5
        it_i = 0
        pair_i = 0
        qm_i = 0
        ast_i = 0

        def load_group(g):
            fw.dma(sp, KTg[g % 2][:], kT_scr[g, :, :], reads=kT_b, writes=[KTg[g % 2]])
            fw.dma(sp, Vg[g % 2][:].rearrange("p t c -> p (t c)"), v_scr[g, :, :, :].rearrange("p t c -> p (t c)"), reads=v_b, writes=[Vg[g % 2]])

        load_group(0)
        for g in range(8):
            if g + 1 < 8:
                load_group(g + 1)
            kt = KTg[g % 2]
            vg = Vg[g % 2]
            isdiff = g < 4
            for s in range(4):
                nkb = 16 * (s + 1)
                a_t = Ast[ast_i % 2]
                ast_i += 1
                if isdiff:
                    qmt = qm[qm_i % 2]
                    qm_i += 1
                    for m_ in range(2):
                        fw.op(dve, lambda m_=m_, qmt=qmt: V.tensor_scalar(out=qmt[:, m_, :], in0=qT[:, g, s * 512:(s + 1) * 512], scalar1=mk[:, m_:m_ + 1], scalar2=None, op0=ALU.mult),
                              reads=[qT, mk], writes=[qmt])
                for m in ((0, 1) if isdiff else (None,)):
                    pos_ = [PSO[(2 * pair_i) % 4], PSO[(2 * pair_i + 1) % 4]]
                    pair_i += 1
                    if isdiff:
                        qsl = [qmt[0:64, m, :], qmt[64:128, m, :]]
                        qsrc = qmt
                    else:
                        qsl = [qT[0:64, g, s * 512:(s + 1) * 512], qT[64:128, g, s * 512:(s + 1) * 512]]
                        qsrc = qT
                    pend = None

                    def pv(kb, pts, pos_=pos_):
                        for hh in range(2):
                            for i4 in range(4):
                                fw.op(pe, lambda hh=hh, i4=i4: PE.matmul(pos_[hh][:, i4 * 65:(i4 + 1) * 65], lhsT=pts[hh][:, i4 * 128:(i4 + 1) * 128], rhs=vg[:, kb, hh * 65:(hh + 1) * 65],
                                                                       start=(kb == 0 and i4 == 0), stop=(kb == nkb - 1 and i4 == 3)),
                                      reads=[vg, pts[hh]], writes=[pos_[hh]], inc=(i4 == 3))

                    for kb in range(nkb):
                        pss = [PSS[(2 * it_i) % 4], PSS[(2 * it_i + 1) % 4]]
                        pts = [PT[(2 * it_i) % 6], PT[(2 * it_i + 1) % 6]]
                        it_i += 1
                        for hh in range(2):
                            fw.op(pe, lambda hh=hh: PE.matmul(pss[hh][:, :], lhsT=kt[hh * 64:(hh + 1) * 64, kb * 128:(kb + 1) * 128], rhs=qsl[hh], start=True, stop=True),
                                  reads=[kt, qsrc], writes=[pss[hh]])
                        if pend is not None:
                            pv(*pend)
                        band = kb - 16 * s
                        for hh in range(2):
                            if isdiff:
                                if band >= 4:
                                    fw.op(act, lambda hh=hh: A.activation(out=pts[hh][:], in_=pss[hh][:, :], func=AF.Exp, scale=DSCALE, bias=visb[:, s, band - 4:band - 3]),
                                          reads=[pss[hh], visb], writes=[pts[hh]])
                                else:
                                    fw.op(act, lambda hh=hh: A.activation(out=pts[hh][:], in_=pss[hh][:, :], func=AF.Exp, scale=DSCALE), reads=[pss[hh]], writes=[pts[hh]])
                            else:
                                fh = 2 * (g - 4) + hh
                                fw.op(act, lambda hh=hh, fh=fh: A.activation(out=pts[hh][:], in_=pss[hh][:, :], func=AF.Exp, scale=FSCALE, bias=bfox[:, s, kb, fh:fh + 1]),
                                      reads=[pss[hh], bfox], writes=[pts[hh]])
                            if 0 <= band < 4:
                                fw.op(pool, lambda hh=hh: G.tensor_tensor(out=pts[hh][:], in0=pts[hh][:], in1=dmask[:, band, :], op=ALU.mult), reads=[pts[hh], dmask], writes=[pts[hh]])
                        pend = (kb, pts)
                    pv(*pend)
                    for hh in range(2):
                        o3 = pos_[hh][:, 0:260].rearrange("p (i c) -> p i c", c=65)
                        rlt = rl4[hh]
                        fw.op(dve, lambda o3=o3, rlt=rlt: V.reciprocal(out=rlt[:], in_=o3[:, :, 64]), reads=[pos_[hh]], writes=[rlt])
                        rb = rlt[:].unsqueeze(2).broadcast_to([128, 4, 64])
                        if not isdiff:
                            fw.op(dve, lambda o3=o3, rb=rb, hh=hh: V.tensor_tensor(out=a_t[:, :, hh * 64:(hh + 1) * 64], in0=o3[:, :, 0:64], in1=rb, op=ALU.mult), reads=[pos_[hh], rlt], writes=[a_t])
                        elif m == 0:
                            fw.op(dve, lambda o3=o3, rb=rb, hh=hh: V.tensor_tensor(out=od1[hh][:], in0=o3[:, :, 0:64], in1=rb, op=ALU.mult), reads=[pos_[hh], rlt], writes=[od1[hh]])
                        else:
                            fw.op(dve, lambda o3=o3, rb=rb: V.tensor_tensor(out=od2[:], in0=o3[:, :, 0:64], in1=rb, op=ALU.mult), reads=[pos_[hh], rlt], writes=[od2])
                            fw.op(dve, lambda hh=hh: V.scalar_tensor_tensor(out=od2[:].rearrange("p a b -> p (a b)"), in0=od2[:].rearrange("p a b -> p (a b)"), scalar=lamt[:, 0:1],
                                                                         in1=od1[hh][:].rearrange("p a b -> p (a b)"), op0=ALU.mult, op1=ALU.add), reads=[od2, od1[hh], lamt], writes=[od2])
                            fw.op(dve, lambda: V.tensor_tensor(out=sqt[:], in0=od2[:], in1=od2[:], op=ALU.mult), reads=[od2], writes=[sqt])
                            fw.op(dve, lambda: V.tensor_reduce(out=ssq4[:], in_=sqt[:], axis=AX.X, op=ALU.add), reads=[sqt], writes=[ssq4])
                            fw.op(act, lambda: A.activation(out=ssq4[:], in_=ssq4[:], func=AF.Sqrt, scale=1.0 / 64.0, bias=RMS_EPS), reads=[ssq4], writes=[ssq4])
                            fw.op(dve, lambda: V.reciprocal(out=ssq4[:], in_=ssq4[:]), reads=[ssq4], writes=[ssq4])
                            fw.op(dve, lambda: V.tensor_tensor(out=od2[:], in0=od2[:], in1=ssq4[:].unsqueeze(2).broadcast_to([128, 4, 64]), op=ALU.mult), reads=[od2, ssq4], writes=[od2])
                            fw.op(dve, lambda hh=hh: V.tensor_tensor(out=a_t[:, :, hh * 64:(hh + 1) * 64], in0=od2[:], in1=gain_bc[:].broadcast_to([128, 4, 64]), op=ALU.mult), reads=[od2, gain_bc], writes=[a_t])
                c0 = g * 128
                fw.dma(sp, attn_scr[s * 512:(s + 1) * 512, c0:c0 + 128].rearrange("(i p) c -> p i c", p=128), a_t[:], reads=[a_t], writes=[attn_b], sembuf=a_t)

        fw.pop(S2)
        fw.pop(S12)
        S345 = fw.push()
        NTOK = 2048 + 16
        hT = fw.sb("hT", [128, 8, NTOK], BF16)
        yacc = fw.sb("yacc", [128, 17, D], F32)
        gates = fw.sb("gates", [128, 17, NE + 1], F32)
        fw.op(dve, lambda: V.memset(gates[:], 1.0), writes=[gates])
        stats = fw.sb("stats", [128, 2, 6], F32)
        mv = fw.sb("mv", [128, 2], F32)
        rs = fw.sb("rs", [128, 1], F32)
        S3 = fw.push()
        wob = fw.sb("wob", [128, 8, D], BF16)
        fw.dma(pool, wob[:], w_o.rearrange("(k p) d -> p k d", p=128), writes=[wob])
        AT = [fw.sb(f"AT{i}", [128, 8, 128], BF16) for i in range(2)]
        g1 = fw.sb("g1", [128, 1, D], F32)
        b1 = fw.sb("b1", [128, 1, D], F32)
        fw.dma(sp, g1[:], ln1[0:1, :].partition_broadcast(128), writes=[g1])
        fw.dma(sp, b1[:], ln1[1:2, :].partition_broadcast(128), writes=[b1])
        wr32 = fw.sb("wr32", [128, 8, NE], F32)
        fw.dma(sp, wr32[:], w_router.rearrange("(k p) e -> p k e", p=128), writes=[wr32])
        rb_bc = fw.sb("rb_bc", [128, 1, NE], F32)
        fw.dma(sp, rb_bc[:], r_bias[0:1, :].partition_broadcast(128), writes=[rb_bc])
        att = [fw.sb(f"att{i}", [128, D], BF16) for i in range(2)]
        xres = [fw.sb(f"xres{i}", [128, D], F32) for i in range(2)]
        pre = fw.sb("pre", [128, D], F32)
        hT32 = fw.sb("hT32", [128, 8, 128], F32)
        sc = fw.sb("sc", [128, NE], F32)
        chs = fw.sb("chs", [128, NE], F32)
        ch2 = fw.sb("ch2", [128, NE], F32)
        eqm = fw.sb("eqm", [128, NE], F32)
        m1 = fw.sb("m1", [128, 8], F32)
        m2 = fw.sb("m2", [128, 8], F32)
        gs = fw.sb("gs", [128, 8], F32)
        top8 = fw.sb("top8", [128, 8], F32)
        gmask = fw.sb("gmask", [128, 8], F32)
        den = fw.sb("den", [128, 1], F32)

        def layer_norm(src, gam, bet, dst, n):
            for hf in range(2):
                fw.op(dve, lambda hf=hf: V.bn_stats(out=stats[0:n, hf, :], in_=src[0:n, hf * 512:(hf + 1) * 512]), reads=[src], writes=[stats])
            fw.op(dve, lambda: V.bn_aggr(out=mv[0:n, :], in_=stats[0:n, :, :].rearrange("p a b -> p (a b)")), reads=[stats], writes=[mv])
            fw.op(act, lambda: A.activation(out=rs[0:n, :], in_=mv[0:n, 1:2], func=AF.Sqrt, bias=LN_EPS), reads=[mv], writes=[rs])
            fw.op(dve, lambda: V.reciprocal(out=rs[0:n, :], in_=rs[0:n, :]), reads=[rs], writes=[rs])
            fw.op(dve, lambda: V.tensor_scalar(out=dst[0:n, :], in0=src[0:n, :], scalar1=mv[0:n, 0:1], scalar2=rs[0:n, 0:1], op0=ALU.subtract, op1=ALU.mult),
                  reads=[src, mv, rs], writes=[dst])
            fw.op(dve, lambda: V.tensor_tensor(out=dst[0:n, :], in0=dst[0:n, :], in1=gam[0:n, 0, :], op=ALU.mult), reads=[dst, gam], writes=[dst])
            fw.op(dve, lambda: V.tensor_tensor(out=dst[0:n, :], in0=dst[0:n, :], in1=bet[0:n, 0, :], op=ALU.add), reads=[dst, bet], writes=[dst])

        hbuf = fw.sb("hbuf", [128, D], F32)
        pre2 = [pre, fw.sb("pre_b", [128, D], F32)]

        def stageA(ti):
            n = 128 if ti < 16 else 16
            t0 = ti * 128
            pre = pre2[ti % 2]
            xr = xres[ti % 2]
            at_t = att[ti % 2]
            if ti < 16:
                fw.dma(sp, at_t[:], attn_scr[t0:t0 + 128, :], reads=[attn_b], writes=[at_t])
            else:
                fw.dma(pool, at_t[0:16, :], attn_s_in[:, :], writes=[at_t])
            att_T = AT[ti % 2]
            pab = PS[6 + ti % 2][:].bitcast(BF16)
            for kc in range(8):
                fw.op(pe, lambda kc=kc: PE.transpose(out=pab[:, kc * 128:kc * 128 + n], in_=at_t[0:n, kc * 128:(kc + 1) * 128], identity=identb[0:n, 0:n]),
                      reads=[at_t, identb], writes=[PS[6 + ti % 2]], inc=(kc == 7))
            fw.op(act, lambda: A.activation(out=att_T[:, :, 0:n], in_=pab[:, 0:1024].rearrange("p (a b) -> p a b", b=128)[:, :, 0:n], func=AF.Copy), reads=[PS[6 + ti % 2]], writes=[att_T])
            if ti < 16:
                so, j = divmod(ti, 4)
                gt = (so * 16 + j)
                fw.dma(sp, xr[:], x_perm[gt * 128:(gt + 1) * 128, :], writes=[xr])
            else:
                fw.dma(sp, xr[0:16, :], x_s[:, :], writes=[xr])
            for hf in range(2):
                py = PS[hf]
                for h in range(8):
                    fw.op(pe, lambda h=h, py=py, hf=hf: PE.matmul(py[0:n, :], lhsT=att_T[:, h, 0:n], rhs=wob[:, h, hf * 512:(hf + 1) * 512], start=(h == 0), stop=(h == 7)),
                          reads=[att_T, wob], writes=[py], inc=(h == 7))
                fw.op(dve, lambda py=py, hf=hf: V.scalar_tensor_tensor(out=pre[0:n, hf * 512:(hf + 1) * 512], in0=xr[0:n, hf * 512:(hf + 1) * 512], scalar=DEEP_ALPHA,
                                                                       in1=py[0:n, :], op0=ALU.mult, op1=ALU.add), reads=[xr, py], writes=[pre])
        def stageB(ti):
            n = 128 if ti < 16 else 16
            t0 = ti * 128
            pre = pre2[ti % 2]
            layer_norm(pre, g1, b1, hbuf, n)
            fw.op(act, lambda: A.mul(out=yacc[0:n, ti, :], in_=hbuf[0:n, :], mul=DEEP_ALPHA), reads=[hbuf], writes=[yacc])
            for kc in range(8):
                pt_ = PS[2 + kc // 4]
                fw.op(pe, lambda kc=kc, pt_=pt_: PE.transpose(out=pt_[:, (kc % 4) * 128:(kc % 4) * 128 + n], in_=hbuf[0:n, kc * 128:(kc + 1) * 128], identity=ident[0:n, 0:n]),
                      reads=[hbuf, ident], writes=[pt_], inc=(kc % 4 == 3))
            for hb in range(2):
                pt_ = PS[2 + hb]
                fw.op(act, lambda pt_=pt_, hb=hb: A.activation(out=hT32[:, hb * 4:hb * 4 + 4, 0:n], in_=pt_[:, :].rearrange("p (a b) -> p a b", b=128)[:, :, 0:n], func=AF.Copy),
                      reads=[pt_], writes=[hT32])
            fw.op(pool, lambda: G.tensor_copy(out=hT[:, :, t0:t0 + n], in_=hT32[:, :, 0:n]), reads=[hT32], writes=[hT])
            pr = PS[4]
            for kc in range(8):
                fw.op(pe, lambda kc=kc: PE.matmul(pr[0:n, 0:NE], lhsT=hT32[:, kc, 0:n], rhs=wr32[:, kc, :], start=(kc == 0), stop=(kc == 7)), reads=[hT32, wr32], writes=[pr], inc=(kc == 7))
            fw.op(act, lambda: A.activation(out=sc[0:n, :], in_=pr[0:n, 0:NE], func=AF.Sigmoid), reads=[pr], writes=[sc])
            fw.op(dve, lambda: V.tensor_tensor(out=chs[0:n, :], in0=sc[0:n, :], in1=rb_bc[0:n, 0, :], op=ALU.add), reads=[sc, rb_bc], writes=[chs])
            c3 = chs[0:n, :].rearrange("p (g k) -> p g k", k=8)
            fw.op(dve, lambda: V.tensor_reduce(out=m1[0:n, :], in_=c3, axis=AX.X, op=ALU.max), reads=[chs], writes=[m1])
            fw.op(dve, lambda: V.tensor_tensor(out=eqm[0:n, :].rearrange("p (g k) -> p g k", k=8), in0=c3, in1=m1[0:n, :].unsqueeze(2).broadcast_to([n, 8, 8]), op=ALU.is_ge), reads=[chs, m1], writes=[eqm])
            fw.op(dve, lambda: V.scalar_tensor_tensor(out=ch2[0:n, :], in0=eqm[0:n, :], scalar=-1e30, in1=chs[0:n, :], op0=ALU.mult, op1=ALU.add), reads=[eqm, chs], writes=[ch2])
            fw.op(dve, lambda: V.tensor_reduce(out=m2[0:n, :], in_=ch2[0:n, :].rearrange("p (g k) -> p g k", k=8), axis=AX.X, op=ALU.max), reads=[ch2], writes=[m2])
            fw.op(dve, lambda: V.tensor_tensor(out=gs[0:n, :], in0=m1[0:n, :], in1=m2[0:n, :], op=ALU.add), reads=[m1, m2], writes=[gs])
            fw.op(dve, lambda: V.max(out=top8[0:n, :], in_=gs[0:n, :]), reads=[gs], writes=[top8])
            fw.op(dve, lambda: V.tensor_scalar(out=gmask[0:n, :], in0=gs[0:n, :], scalar1=top8[0:n, 3:4], scalar2=None, op0=ALU.is_ge), reads=[gs, top8], writes=[gmask])
            fw.op(dve, lambda: V.tensor_tensor(out=ch2[0:n, :].rearrange("p (g k) -> p g k", k=8), in0=c3, in1=gmask[0:n, :].unsqueeze(2).broadcast_to([n, 8, 8]), op=ALU.mult), reads=[chs, gmask], writes=[ch2])
            fw.op(dve, lambda: V.tensor_scalar(out=eqm[0:n, 0:8], in0=gmask[0:n, :], scalar1=-1.0, scalar2=1e30, op0=ALU.add, op1=ALU.mult), reads=[gmask], writes=[eqm])
            fw.op(dve, lambda: V.tensor_tensor(out=ch2[0:n, :].rearrange("p (g k) -> p g k", k=8), in0=ch2[0:n, :].rearrange("p (g k) -> p g k", k=8),
                                               in1=eqm[0:n, 0:8].unsqueeze(2).broadcast_to([n, 8, 8]), op=ALU.add), reads=[ch2, eqm], writes=[ch2])
            fw.op(dve, lambda: V.max(out=top8[0:n, :], in_=ch2[0:n, :]), reads=[ch2], writes=[top8])
            fw.op(dve, lambda: V.tensor_scalar(out=eqm[0:n, :], in0=ch2[0:n, :], scalar1=top8[0:n, 7:8], scalar2=None, op0=ALU.is_ge), reads=[ch2, top8], writes=[eqm])
            fw.op(dve, lambda: V.tensor_tensor(out=ch2[0:n, :], in0=eqm[0:n, :], in1=sc[0:n, :], op=ALU.mult), reads=[eqm, sc], writes=[ch2])
            fw.op(dve, lambda: V.tensor_reduce(out=den[0:n, :], in_=ch2[0:n, :], axis=AX.X, op=ALU.add), reads=[ch2], writes=[den])
            fw.op(dve, lambda: V.tensor_scalar(out=den[0:n, :], in0=den[0:n, :], scalar1=1e-20, scalar2=None, op0=ALU.add), reads=[den], writes=[den])
            fw.op(dve, lambda: V.reciprocal(out=den[0:n, :], in_=den[0:n, :]), reads=[den], writes=[den])
            fw.op(dve, lambda: V.tensor_scalar(out=gates[0:n, ti, 0:NE], in0=ch2[0:n, :], scalar1=den[0:n, 0:1], scalar2=2.5, op0=ALU.mult, op1=ALU.mult), reads=[ch2, den], writes=[gates])

        stageA(0)
        for ti in range(17):
            if ti + 1 < 17:
                stageA(ti + 1)
            stageB(ti)
        fw.pop(S3)
        S4 = fw.push()
        wgb = [fw.sb(f"wgb{i}", [128, 8, 256], BF16) for i in range(2)]
        wub = [fw.sb(f"wub{i}", [128, 8, 256], BF16) for i in range(2)]
        wdb = [fw.sb(f"wdb{i}", [128, 2, D], BF16) for i in range(2)]
        sa = [fw.sb(f"sa{i}", [128, 512], F32) for i in range(2)]
        actb = [fw.sb(f"actb{i}", [128, 2, 512], BF16) for i in range(2)]
        PA = [PS[0], PS[1]]
        PU = [PS[2], PS[3]]
        PY = [[PS[4], PS[5]], [PS[6], PS[7]]]

        wg32 = [fw.sb(f"wg32_{i}", [128, 8, 256], F32) for i in range(2)]
        wu32 = [fw.sb(f"wu32_{i}", [128, 8, 256], F32) for i in range(2)]
        wd32 = [fw.sb(f"wd32_{i}", [128, 2, D], F32) for i in range(2)]

        def load_expert(e):
            i = e % 2
            fw.dma(sp, wg32[i][:], w_g[e].rearrange("(k p) f -> p k f", p=128), writes=[wg32[i]])
            fw.dma(sp, wu32[i][:], w_u[e].rearrange("(k p) f -> p k f", p=128), writes=[wu32[i]])
            fw.dma(sp, wd32[i][:], w_d[e].rearrange("(k p) d -> p k d", p=128), writes=[wd32[i]])

        def cast_expert(e):
            i = e % 2
            fw.op(pool, lambda: G.tensor_copy(out=wgb[i][:].rearrange("p a b -> p (a b)"), in_=wg32[i][:].rearrange("p a b -> p (a b)")), reads=[wg32[i]], writes=[wgb[i]])
            fw.op(act, lambda: A.activation(out=wub[i][:].rearrange("p a b -> p (a b)"), in_=wu32[i][:].rearrange("p a b -> p (a b)"), func=AF.Copy), reads=[wu32[i]], writes=[wub[i]])
            fw.op(act, lambda: A.activation(out=wdb[i][:].rearrange("p a b -> p (a b)"), in_=wd32[i][:].rearrange("p a b -> p (a b)"), func=AF.Copy), reads=[wd32[i]], writes=[wdb[i]])

        load_expert(0)
        cast_expert(0)
        load_expert(1)
        chunks = [(c * 512, 512) for c in range(4)] + [(2048, 16)]
        cnt = {"au": 0, "y": 0, "ck": 0}

        def gate_up(e, c0, cn):
            i = e % 2
            ab = actb[cnt["ck"] % 2]
            cnt["ck"] += 1
            for fc in range(2):
                pa = PA[cnt["au"] % 2]
                pu = PU[cnt["au"] % 2]
                sat = sa[cnt["au"] % 2]
                cnt["au"] += 1
                for kc in range(8):
                    fw.op(pe, lambda kc=kc: PE.matmul(pa[:, 0:cn], lhsT=wgb[i][:, kc, fc * 128:(fc + 1) * 128], rhs=hT[:, kc, c0:c0 + cn], start=(kc == 0), stop=(kc == 7)),
                          reads=[wgb[i], hT], writes=[pa], inc=(kc == 7))
                for kc in range(8):
                    fw.op(pe, lambda kc=kc: PE.matmul(pu[:, 0:cn], lhsT=wub[i][:, kc, fc * 128:(fc + 1) * 128], rhs=hT[:, kc, c0:c0 + cn], start=(kc == 0), stop=(kc == 7)),
                          reads=[wub[i], hT], writes=[pu], inc=(kc == 7))
                fw.op(act, lambda: A.activation(out=sat[:, 0:cn], in_=pa[:, 0:cn], func=AF.Silu), reads=[pa], writes=[sat])
                fw.op(dve, lambda: V.tensor_tensor(out=ab[:, fc, 0:cn], in0=sat[:, 0:cn], in1=pu[:, 0:cn], op=ALU.mult), reads=[sat, pu], writes=[ab])
            return ab

        def down(e, c0, cn, ab):
            i = e % 2
            nsub = max(1, cn // 128)
            for sb_ in range(nsub):
                n = min(128, cn)
                ti = c0 // 128 + sb_
                py = PY[cnt["y"] % 2]
                cnt["y"] += 1
                for hf in range(2):
                    for fc in range(2):
                        fw.op(pe, lambda fc=fc: PE.matmul(py[hf][0:n, :], lhsT=ab[:, fc, sb_ * 128:sb_ * 128 + n], rhs=wdb[i][:, fc, hf * 512:(hf + 1) * 512], start=(fc == 0), stop=(fc == 1)),
                              reads=[ab, wdb[i]], writes=[py[hf]], inc=(fc == 1))
                    fw.op(dve, lambda: V.scalar_tensor_tensor(out=yacc[0:n, ti, hf * 512:(hf + 1) * 512], in0=py[hf][0:n, :], scalar=gates[0:n, ti, e:e + 1],
                                                              in1=yacc[0:n, ti, hf * 512:(hf + 1) * 512], op0=ALU.mult, op1=ALU.add),
                          reads=[py[hf], gates, yacc], writes=[yacc])

        prev = None
        for e in range(NE + 1):
            for (c0, cn) in chunks:
                ab = gate_up(e, c0, cn)
                if prev is not None:
                    down(*prev)
                prev = (e, c0, cn, ab)
                if c0 == 0 and e + 1 <= NE:
                    cast_expert(e + 1)
                    if e + 2 <= NE:
                        load_expert(e + 2)
        down(*prev)


        fw.pop(S4)
        S5 = fw.push()
        g2 = fw.sb("g2", [128, 1, D], F32)
        b2 = fw.sb("b2", [128, 1, D], F32)
        fw.dma(sp, g2[:], ln2[0:1, :].partition_broadcast(128), writes=[g2])
        fw.dma(sp, b2[:], ln2[1:2, :].partition_broadcast(128), writes=[b2])
        yo = [fw.sb(f"yo{i}", [128, D], F32) for i in range(2)]
        ysrc = [fw.sb(f"ysrc{i}", [128, D], F32) for i in range(2)]
        for ti in range(17):
            n = 128 if ti < 16 else 16
            o_t = yo[ti % 2]
            s_t = ysrc[ti % 2]
            fw.op(act, lambda s_t=s_t, ti=ti, n=n: A.activation(out=s_t[0:n, :], in_=yacc[0:n, ti, :], func=AF.Copy), reads=[yacc], writes=[s_t])
            layer_norm(s_t, g2, b2, o_t, n)
            if ti < 16:
                fw.dma(sp, y_own[ti * 128:(ti + 1) * 128, :], o_t[:], reads=[o_t], writes=[Buf("o")], sembuf=o_t, final=True)
            else:
                fw.dma(sp, y_s[:, :], o_t[0:16, :], reads=[o_t], writes=[Buf("o")], sembuf=o_t, final=True)
        fw.finish()
        fw.pop(S5)
        fw.pop(S345)
        print("main program: instructions", fw.ninst, "semaphores", fw.nsem)
    return nc


NPOOL = 2560
_NC_CACHE = {}


def host_consts_A():
    p = np.arange(128)
    SU = (p[:, None] > p[None, :]).astype(np.float32)
    j = np.arange(64)
    SUP = (j[:, None] > j[None, :]).astype(np.float32)
    rm = np.zeros((128, 4), np.float32)
    rm[:, 3] = p
    rm[:, 0] = (p < 32)
    rm[:, 1] = (p >= 32) & (p < 64)
    rm[:, 2] = (p >= 64)
    mN = np.zeros((128, 32, 12), np.float32)
    for sq in range(32):
        for i in range(4):
            for ip in range(i + 1):
                mN[sq * 4 + ip, sq, [i, 4 + i, 8 + i]] = 1.0
    E = np.zeros((3, 12, 32, 128), np.float32)
    for sq in range(32):
        for i in range(4):
            E[0, i, sq, sq * 4 + i] = 1.0
            E[1, 4 + i, sq, sq * 4 + i] = 1.0
            E[2, 8 + i, sq, sq * 4 + i] = 1.0
    t = np.arange(128)
    PN = ((t[:, None] // 4 == t[None, :] // 4) & (t[:, None] <= t[None, :])).astype(np.float32)
    return SU, SUP, rm, mN.reshape(128, 384), E.reshape(3, 12, 4096), PN


def build_sample():
    nc = bass.Bass("TRN2", target_bir_lowering=False)

    def din(name, shape, dt=F32):
        return nc.dram_tensor(name, list(shape), dt, kind="ExternalInput").ap()

    def dout(name, shape, dt=F32):
        return nc.dram_tensor(name, list(shape), dt, kind="ExternalOutput").ap()

    xs = din("xs", [128, D])
    w_s = din("w_s", [D, 385])
    bf1 = din("bf1", [1, 1])
    lam4 = din("lam4", [4, 32])
    subln = din("subln", [1, 64])
    spos = din("spos", [128, 1])
    ptb_in = din("ptb", [1, 2048], I32)
    ptP_in = din("ptP", [128, 16], I32)
    kv_pool = din("kv_pool", [NPOOL * 128, 256])
    lf_pool = din("lf_pool", [NPOOL, 128])
    c_ident = din("c_ident", [128, 128])
    c_SU = din("c_SU", [128, 128])
    c_SUP = din("c_SUP", [64, 64])
    c_rm = din("c_rm", [128, 4])
    c_mN = din("c_mN", [128, 384])
    c_E = din("c_E", [3, 12, 4096])
    c_PN = din("c_PN", [128, 128])
    c_invf = din("c_invf", [128, 4])

    attn_c = dout("attn_c", [128, 128])
    nkd = dout("nkd", [128, 64])
    nvd = dout("nvd", [128, 64])
    nkf = dout("nkf", [128, 64])
    nvf = dout("nvf", [128, 64])
    nlf = dout("nlf", [128, 1])

    with ExitStack() as st:
        fw = FW(nc, st)
        pe, act, dve, pool, sp = fw.pe, fw.act, fw.dve, fw.pool, fw.sp
        V = nc.vector
        A = nc.scalar
        G = nc.gpsimd
        PE = nc.tensor
        PS = [fw.ps(f"ps{i}", [128, 512], F32) for i in range(8)]
        ld = lambda t, src, q=sp: fw.dma(q, t[:], src, writes=[t])

        ident = fw.sb("ident", [128, 128], F32)
        identb = fw.sb("identb", [128, 128], BF16)
        SU = fw.sb("SU", [128, 128], F32)
        SUP = fw.sb("SUP", [64, 64], F32)
        rm = fw.sb("rm", [128, 4], F32)
        mN = fw.sb("mN", [128, 32, 12], F32)
        PN = fw.sb("PN", [128, 128], F32)
        invf = fw.sb("invf", [128, 4], F32)
        ones32 = fw.sb("ones32", [128, 128], F32)
        ld(ident, c_ident[:, :])
        ld(SU, c_SU[:, :])
        ld(SUP, c_SUP[:, :])
        ld(rm, c_rm[:, :])
        ld(PN, c_PN[:, :])
        ld(invf, c_invf[:, :])
        fw.dma(sp, mN[:].rearrange("p a b -> p (a b)"), c_mN[:, :], writes=[mN])
        fw.op(dve, lambda: V.tensor_copy(out=identb[:], in_=ident[:]), reads=[ident], writes=[identb])
        fw.op(dve, lambda: V.memset(ones32[:], 1.0), writes=[ones32])

        lamv = fw.sb("lamv", [128, 4, 32], F32)
        fw.dma(sp, lamv[:].rearrange("p a b -> p (a b)"), lam4.rearrange("(o a) b -> o (a b)", o=1).partition_broadcast(128).squeeze(1), writes=[lamv])
        lprod = fw.sb("lprod", [128, 2, 32], F32)
        lsum = fw.sb("lsum", [128, 2], F32)
        lexp = fw.sb("lexp", [128, 2], F32)
        lamt = fw.sb("lamt", [128, 1], F32)
        fw.op(dve, lambda: V.tensor_tensor(out=lprod[:, 0, :], in0=lamv[:, 0, :], in1=lamv[:, 1, :], op=ALU.mult), reads=[lamv], writes=[lprod])
        fw.op(dve, lambda: V.tensor_tensor(out=lprod[:, 1, :], in0=lamv[:, 2, :], in1=lamv[:, 3, :], op=ALU.mult), reads=[lamv], writes=[lprod])
        fw.op(dve, lambda: V.tensor_reduce(out=lsum[:], in_=lprod[:], axis=AX.X, op=ALU.add), reads=[lprod], writes=[lsum])
        fw.op(act, lambda: A.activation(out=lexp[:], in_=lsum[:], func=AF.Exp), reads=[lsum], writes=[lexp])
        fw.op(dve, lambda: V.tensor_tensor(out=lamt[:], in0=lexp[:, 1:2], in1=lexp[:, 0:1], op=ALU.subtract), reads=[lexp], writes=[lamt])
        fw.op(dve, lambda: V.tensor_scalar(out=lamt[:], in0=lamt[:], scalar1=-LAM_INIT, scalar2=None, op0=ALU.add), reads=[lamt], writes=[lamt])
        E0 = fw.sb("E0", [12, 4096], F32)
        E1 = fw.sb("E1", [12, 4096], F32)
        SelF = fw.sb("SelF", [12, 4096], F32)
        SelD = fw.sb("SelD", [12, 4096], F32)
        ld(E0, c_E[0, :, :])
        ld(E1, c_E[1, :, :])
        ld(SelF, c_E[2, :, :])
        fw.op(dve, lambda: V.scalar_tensor_tensor(out=SelD[:], in0=E1[:], scalar=lamt[0:12, 0:1], in1=E0[:], op0=ALU.mult, op1=ALU.add), reads=[E1, E0, lamt], writes=[SelD])

        xsb = fw.sb("xsb", [128, D], BF16)
        xsT = fw.sb("xsT", [128, 8, 128], BF16)
        wsb = fw.sb("wsb", [128, 8, 385], BF16)
        fw.dma(pool, xsb[:], xs[:, :], writes=[xsb])
        fw.dma(pool, wsb[:], w_s.rearrange("(k p) c -> p k c", p=128), writes=[wsb])
        pxb = PS[0][:].bitcast(BF16)
        for kc in range(8):
            fw.op(pe, lambda kc=kc: PE.transpose(out=pxb[:, kc * 128:(kc + 1) * 128], in_=xsb[:, kc * 128:(kc + 1) * 128], identity=identb[:]), reads=[xsb, identb], writes=[PS[0]], inc=(kc == 7))
        fw.op(dve, lambda: V.tensor_copy(out=xsT[:].rearrange("p a b -> p (a b)"), in_=pxb[:, 0:1024]), reads=[PS[0]], writes=[xsT])
        for kc in range(8):
            fw.op(pe, lambda kc=kc: PE.matmul(PS[1][:, 0:385], lhsT=xsT[:, kc, :], rhs=wsb[:, kc, :], start=(kc == 0), stop=(kc == 7)), reads=[xsT, wsb], writes=[PS[1]], inc=(kc == 7))
        z = fw.sb("z", [128, 385], F32)
        fw.op(act, lambda: A.activation(out=z[:], in_=PS[1][:, 0:385], func=AF.Copy), reads=[PS[1]], writes=[z])
        pos = fw.sb("pos", [128, 1], F32)
        ld(pos, spos[:, :])
        ang = fw.sb("ang", [128, 4], F32)
        angk = fw.sb("angk", [128, 4], F32)
        angi = fw.sb("angi", [128, 4], I32)
        angr = fw.sb("angr", [128, 4], F32)
        angc = fw.sb("angc", [128, 4], F32)
        cosS = fw.sb("cosS", [128, 4], F32)
        sinS = fw.sb("sinS", [128, 4], F32)
        TWO_PI = 2.0 * math.pi
        fw.op(dve, lambda: V.tensor_scalar(out=ang[:], in0=invf[:], scalar1=pos[:, 0:1], scalar2=None, op0=ALU.mult), reads=[invf, pos], writes=[ang])

        def reduce_sin(dst, shift):
            fw.op(dve, lambda: V.tensor_scalar(out=angk[:], in0=ang[:], scalar1=shift, scalar2=1.0 / TWO_PI, op0=ALU.add, op1=ALU.mult), reads=[ang], writes=[angk])
            fw.op(dve, lambda: V.tensor_copy(out=angi[:], in_=angk[:]), reads=[angk], writes=[angi])
            fw.op(dve, lambda: V.tensor_copy(out=angk[:], in_=angi[:]), reads=[angi], writes=[angk])
            fw.op(dve, lambda: V.tensor_scalar(out=angr[:], in0=ang[:], scalar1=shift, scalar2=None, op0=ALU.add), reads=[ang], writes=[angr])
            fw.op(dve, lambda: V.scalar_tensor_tensor(out=angr[:], in0=angk[:], scalar=-TWO_PI, in1=angr[:], op0=ALU.mult, op1=ALU.add), reads=[angk, angr], writes=[angr])
            fw.op(dve, lambda: V.tensor_scalar(out=angc[:], in0=angr[:], scalar1=math.pi, scalar2=-TWO_PI, op0=ALU.is_gt, op1=ALU.mult), reads=[angr], writes=[angc])
            fw.op(dve, lambda: V.tensor_tensor(out=angr[:], in0=angr[:], in1=angc[:], op=ALU.add), reads=[angr, angc], writes=[angr])
            fw.op(dve, lambda: V.tensor_scalar(out=angc[:], in0=angr[:], scalar1=-math.pi, scalar2=TWO_PI, op0=ALU.is_lt, op1=ALU.mult), reads=[angr], writes=[angc])
            fw.op(dve, lambda: V.tensor_tensor(out=angr[:], in0=angr[:], in1=angc[:], op=ALU.add), reads=[angr, angc], writes=[angr])
            fw.op(dve, lambda: V.tensor_scalar(out=angr[:], in0=angr[:], scalar1=-3.14159, scalar2=3.14159, op0=ALU.max, op1=ALU.min), reads=[angr], writes=[angr])
            fw.op(act, lambda: A.activation(out=dst[:], in_=angr[:], func=AF.Sin), reads=[angr], writes=[dst])

        reduce_sin(sinS, 0.0)
        reduce_sin(cosS, math.pi / 2.0)
        rt = [fw.sb(f"rt{i}", [128, 8], F32) for i in range(4)]

        def rope64(c0):
            v3 = z[:, c0:c0 + 64].rearrange("p (g d) -> p g d", d=32)
            x1 = v3[:, :, 0:4]
            x2 = v3[:, :, 4:8]
            cb = cosS[:].unsqueeze(1).broadcast_to([128, 2, 4])
            sbb = sinS[:].unsqueeze(1).broadcast_to([128, 2, 4])
            r = [x[:].rearrange("p (g d) -> p g d", d=4) for x in rt]
            fw.op(dve, lambda: V.tensor_tensor(out=r[0], in0=x1, in1=cb, op=ALU.mult), reads=[z, cosS], writes=[rt[0]])
            fw.op(dve, lambda: V.tensor_tensor(out=r[1], in0=x2, in1=sbb, op=ALU.mult), reads=[z, sinS], writes=[rt[1]])
            fw.op(dve, lambda: V.tensor_tensor(out=r[2], in0=x2, in1=cb, op=ALU.mult), reads=[z, cosS], writes=[rt[2]])
            fw.op(dve, lambda: V.tensor_tensor(out=r[3], in0=x1, in1=sbb, op=ALU.mult), reads=[z, sinS], writes=[rt[3]])
            fw.op(dve, lambda: V.tensor_tensor(out=x1, in0=r[0], in1=r[1], op=ALU.subtract), reads=[rt[0], rt[1]], writes=[z])
            fw.op(dve, lambda: V.tensor_tensor(out=x2, in0=r[2], in1=r[3], op=ALU.add), reads=[rt[2], rt[3]], writes=[z])

        rope64(0)
        rope64(64)
        bfc = fw.sb("bfc", [128, 1, 1], F32)
        fw.dma(sp, bfc[:], bf1[0:1, :].partition_broadcast(128), writes=[bfc])
        slf = fw.sb("slf", [128, 1], F32)
        e1 = fw.sb("e1", [128, 1], F32)
        fw.op(dve, lambda: V.tensor_tensor(out=slf[:], in0=z[:, 384:385], in1=bfc[:, 0, :], op=ALU.add), reads=[z, bfc], writes=[slf])
        fw.op(act, lambda: A.activation(out=e1[:], in_=slf[:], func=AF.Exp, scale=-1.0), reads=[slf], writes=[e1])
        fw.op(act, lambda: A.activation(out=slf[:], in_=e1[:], func=AF.Ln, bias=1.0), reads=[e1], writes=[slf])
        fw.op(dve, lambda: V.tensor_scalar(out=slf[:], in0=slf[:], scalar1=-1.0, scalar2=None, op0=ALU.mult), reads=[slf], writes=[slf])
        fw.dma(sp, nkd[:, :], z[:, 64:128], reads=[z], writes=[Buf("o")], sembuf=z, final=True)
        fw.dma(sp, nvd[:, :], z[:, 128:192], reads=[z], writes=[Buf("o")], sembuf=z, final=True)
        fw.dma(sp, nkf[:, :], z[:, 256:320], reads=[z], writes=[Buf("o")], sembuf=z, final=True)
        fw.dma(sp, nvf[:, :], z[:, 320:384], reads=[z], writes=[Buf("o")], sembuf=z, final=True)
        fw.dma(sp, nlf[:, :], slf[:], reads=[slf], writes=[Buf("o")], sembuf=slf, final=True)

        qk = fw.sb("qk", [128, 2, 128], F32)
        fw.op(dve, lambda: V.tensor_scalar(out=qk[:, 0, 0:64], in0=z[:, 0:64], scalar1=32.0 ** -0.5, scalar2=None, op0=ALU.mult), reads=[z], writes=[qk])
        fw.op(dve, lambda: V.tensor_scalar(out=qk[:, 0, 64:128], in0=z[:, 192:256], scalar1=0.125, scalar2=None, op0=ALU.mult), reads=[z], writes=[qk])
        fw.op(dve, lambda: V.tensor_copy(out=qk[:, 1, 0:64], in_=z[:, 64:128]), reads=[z], writes=[qk])
        fw.op(dve, lambda: V.tensor_copy(out=qk[:, 1, 64:128], in_=z[:, 256:320]), reads=[z], writes=[qk])
        pqb = PS[2][:]
        for a in range(2):
            fw.op(pe, lambda a=a: PE.transpose(out=pqb[:, a * 128:(a + 1) * 128], in_=qk[:, a, :], identity=ident[:]), reads=[qk, ident], writes=[PS[2]], inc=(a == 1))
        Qblk = fw.sb("Qblk", [128, 32, 12], F32)
        KTn = fw.sb("KTn", [128, 128], F32)
        for jb in range(3):
            fw.op(dve, lambda jb=jb: V.tensor_scalar(out=Qblk[:, :, jb * 4:(jb + 1) * 4], in0=pqb[:, 0:128].rearrange("p (s i) -> p s i", i=4), scalar1=rm[:, jb:jb + 1], scalar2=None, op0=ALU.mult),
                  reads=[PS[2], rm], writes=[Qblk])
        fw.op(dve, lambda: V.tensor_copy(out=KTn[:], in_=pqb[:, 128:256]), reads=[PS[2]], writes=[KTn])
        Vn = fw.sb("Vn", [128, 129], F32)
        fw.op(dve, lambda: V.memset(Vn[:], 1.0), writes=[Vn])
        fw.op(dve, lambda: V.tensor_copy(out=Vn[:, 0:64], in_=z[:, 128:192]), reads=[z], writes=[Vn])
        fw.op(dve, lambda: V.tensor_copy(out=Vn[:, 64:128], in_=z[:, 320:384]), reads=[z], writes=[Vn])
        biasN = fw.sb("biasN", [128, 1], F32)
        fw.op(pe, lambda: PE.matmul(PS[3][:, 0:1], lhsT=PN[:], rhs=slf[:], start=True, stop=True), reads=[PN, slf], writes=[PS[3]])
        fw.op(dve, lambda: V.tensor_scalar(out=biasN[:], in0=PS[3][:, 0:1], scalar1=-1.0, scalar2=None, op0=ALU.mult), reads=[PS[3]], writes=[biasN])

        ptb = fw.sb("ptb_sb", [128, 1, 2048], I32)
        fw.dma(sp, ptb[:], ptb_in[0:1, :].partition_broadcast(128), writes=[ptb])
        idxf = fw.sb("idxf", [128, 2048], F32)
        idx = fw.sb("idx", [128, 2048], I32)
        fw.op(dve, lambda: V.tensor_copy(out=idxf[:], in_=ptb[:, 0, :]), reads=[ptb], writes=[idxf])
        fw.op(dve, lambda: V.tensor_scalar(out=idxf[:], in0=idxf[:], scalar1=128.0, scalar2=rm[:, 3:4], op0=ALU.mult, op1=ALU.add), reads=[idxf, rm], writes=[idxf])
        fw.op(dve, lambda: V.tensor_copy(out=idx[:], in_=idxf[:]), reads=[idxf], writes=[idx])
        ptP = fw.sb("ptP_sb", [128, 16], I32)
        ld(ptP, ptP_in[:, :])
        LT = [fw.sb(f"LT{i}", [128, 128], F32) for i in range(2)]
        Lall = fw.sb("Lall", [128, 32, 64], F32)
        for i in range(16):
            lt = LT[i % 2]
            fw.dma(pool, lt[:], lf_pool[:, :], reads=[ptP], writes=[lt], indirect=ptP[:, i:i + 1].bitcast(U32))
            pt_ = PS[4 + (i % 2)]
            fw.op(pe, lambda lt=lt, pt_=pt_: PE.transpose(out=pt_[:, 0:128], in_=lt[:], identity=ident[:]), reads=[lt, ident], writes=[pt_])
            fw.op(dve, lambda i=i, pt_=pt_: V.tensor_copy(out=Lall[:, 2 * i:2 * i + 2, :].rearrange("p a b -> p (a b)"), in_=pt_[:, 0:128]), reads=[pt_], writes=[Lall])
        Tcol = fw.sb("Tcol", [64, 32], F32)
        Rm = fw.sb("Rm", [64, 32, 64], F32)
        biasP = fw.sb("biasP", [128, 32, 64], F32)
        for sq in range(32):
            fw.op(pe, lambda sq=sq: PE.matmul(PS[6][0:64, sq:sq + 1], lhsT=Lall[:, sq, :], rhs=ones32[:, 0:1], start=True, stop=True), reads=[Lall, ones32], writes=[PS[6]], inc=(sq == 31))
        fw.op(dve, lambda: V.tensor_copy(out=Tcol[:], in_=PS[6][0:64, 0:32]), reads=[PS[6]], writes=[Tcol])
        fw.op(dve, lambda: V.tensor_tensor(out=Rm[:], in0=Tcol[:].unsqueeze(2).broadcast_to([64, 32, 64]), in1=SUP[:].unsqueeze(1).broadcast_to([64, 32, 64]), op=ALU.mult), reads=[Tcol, SUP], writes=[Rm])
        for q4 in range(4):
            pb = PS[q4 % 2]
            fw.op(pe, lambda q4=q4, pb=pb: PE.matmul(pb[:, :], lhsT=SU[:], rhs=Lall[:, q4 * 8:(q4 + 1) * 8, :].rearrange("p a b -> p (a b)"), start=True, stop=False), reads=[SU, Lall], writes=[pb], inc=False)
            fw.op(pe, lambda q4=q4, pb=pb: PE.matmul(pb[:, :], lhsT=ones32[0:64, :], rhs=Rm[:, q4 * 8:(q4 + 1) * 8, :].rearrange("p a b -> p (a b)"), start=False, stop=True), reads=[ones32, Rm], writes=[pb])
            fw.op(dve, lambda q4=q4, pb=pb: V.tensor_copy(out=biasP[:, q4 * 8:(q4 + 1) * 8, :].rearrange("p a b -> p (a b)"), in_=pb[:, :]), reads=[pb], writes=[biasP])

        NKV = 48
        kv = [fw.sb(f"kv{i}", [128, 257], F32) for i in range(NKV)]
        for t_ in kv:
            fw.op(dve, lambda t_=t_: V.memset(t_[:, 256:257], 1.0), writes=[t_])
        PTt = [fw.sb(f"PTt{i}", [128, 4, 12], F32) for i in range(2)]
        sx = [fw.sb(f"sx{i}", [128, 4, 4], F32) for i in range(2)]
        PTn = fw.sb("PTn", [128, 12], F32)
        sxn = fw.sb("sxn", [128, 4], F32)
        On = [fw.sb(f"On{i}", [12, 129], F32) for i in range(2)]
        rl = [fw.sb(f"rl{i}", [12, 1], F32) for i in range(2)]
        PSS = [PS[0], PS[1]]
        PSO = [PS[2], PS[3]]
        PFD = PS[4]
        PFF = PS[5]
        kv_i = 0
        grp_i = 0
        for sq in range(32):
            po = PSO[sq % 2]
            pend = []

            def flush(pend=pend, po=po):
                for (first, last, lhs, rhs_t, rd) in pend:
                    fw.op(pe, lambda: PE.matmul(po[0:12, 0:129], lhsT=lhs, rhs=rhs_t[:, 128:257] if rhs_t is not Vn else rhs_t[:, :], start=first, stop=last), reads=rd, writes=[po])
                del pend[:]

            for g4 in range(16):
                pss = PSS[grp_i % 2]
                ptt = PTt[grp_i % 2]
                sxt = sx[grp_i % 2]
                grp_i += 1
                tiles = []
                fw._need(pool, [idx.b], [kv[(kv_i + 3) % NKV].b])
                for jj in range(4):
                    j = g4 * 4 + jj
                    t_ = kv[kv_i % NKV]
                    kv_i += 1
                    fw.dma(pool, t_[:, 0:256], kv_pool[:, :], reads=[idx], writes=[t_], indirect=idx[:, sq * 64 + j:sq * 64 + j + 1].bitcast(U32))
                    fw.op(pe, lambda jj=jj, t_=t_, pss=pss: PE.matmul(pss[:, jj * 12:(jj + 1) * 12], lhsT=t_[:, 0:128], rhs=Qblk[:, sq, :], start=True, stop=True), reads=[t_, Qblk], writes=[pss], inc=(jj == 3))
                    tiles.append(t_)
                flush()
                p3 = pss[:, 0:48].rearrange("p (a b) -> p a b", b=12)
                fw.op(dve, lambda: V.tensor_tensor(out=sxt[:], in0=p3[:, :, 8:12], in1=biasP[:, sq, g4 * 4:g4 * 4 + 4].unsqueeze(2).broadcast_to([128, 4, 4]), op=ALU.add), reads=[pss, biasP], writes=[sxt])
                fw.op(act, lambda: A.activation(out=ptt[:, :, 0:8], in_=p3[:, :, 0:8], func=AF.Exp), reads=[pss], writes=[ptt])
                fw.op(act, lambda: A.activation(out=ptt[:, :, 8:12], in_=sxt[:], func=AF.Exp), reads=[sxt], writes=[ptt])
                for jj in range(4):
                    pend.append((g4 == 0 and jj == 0, False, ptt[:, jj, :], tiles[jj], [ptt, tiles[jj]]))
            pss = PSS[grp_i % 2]
            grp_i += 1
            fw.op(pe, lambda: PE.matmul(pss[:, 0:12], lhsT=KTn[:], rhs=Qblk[:, sq, :], start=True, stop=True), reads=[KTn, Qblk], writes=[pss])
            flush()
            fw.op(dve, lambda: V.tensor_scalar(out=sxn[:], in0=pss[:, 8:12], scalar1=biasN[:, 0:1], scalar2=None, op0=ALU.add), reads=[pss, biasN], writes=[sxn])
            fw.op(act, lambda: A.activation(out=PTn[:, 0:8], in_=pss[:, 0:8], func=AF.Exp), reads=[pss], writes=[PTn])
            fw.op(act, lambda: A.activation(out=PTn[:, 8:12], in_=sxn[:], func=AF.Exp), reads=[sxn], writes=[PTn])
            fw.op(dve, lambda: V.tensor_tensor(out=PTn[:], in0=PTn[:], in1=mN[:, sq, :], op=ALU.mult), reads=[PTn, mN], writes=[PTn])
            pend.append((False, True, PTn[:, :], Vn, [PTn, Vn]))
            flush()
            on = On[sq % 2]
            rlt = rl[sq % 2]
            fw.op(dve, lambda: V.reciprocal(out=rlt[:], in_=po[0:12, 128:129]), reads=[po], writes=[rlt])
            fw.op(dve, lambda: V.tensor_scalar(out=on[:], in0=po[0:12, 0:129], scalar1=rlt[:, 0:1], scalar2=None, op0=ALU.mult), reads=[po, rlt], writes=[on])
            fw.op(pe, lambda: PE.matmul(PFD[:, 0:64], lhsT=SelD[:, sq * 128:(sq + 1) * 128], rhs=on[:, 0:64], start=(sq == 0), stop=(sq == 31)), reads=[SelD, on], writes=[PFD])
            fw.op(pe, lambda: PE.matmul(PFF[:, 0:64], lhsT=SelF[:, sq * 128:(sq + 1) * 128], rhs=on[:, 64:128], start=(sq == 0), stop=(sq == 31)), reads=[SelF, on], writes=[PFF])

        res = fw.sb("res", [128, 128], F32)
        od = fw.sb("od", [128, 64], F32)
        sqv = fw.sb("sqv", [128, 64], F32)
        ssq = fw.sb("ssq", [128, 1], F32)
        gb = fw.sb("gb", [128, 1, 64], F32)
        fw.dma(sp, gb[:], subln[0:1, :].partition_broadcast(128), writes=[gb])
        fw.op(dve, lambda: V.tensor_copy(out=od[:], in_=PFD[:, 0:64]), reads=[PFD], writes=[od])
        fw.op(dve, lambda: V.tensor_tensor(out=sqv[:], in0=od[:], in1=od[:], op=ALU.mult), reads=[od], writes=[sqv])
        fw.op(dve, lambda: V.tensor_reduce(out=ssq[:], in_=sqv[:], axis=AX.X, op=ALU.add), reads=[sqv], writes=[ssq])
        fw.op(act, lambda: A.activation(out=ssq[:], in_=ssq[:], func=AF.Sqrt, scale=1.0 / 64.0, bias=RMS_EPS), reads=[ssq], writes=[ssq])
        fw.op(dve, lambda: V.reciprocal(out=ssq[:], in_=ssq[:]), reads=[ssq], writes=[ssq])
        fw.op(dve, lambda: V.tensor_scalar(out=od[:], in0=od[:], scalar1=ssq[:, 0:1], scalar2=1.0 - LAM_INIT, op0=ALU.mult, op1=ALU.mult), reads=[od, ssq], writes=[od])
        fw.op(dve, lambda: V.tensor_tensor(out=res[:, 0:64], in0=od[:], in1=gb[:, 0, :], op=ALU.mult), reads=[od, gb], writes=[res])
        fw.op(dve, lambda: V.tensor_copy(out=res[:, 64:128], in_=PFF[:, 0:64]), reads=[PFF], writes=[res])
        fw.dma(sp, attn_c[:, :], res[:], reads=[res], writes=[Buf("o")], sembuf=res, final=True)
        fw.finish()
        print("sample program: instructions", fw.ninst, "semaphores", fw.nsem)
    return nc


def run_sample(inputs):
    f32 = np.float32
    if "sample" not in _NC_CACHE:
        _NC_CACHE["sample"] = build_sample()
    nc = _NC_CACHE["sample"]
    ident, U, dm, mk, invf = host_consts()
    SU, SUP, rm, mN, E, PN = host_consts_A()
    xs = np.ascontiguousarray(np.asarray(inputs["x_sample"], f32).reshape(128, D))
    w_in = np.asarray(inputs["w_in"][0], f32)
    pt = np.asarray(inputs["page_table"]).astype(np.int32)
    lam4 = np.stack([inputs["lambda_q1"][0], inputs["lambda_k1"][0], inputs["lambda_q2"][0], inputs["lambda_k2"][0]]).astype(f32)
    spos = (PAST + (np.arange(128) % 4)).astype(f32).reshape(128, 1)
    ptb = np.ascontiguousarray(pt.reshape(1, 2048))
    ptP = np.ascontiguousarray(pt.reshape(16, 128).T)
    ck = np.asarray(inputs["cache_diff_k"][0], f32).reshape(NPOOL, 128, 8, 64)
    cv = np.asarray(inputs["cache_diff_v"][0], f32)
    fk = np.asarray(inputs["cache_fox_k"][0], f32)
    fv = np.asarray(inputs["cache_fox_v"][0], f32)
    fl = np.asarray(inputs["cache_fox_logf"][0], f32)
    shared = {"xs": xs, "lam4": lam4, "subln": np.asarray(inputs["subln_gain"], f32).reshape(1, 64), "spos": spos, "ptb": ptb, "ptP": ptP,
              "c_ident": ident, "c_SU": SU, "c_SUP": SUP, "c_rm": rm, "c_mN": mN, "c_E": E, "c_PN": PN, "c_invf": invf}
    in_maps = []
    for c in range(8):
        m = dict(shared)
        cols = np.concatenate([C_QD + c * 64 + np.arange(64), C_KD + c * 64 + np.arange(64), C_VD + c * 64 + np.arange(64),
                               C_QF + c * 64 + np.arange(64), C_KF + c * 64 + np.arange(64), C_VF + c * 64 + np.arange(64), [C_FL + c]])
        m["w_s"] = np.ascontiguousarray(w_in[:, cols])
        m["bf1"] = np.asarray(inputs["b_forget"], f32).reshape(8)[c].reshape(1, 1)
        kvp = np.empty((NPOOL, 128, 256), f32)
        kvp[:, 0:64, 0:128] = ck[:, :, c, :].transpose(0, 2, 1)
        kvp[:, 64:128, 0:128] = fk[:, :, c, :].transpose(0, 2, 1)
        kvp[:, :, 128:192] = cv[:, :, c, :]
        kvp[:, :, 192:256] = fv[:, :, c, :]
        m["kv_pool"] = kvp.reshape(NPOOL * 128, 256)
        m["lf_pool"] = np.ascontiguousarray(fl[:, :, c])
        in_maps.append(m)
    res = run_bass_kernel_spmd(nc, in_maps, core_ids=list(range(8)))
    attn = np.zeros((128, 16, 64), f32)
    nkd = np.zeros((128, 8, 64), f32)
    nvd = np.zeros((128, 8, 64), f32)
    nkf = np.zeros((128, 8, 64), f32)
    nvf = np.zeros((128, 8, 64), f32)
    nlf = np.zeros((128, 8), f32)
    for c in range(8):
        r = res.results[c]
        attn[:, c, :] = r["attn_c"][:, 0:64]
        attn[:, 8 + c, :] = r["attn_c"][:, 64:128]
        nkd[:, c] = r["nkd"]
        nvd[:, c] = r["nvd"]
        nkf[:, c] = r["nkf"]
        nvf[:, c] = r["nvf"]
        nlf[:, c] = r["nlf"][:, 0]
    return (attn.reshape(128, 1024), nkd.reshape(1, 32, 4, 8, 2, 32), nvd.reshape(1, 32, 4, 8, 64), nkf.reshape(1, 32, 4, 8, 64),
            nvf.reshape(1, 32, 4, 8, 64), nlf.reshape(1, 32, 4, 8))


def kernel(**inputs):
    attn_s, nkd, nvd, nkf, nvf, nlf = run_sample(inputs)
    y_p, y_s, kd, vd, kf, vf, lf = run_main(inputs, attn_s)
    return (y_p, y_s, kd, vd, kf, vf, lf, nkd, nvd, nkf, nvf, nlf)


def chunk_order(cc):
    order = []
    for s in range(4):
        order += [4 * s + cc] + [4 * s + j for j in range(4) if j != cc]
    return order


def run_main(inputs, attn_s):
    f32 = np.float32
    if "main" not in _NC_CACHE:
        _NC_CACHE["main"] = build_main()
    nc = _NC_CACHE["main"]
    ident, U, dm, mk, invf = host_consts()
    xp = np.asarray(inputs["x_prompt"], f32)
    xs = np.asarray(inputs["x_sample"], f32).reshape(128, D)
    w_g = np.concatenate([np.asarray(inputs["w_exp_gate"][0], f32), np.asarray(inputs["w_sh_gate"][0], f32)[None]], axis=0)
    w_u = np.concatenate([np.asarray(inputs["w_exp_up"][0], f32), np.asarray(inputs["w_sh_up"][0], f32)[None]], axis=0)
    w_d = np.concatenate([np.asarray(inputs["w_exp_down"][0], f32), np.asarray(inputs["w_sh_down"][0], f32)[None]], axis=0)
    lam4 = np.stack([inputs["lambda_q1"][0], inputs["lambda_k1"][0], inputs["lambda_q2"][0], inputs["lambda_k2"][0]]).astype(f32)
    shared = {
        "w_in": np.ascontiguousarray(inputs["w_in"][0], f32), "b_forget": np.asarray(inputs["b_forget"], f32).reshape(1, 8),
        "lam4": lam4, "subln": np.asarray(inputs["subln_gain"], f32).reshape(64, 1), "w_o": np.ascontiguousarray(inputs["w_o"][0], f32),
        "ln1": np.stack([inputs["ln1_g"][0], inputs["ln1_b"][0]]).astype(f32), "ln2": np.stack([inputs["ln2_g"][0], inputs["ln2_b"][0]]).astype(f32),
        "w_router": np.ascontiguousarray(inputs["w_router"][0], f32), "r_bias": np.asarray(inputs["router_bias"], f32).reshape(1, NE),
        "w_g": w_g, "w_u": w_u, "w_d": w_d,
        "c_ident": ident, "c_U": U, "c_dm": dm, "c_mk": mk, "c_invf": invf,
    }
    in_maps = []
    toks = []
    for c in range(8):
        b, cc = divmod(c, 4)
        tok = np.concatenate([np.arange(ch * 512, (ch + 1) * 512) for ch in chunk_order(cc)])
        toks.append(tok)
        tt = tok.reshape(NT, 128)
        m = dict(shared)
        m["x_perm"] = np.ascontiguousarray(xp[b][tok])
        m["kpos"] = np.ascontiguousarray(tt.T.astype(f32))
        m["tpos"] = np.ascontiguousarray(tt[:, 0:1].astype(f32))
        m["qfirst"] = np.array([[tok[s * 2048] for s in range(4)]], f32)
        m["x_s"] = np.ascontiguousarray(xs[16 * c:16 * c + 16])
        m["attn_s"] = np.ascontiguousarray(attn_s[16 * c:16 * c + 16])
        in_maps.append(m)
    res = run_bass_kernel_spmd(nc, in_maps, core_ids=list(range(8)))
    y_p = np.zeros((2, SEQ, D), f32)
    y_s = np.zeros((128, D), f32)
    kd = np.zeros((2, SEQ, 512), f32)
    vd = np.zeros((2, SEQ, 512), f32)
    kf = np.zeros((2, SEQ, 512), f32)
    vf = np.zeros((2, SEQ, 512), f32)
    lf = np.zeros((2, SEQ, 8), f32)
    for c in range(8):
        b, cc = divmod(c, 4)
        r = res.results[c]
        own = np.concatenate([toks[c][s * 2048:s * 2048 + 512] for s in range(4)])
        y_p[b, own] = r["y_own"]
        kd[b, own] = r["kd_own"]
        vd[b, own] = r["vd_own"]
        kf[b, own] = r["kf_own"]
        vf[b, own] = r["vf_own"]
        lf[b, own] = r["lf_own"]
        y_s[16 * c:16 * c + 16] = r["y_s"]
    return (y_p, y_s.reshape(32, 4, D), kd.reshape(1, 2, SEQ, 8, 2, 32), vd.reshape(1, 2, SEQ, 8, 64),
            kf.reshape(1, 2, SEQ, 8, 64), vf.reshape(1, 2, SEQ, 8, 64), lf.reshape(1, 2, SEQ, 8))


if __name__ == "__main__":
    build_sample()
    build_main()
```

```python
import math
import numpy as np
from contextlib import ExitStack
import concourse.bass as bass
import concourse.mybir as mybir
from concourse.bass_utils import run_bass_kernel_spmd

F32 = mybir.dt.float32
BF16 = mybir.dt.bfloat16
I32 = mybir.dt.int32
U32 = mybir.dt.uint32
AF = mybir.ActivationFunctionType
ALU = mybir.AluOpType
AX = mybir.AxisListType

D = 1024
SEQ = 8192
NT = 64
NCH = 16
NOWN = 16
NE = 64
DEEP_ALPHA = 2.0 ** 0.25
LAM_INIT = 0.8 - 0.6 * math.exp(0.0)
LN_EPS = 1e-5
RMS_EPS = 1e-5
ROPE_THETA = 500000.0
C_QD, C_KD, C_VD, C_QF, C_KF, C_VF, C_FL = 0, 512, 1024, 1536, 2048, 2560, 3072
D_IN = 3080
NEGBIG = -30000.0
PAST = 8192


class Buf:
    __slots__ = ("name", "lw", "rd", "sem", "semval")

    def __init__(self, name):
        self.name = name
        self.lw = None
        self.rd = {}
        self.sem = None
        self.semval = 0


class Eng:
    def __init__(self, name, handle, sem):
        self.name = name
        self.h = handle
        self.sem = sem
        self.count = 0
        self.waited = {}
        self.same_engine_sync = name in ("act", "dve", "pool")


class T:
    def __init__(self, t, name):
        self.t = t
        self.b = Buf(name)

    def __getitem__(self, k):
        return self.t[k]


class FW:
    def __init__(self, nc, stack):
        self.nc = nc
        self.stack = stack
        self.root = stack
        self.nsem = 0
        self.dma_bufs = []
        self.pe = Eng("pe", nc.tensor, self.new_sem("pe"))
        self.act = Eng("act", nc.scalar, self.new_sem("act"))
        self.dve = Eng("dve", nc.vector, self.new_sem("dve"))
        self.pool = Eng("pool", nc.gpsimd, self.new_sem("pool"))
        self.sp = Eng("sp", nc.sync, self.new_sem("sp"))
        self.ninst = 0
        self.out_events = []

    def new_sem(self, name):
        s = self.root.enter_context(self.nc.semaphore(name))
        self.nsem += 1
        return s

    def sb(self, name, shape, dt):
        return T(self.stack.enter_context(self.nc.sbuf_tensor(name, list(shape), dt)), name)

    def ps(self, name, shape, dt):
        return T(self.stack.enter_context(self.nc.psum_tensor(name, list(shape), dt)), name)

    def _need(self, eng, reads, writes):
        deps = {}

        def add(ev):
            if ev is None:
                return
            k, v = ev
            if deps.get(id(k), (None, 0))[1] < v:
                deps[id(k)] = (k, v)

        for b in reads:
            add(b.lw)
        for b in writes:
            add(b.lw)
            for kv in b.rd.values():
                add(kv)
        for k, v in deps.values():
            if k is eng.sem:
                if not eng.same_engine_sync or v > eng.count:
                    continue
            if eng.waited.get(id(k), 0) >= v:
                continue
            eng.h.wait_ge(k, v)
            eng.waited[id(k)] = v

    def op(self, eng, fn, reads=(), writes=(), inc=True):
        reads = [r.b if isinstance(r, T) else r for r in reads]
        writes = [w.b if isinstance(w, T) else w for w in writes]
        self._need(eng, reads, writes)
        ins = fn()
        self.ninst += 1
        if inc:
            eng.count += 1
            ins.then_inc(eng.sem, 1)
            ev = (eng.sem, eng.count)
        else:
            ev = (eng.sem, eng.count + 1)
        for b in writes:
            b.lw = ev
            b.rd = {}
        for b in reads:
            if b.rd.get(id(ev[0]), (None, 0))[1] < ev[1]:
                b.rd[id(ev[0])] = ev
        return ins

    def dma(self, q, out, in_, reads=(), writes=(), sembuf=None, indirect=None, final=False):
        reads = [r.b if isinstance(r, T) else r for r in reads]
        writes = [w.b if isinstance(w, T) else w for w in writes]
        if sembuf is None:
            sembuf = writes[0] if writes else reads[0]
        if isinstance(sembuf, T):
            sembuf = sembuf.b
        if sembuf.sem is None:
            sembuf.sem = self.new_sem("d_" + sembuf.name)
            self.dma_bufs.append(sembuf)
        self._need(q, reads, writes)
        if sembuf.semval > 0 and q.waited.get(id(sembuf.sem), 0) < sembuf.semval:
            q.h.wait_ge(sembuf.sem, sembuf.semval)
            q.waited[id(sembuf.sem)] = sembuf.semval
        if indirect is not None:
            ins = q.h.indirect_dma_start(out=out, out_offset=None, in_=in_,
                                         in_offset=bass.IndirectOffsetOnAxis(ap=indirect, axis=0))
        else:
            ins = q.h.dma_start(out=out, in_=in_)
        self.ninst += 1
        sembuf.semval += 16
        ins.then_inc(sembuf.sem, 16)
        ev = (sembuf.sem, sembuf.semval)
        for b in writes:
            b.lw = ev
            b.rd = {}
        for b in reads:
            b.rd[id(ev[0])] = ev
        if final:
            self.out_events = [e for e in self.out_events if e[0] is not ev[0]] + [ev]
        return ev

    def barrier(self):
        sp = self.sp
        engs = [self.pe, self.act, self.dve, self.pool]
        for b in self.dma_bufs:
            if b.semval > 0 and sp.waited.get(id(b.sem), 0) < b.semval:
                sp.h.wait_ge(b.sem, b.semval)
                sp.waited[id(b.sem)] = b.semval
        for f in engs:
            if f.count > sp.waited.get(id(f.sem), 0):
                sp.h.wait_ge(f.sem, f.count)
                sp.waited[id(f.sem)] = f.count
        sp.count += 1
        sp.h.nop().then_inc(sp.sem, 1)
        for e in engs:
            e.h.wait_ge(sp.sem, sp.count)
            e.waited[id(sp.sem)] = sp.count

    def push(self):
        es = ExitStack()
        es.__enter__()
        prev = self.stack
        self.stack = es
        return (es, prev)

    def pop(self, ph):
        self.barrier()
        ph[0].__exit__(None, None, None)
        self.stack = ph[1]

    def finish(self):
        for k, v in self.out_events:
            self.sp.h.wait_ge(k, v)


def host_consts():
    p = np.arange(128)
    ident = np.eye(128, dtype=np.float32)
    U = (p[:, None] <= p[None, :]).astype(np.float32)
    q = np.arange(512)
    dm = np.stack([(128 * j + p[:, None] <= q[None, :]) for j in range(4)], axis=1).astype(np.float32)
    mk = np.zeros((128, 4), np.float32)
    mk[:, 0] = ((p // 32) % 2 == 0)
    mk[:, 1] = ((p // 32) % 2 == 1)
    mk[:, 2] = (p == 0)
    mk[:, 3] = 1.0
    half = 4
    inv_freq = np.power(np.float32(ROPE_THETA), -np.arange(half, dtype=np.float32) * np.float32(2.0) / np.float32(8)).astype(np.float32)
    invf = np.tile(inv_freq[None, :], (128, 1)).astype(np.float32)
    return ident, U, dm.reshape(128, 2048), mk, invf


def build_main():
    nc = bass.Bass("TRN2", target_bir_lowering=False)

    def din(name, shape, dt=F32):
        return nc.dram_tensor(name, list(shape), dt, kind="ExternalInput").ap()

    def dout(name, shape, dt=F32):
        return nc.dram_tensor(name, list(shape), dt, kind="ExternalOutput").ap()

    x_perm = din("x_perm", [SEQ, D])
    kpos = din("kpos", [128, NT])
    tpos_in = din("tpos", [NT, 1])
    qfirst_in = din("qfirst", [1, 4])
    x_s = din("x_s", [16, D])
    attn_s_in = din("attn_s", [16, D])
    w_in = din("w_in", [D, D_IN])
    b_forget = din("b_forget", [1, 8])
    lam4 = din("lam4", [4, 32])
    subln = din("subln", [64, 1])
    w_o = din("w_o", [D, D])
    ln1 = din("ln1", [2, D])
    ln2 = din("ln2", [2, D])
    w_router = din("w_router", [D, NE])
    r_bias = din("r_bias", [1, NE])
    w_g = din("w_g", [NE + 1, D, 256])
    w_u = din("w_u", [NE + 1, D, 256])
    w_d = din("w_d", [NE + 1, 256, D])
    c_ident = din("c_ident", [128, 128])
    c_U = din("c_U", [128, 128])
    c_dm = din("c_dm", [128, 2048])
    c_mk = din("c_mk", [128, 4])
    c_invf = din("c_invf", [128, 4])

    y_own = dout("y_own", [2048, D])
    y_s = dout("y_s", [16, D])
    kd_own = dout("kd_own", [2048, 512])
    vd_own = dout("vd_own", [2048, 512])
    kf_own = dout("kf_own", [2048, 512])
    vf_own = dout("vf_own", [2048, 512])
    lf_own = dout("lf_own", [2048, 8])

    kT_scr = nc.dram_tensor("kT_scr", [8, 128, SEQ], BF16, kind="Internal").ap()
    v_scr = nc.dram_tensor("v_scr", [8, 128, NT, 130], BF16, kind="Internal").ap()
    attn_scr = nc.dram_tensor("attn_scr", [2048, D], BF16, kind="Internal").ap()

    with ExitStack() as st:
        fw = FW(nc, st)
        pe, act, dve, pool, sp = fw.pe, fw.act, fw.dve, fw.pool, fw.sp
        V = nc.vector
        A = nc.scalar
        G = nc.gpsimd
        PE = nc.tensor

        PS = [fw.ps(f"ps{i}", [128, 512], F32) for i in range(8)]

        ident = fw.sb("ident", [128, 128], F32)
        identb = fw.sb("identb", [128, 128], BF16)
        Umat = fw.sb("Umat", [128, 128], F32)
        mk = fw.sb("mk", [128, 4], F32)
        invf = fw.sb("invf", [128, 4], F32)
        kpos_sb = fw.sb("kpos_sb", [128, NT], F32)
        cosT = fw.sb("cosT", [128, NT, 4], F32)
        sinT = fw.sb("sinT", [128, NT, 4], F32)
        bf_bc = fw.sb("bf_bc", [128, 1, 8], F32)
        logf = fw.sb("logf", [128, NT, 8], F32)
        ones32 = fw.sb("ones32", [128, 128], F32)
        onesb = fw.sb("onesb", [128, 128], BF16)
        lamt = fw.sb("lamt", [128, 1], F32)
        gainc = fw.sb("gainc", [64, 1], F32)

        ld = lambda t, src, q=sp: fw.dma(q, t[:], src, writes=[t])
        ld(ident, c_ident[:, :])
        ld(Umat, c_U[:, :])
        ld(mk, c_mk[:, :])
        ld(invf, c_invf[:, :])
        ld(kpos_sb, kpos[:, :])
        fw.dma(sp, bf_bc[:], b_forget[0:1, :].partition_broadcast(128), writes=[bf_bc])
        fw.op(dve, lambda: V.tensor_copy(out=identb[:], in_=ident[:]), reads=[ident], writes=[identb])
        fw.op(dve, lambda: V.memset(ones32[:], 1.0), writes=[ones32])
        fw.op(dve, lambda: V.memset(onesb[:], 1.0), writes=[onesb])

        lamv = fw.sb("lamv", [128, 4, 32], F32)
        fw.dma(sp, lamv[:].rearrange("p a b -> p (a b)"),
               lam4.rearrange("a b -> (a b)").unsqueeze(0).partition_broadcast(128).squeeze(1)
               if False else lam4.rearrange("(o a) b -> o (a b)", o=1).partition_broadcast(128).squeeze(1),
               writes=[lamv])
        lprod = fw.sb("lprod", [128, 2, 32], F32)
        lsum = fw.sb("lsum", [128, 2], F32)
        lexp = fw.sb("lexp", [128, 2], F32)
        fw.op(dve, lambda: V.tensor_tensor(out=lprod[:, 0, :], in0=lamv[:, 0, :], in1=lamv[:, 1, :], op=ALU.mult), reads=[lamv], writes=[lprod])
        fw.op(dve, lambda: V.tensor_tensor(out=lprod[:, 1, :], in0=lamv[:, 2, :], in1=lamv[:, 3, :], op=ALU.mult), reads=[lamv], writes=[lprod])
        fw.op(dve, lambda: V.tensor_reduce(out=lsum[:], in_=lprod[:], axis=AX.X, op=ALU.add), reads=[lprod], writes=[lsum])
        fw.op(act, lambda: A.activation(out=lexp[:], in_=lsum[:], func=AF.Exp), reads=[lsum], writes=[lexp])
        fw.op(dve, lambda: V.tensor_tensor(out=lamt[:], in0=lexp[:, 1:2], in1=lexp[:, 0:1], op=ALU.subtract), reads=[lexp], writes=[lamt])
        fw.op(dve, lambda: V.tensor_scalar(out=lamt[:], in0=lamt[:], scalar1=-LAM_INIT, scalar2=None, op0=ALU.add), reads=[lamt], writes=[lamt])
        ld(gainc, subln[:, :])
        fw.op(dve, lambda: V.tensor_scalar(out=gainc[:], in0=gainc[:], scalar1=1.0 - LAM_INIT, scalar2=None, op0=ALU.mult), reads=[gainc], writes=[gainc])

        ang = fw.sb("ang", [128, NT, 4], F32)
        angi = fw.sb("angi", [128, NT, 4], I32)
        angk = fw.sb("angk", [128, NT, 4], F32)
        angr = fw.sb("angr", [128, NT, 4], F32)
        angc = fw.sb("angc", [128, NT, 4], F32)
        TWO_PI = 2.0 * math.pi

        def reduce_sin(dst, shift):
            fw.op(dve, lambda: V.tensor_scalar(out=angk[:], in0=ang[:], scalar1=shift, scalar2=1.0 / TWO_PI, op0=ALU.add, op1=ALU.mult), reads=[ang], writes=[angk])
            fw.op(dve, lambda: V.tensor_copy(out=angi[:], in_=angk[:]), reads=[angk], writes=[angi])
            fw.op(dve, lambda: V.tensor_copy(out=angk[:], in_=angi[:]), reads=[angi], writes=[angk])
            fw.op(dve, lambda: V.tensor_scalar(out=angr[:], in0=ang[:], scalar1=shift, scalar2=None, op0=ALU.add), reads=[ang], writes=[angr])
            fw.op(dve, lambda: V.scalar_tensor_tensor(out=angr[:], in0=angk[:], scalar=-TWO_PI, in1=angr[:], op0=ALU.mult, op1=ALU.add), reads=[angk, angr], writes=[angr])
            fw.op(dve, lambda: V.tensor_scalar(out=angc[:], in0=angr[:], scalar1=math.pi, scalar2=-TWO_PI, op0=ALU.is_gt, op1=ALU.mult), reads=[angr], writes=[angc])
            fw.op(dve, lambda: V.tensor_tensor(out=angr[:], in0=angr[:], in1=angc[:], op=ALU.add), reads=[angr, angc], writes=[angr])
            fw.op(dve, lambda: V.tensor_scalar(out=angc[:], in0=angr[:], scalar1=-math.pi, scalar2=TWO_PI, op0=ALU.is_lt, op1=ALU.mult), reads=[angr], writes=[angc])
            fw.op(dve, lambda: V.tensor_tensor(out=angr[:], in0=angr[:], in1=angc[:], op=ALU.add), reads=[angr, angc], writes=[angr])
            fw.op(dve, lambda: V.tensor_scalar(out=angr[:], in0=angr[:], scalar1=-3.14159, scalar2=3.14159, op0=ALU.max, op1=ALU.min), reads=[angr], writes=[angr])
            fw.op(act, lambda: A.activation(out=dst[:], in_=angr[:], func=AF.Sin), reads=[angr], writes=[dst])

        fw.op(dve, lambda: V.tensor_tensor(out=ang[:], in0=kpos_sb[:].unsqueeze(2).broadcast_to([128, NT, 4]),
                                           in1=invf[:].unsqueeze(1).broadcast_to([128, NT, 4]), op=ALU.mult),
              reads=[kpos_sb, invf], writes=[ang])
        reduce_sin(sinT, 0.0)
        reduce_sin(cosT, math.pi / 2.0)

        S12 = fw.push()
        qT = fw.sb("qT", [128, 8, 2048], BF16)
        S1 = fw.push()
        win = fw.sb("win", [128, 8, D_IN], BF16)
        for kc in range(8):
            for hlf in range(2):
                c0 = hlf * 1540
                fw.dma(pool, win[:, kc, c0:c0 + 1540], w_in[kc * 128:(kc + 1) * 128, c0:c0 + 1540], writes=[win])
        xb = [fw.sb(f"xb{i}", [128, D], BF16) for i in range(3)]
        xf = [fw.sb(f"xf{i}", [128, D], F32) for i in range(3)]
        xT = [fw.sb(f"xT{i}", [128, 8, 128], BF16) for i in range(2)]
        NST = 2
        stg = {nm: [fw.sb(f"s_{nm}{i}", [128, 512], F32) for i in range(NST)] for nm in ("kd", "vd", "kf", "vf", "qd", "qf")}
        stb = {nm: [fw.sb(f"b_{nm}{i}", [128, 512], BF16) for i in range(NST)] for nm in ("kd", "kf", "qd", "qf")}
        vaug = [fw.sb(f"vaug{i}", [128, 16, 65], BF16) for i in range(2)]
        for vv in vaug:
            fw.op(pool, lambda vv=vv: G.memset(vv[:], 1.0), writes=[vv])
        kTst = [fw.sb(f"kTst{i}", [128, 8, 512], BF16) for i in range(2)]
        rt = [fw.sb(f"rt{i}", [128, 64], F32) for i in range(4)]
        zt = fw.sb("zt", [128, 32], F32)
        et = fw.sb("et", [128, 32], F32)
        PX = [PS[0], PS[1]]
        PJ = [PS[2], PS[3], PS[4]]
        PL = PS[5]
        PK = [PS[6], PS[7]]
        colof = {"qd": C_QD, "kd": C_KD, "vd": C_VD, "qf": C_QF, "kf": C_KF, "vf": C_VF}
        outd = {"kd": kd_own, "vd": vd_own, "kf": kf_own, "vf": vf_own}
        kT_b = [Buf(f"kTscr{c}") for c in range(NCH)]
        v_b = [Buf(f"vscr{t}") for t in range(NT)]

        def rope(s_t, t):
            v3 = s_t[:].rearrange("p (g d) -> p g d", d=32)
            x1 = v3[:, :, 0:4]
            x2 = v3[:, :, 4:8]
            cb = cosT[:, t, :].unsqueeze(1).broadcast_to([128, 16, 4])
            sbb = sinT[:, t, :].unsqueeze(1).broadcast_to([128, 16, 4])
            r = [x[:].rearrange("p (g d) -> p g d", d=4) for x in rt]
            fw.op(dve, lambda: V.tensor_tensor(out=r[0], in0=x1, in1=cb, op=ALU.mult), reads=[s_t, cosT], writes=[rt[0]])
            fw.op(dve, lambda: V.tensor_tensor(out=r[1], in0=x2, in1=sbb, op=ALU.mult), reads=[s_t, sinT], writes=[rt[1]])
            fw.op(dve, lambda: V.tensor_tensor(out=r[2], in0=x2, in1=cb, op=ALU.mult), reads=[s_t, cosT], writes=[rt[2]])
            fw.op(dve, lambda: V.tensor_tensor(out=r[3], in0=x1, in1=sbb, op=ALU.mult), reads=[s_t, sinT], writes=[rt[3]])
            fw.op(dve, lambda: V.tensor_tensor(out=x1, in0=r[0], in1=r[1], op=ALU.subtract), reads=[rt[0], rt[1]], writes=[s_t])
            fw.op(dve, lambda: V.tensor_tensor(out=x2, in0=r[2], in1=r[3], op=ALU.add), reads=[rt[2], rt[3]], writes=[s_t])

        pjc = [0]

        def x_load(t):
            xft = xf[t % 3]
            fw.dma(sp, xft[:], x_perm[t * 128:(t + 1) * 128, :], writes=[xft])

        def P1(t):
            xbt = xb[t % 3]
            xTt = xT[t % 2]
            px = PX[t % 2]
            xft = xf[t % 3]
            fw.op(pool, lambda: G.tensor_copy(out=xbt[:], in_=xft[:]), reads=[xft], writes=[xbt])
            pxb = px[:].bitcast(BF16)
            for kc in range(8):
                fw.op(pe, lambda kc=kc: PE.transpose(out=pxb[:, kc * 128:(kc + 1) * 128], in_=xbt[:, kc * 128:(kc + 1) * 128], identity=identb[:]),
                      reads=[xbt, identb], writes=[px], inc=(kc == 7))
            fw.op(dve, lambda: V.tensor_copy(out=xTt[:].rearrange("p a b -> p (a b)"), in_=pxb[:, 0:1024]), reads=[px], writes=[xTt])

        def P2(t):
            ch, j = divmod(t, 4)
            own = (ch % 4 == 0)
            so = ch // 4
            xTt = xT[t % 2]
            groups = ["kd", "vd", "kf", "vf"] + (["qd", "qf"] if own else [])
            si = t % NST
            for nm in groups:
                pj = PJ[pjc[0] % 3]
                pjc[0] += 1
                c0 = colof[nm]
                for kc in range(8):
                    fw.op(pe, lambda kc=kc, pj=pj, c0=c0: PE.matmul(pj[:, :], lhsT=xTt[:, kc, :], rhs=win[:, kc, c0:c0 + 512], start=(kc == 0), stop=(kc == 7)),
                          reads=[xTt, win], writes=[pj], inc=(kc == 7))
                s_t = stg[nm][si]
                va = vaug[t % 2]
                h0 = 0 if nm == "vd" else 8
                need32 = own or nm in ("kd", "qd")
                if need32:
                    fw.op(act, lambda pj=pj, s_t=s_t: A.activation(out=s_t[:], in_=pj[:, :], func=AF.Copy), reads=[pj], writes=[s_t])
                    if nm in ("kd", "qd"):
                        rope(s_t, t)
                    if nm in ("kd", "kf", "qd", "qf"):
                        b_t = stb[nm][si]
                        fw.op(dve, lambda s_t=s_t, b_t=b_t: V.tensor_copy(out=b_t[:], in_=s_t[:]), reads=[s_t], writes=[b_t])
                    else:
                        fw.op(dve, lambda s_t=s_t, va=va, h0=h0: V.tensor_copy(out=va[:, h0:h0 + 8, 0:64], in_=s_t[:].rearrange("p (h e) -> p h e", e=64)),
                              reads=[s_t], writes=[va])
                elif nm == "kf":
                    b_t = stb[nm][si]
                    fw.op(act, lambda pj=pj, b_t=b_t: A.activation(out=b_t[:], in_=pj[:, :], func=AF.Copy), reads=[pj], writes=[b_t])
                else:
                    fw.op(act, lambda pj=pj, va=va, h0=h0: A.activation(out=va[:, h0:h0 + 8, 0:64], in_=pj[:, :].rearrange("p (h e) -> p h e", e=64), func=AF.Copy),
                          reads=[pj], writes=[va])
                if own and nm in outd:
                    r0 = so * 512 + j * 128
                    fw.dma(sp, outd[nm][r0:r0 + 128, :], s_t[:], reads=[s_t], writes=[Buf("o")], sembuf=s_t, final=True)
            for kc in range(8):
                fw.op(pe, lambda kc=kc: PE.matmul(PL[:, j * 8:(j + 1) * 8], lhsT=xTt[:, kc, :], rhs=win[:, kc, C_FL:C_FL + 8], start=(kc == 0), stop=(kc == 7)),
                      reads=[xTt, win], writes=[PL], inc=(kc == 7))
            if j == 3:
                fw.op(dve, lambda: V.tensor_tensor(out=zt[:].rearrange("p (a h) -> p a h", h=8), in0=PL[:, 0:32].rearrange("p (a h) -> p a h", h=8),
                                                   in1=bf_bc[:].broadcast_to([128, 4, 8]), op=ALU.add), reads=[PL, bf_bc], writes=[zt])
                fw.op(act, lambda: A.activation(out=et[:], in_=zt[:], func=AF.Exp, scale=-1.0), reads=[zt], writes=[et])
                fw.op(act, lambda: A.activation(out=zt[:], in_=et[:], func=AF.Ln, bias=1.0), reads=[et], writes=[zt])
                fw.op(dve, lambda: V.tensor_scalar(out=logf[:, ch * 4:ch * 4 + 4, :], in0=zt[:].rearrange("p (a h) -> p a h", h=8), scalar1=-1.0, scalar2=None, op0=ALU.mult),
                      reads=[zt], writes=[logf])
                if own:
                    fw.dma(sp, lf_own[so * 512:(so + 1) * 512, :].rearrange("(a p) h -> p a h", p=128), logf[:, ch * 4:ch * 4 + 4, :],
                           reads=[logf], writes=[Buf("o")], sembuf=logf, final=True)

        def P3(t):
            ch, j = divmod(t, 4)
            own = (ch % 4 == 0)
            so = ch // 4
            si = t % NST
            pk = PK[t % 2]
            pkb = pk[:].bitcast(BF16)
            kst = kTst[ch % 2]
            for gi in range(8):
                src = stb["kd"][si] if gi < 4 else stb["kf"][si]
                g4 = gi % 4
                fw.op(pe, lambda gi=gi, src=src, g4=g4: PE.transpose(out=pkb[:, gi * 128:(gi + 1) * 128], in_=src[:, g4 * 128:(g4 + 1) * 128], identity=identb[:]),
                      reads=[src, identb], writes=[pk], inc=(gi == 7))
            fw.op(dve, lambda: V.tensor_copy(out=kst[:, :, j * 128:(j + 1) * 128], in_=pkb[:, 0:1024].rearrange("p (g n) -> p g n", n=128)),
                  reads=[pk], writes=[kst])
            if own:
                pq = PK[(t + 1) % 2]
                pqb = pq[:].bitcast(BF16)
                for gi in range(8):
                    src = stb["qd"][si] if gi < 4 else stb["qf"][si]
                    g4 = gi % 4
                    fw.op(pe, lambda gi=gi, src=src, g4=g4: PE.transpose(out=pqb[:, gi * 128:(gi + 1) * 128], in_=src[:, g4 * 128:(g4 + 1) * 128], identity=identb[:]),
                          reads=[src, identb], writes=[pq], inc=(gi == 7))
                q0 = so * 512 + j * 128
                pq3 = pqb[:, 0:1024].rearrange("p (g n) -> p g n", n=128)
                fw.op(dve, lambda: V.tensor_copy(out=qT[:, :, q0:q0 + 128], in_=pq3), reads=[pq], writes=[qT])
            va = vaug[t % 2]
            fw.dma(sp, v_scr[:, :, t, :].rearrange("g p c -> p g c"), va[:].rearrange("p (g a) c -> p g (a c)", a=2), reads=[va], writes=[v_b[t]], sembuf=va)
            if j == 3:
                fw.dma(sp, kT_scr[:, :, ch * 512:(ch + 1) * 512].rearrange("g p n -> p g n"), kst[:], reads=[kst], writes=[kT_b[ch]], sembuf=kst)

        x_load(0)
        x_load(1)
        P1(0)
        for t in range(NT):
            if t + 2 < NT:
                x_load(t + 2)
            if t + 1 < NT:
                P1(t + 1)
            P2(t)
            if t >= 1:
                P3(t - 1)
        P3(NT - 1)

        fw.pop(S1)
        S2 = fw.push()
        tposc = fw.sb("tposc", [NT, 1], F32)
        tposr = fw.sb("tposr", [NT, 1, NT], F32)
        Bm = fw.sb("Bm", [NT, NT], F32)
        Tt = fw.sb("Tt", [NT, 8], F32)
        Rm = fw.sb("Rm", [NT, NT, 8], F32)
        cc_ = fw.sb("cc", [128, NT, 8], F32)
        rbc = fw.sb("rbc", [128, 4, 8], F32)
        qfirst = fw.sb("qfirst_sb", [128, 1, 4], F32)
        visb = fw.sb("visb", [128, 4, 12], F32)
        bfox = fw.sb("bfox", [128, 4, NT, 8], F32)
        sel0 = fw.sb("sel0", [128, 128], F32)
        ld(tposc, tpos_in[:, :])
        fw.dma(sp, tposr[:], kpos[0:1, :].partition_broadcast(NT), writes=[tposr])
        fw.op(dve, lambda: V.tensor_scalar(out=Bm[:], in0=tposr[:, 0, :], scalar1=tposc[:, 0:1], scalar2=None, op0=ALU.is_gt), reads=[tposr, tposc], writes=[Bm])
        for h in range(8):
            fw.op(pe, lambda h=h: PE.matmul(PS[0][0:NT, h:h + 1], lhsT=logf[:, :, h], rhs=ones32[:, 0:1], start=True, stop=True), reads=[logf, ones32], writes=[PS[0]], inc=(h == 7))
        fw.op(dve, lambda: V.tensor_copy(out=Tt[:], in_=PS[0][0:NT, 0:8]), reads=[PS[0]], writes=[Tt])
        fw.op(dve, lambda: V.tensor_tensor(out=Rm[:], in0=Bm[:].unsqueeze(2).broadcast_to([NT, NT, 8]), in1=Tt[:].unsqueeze(1).broadcast_to([NT, NT, 8]), op=ALU.mult),
              reads=[Bm, Tt], writes=[Rm])
        fw.op(pe, lambda: PE.matmul(PS[1][:, :], lhsT=Umat[:], rhs=logf[:].rearrange("p t h -> p (t h)"), start=True, stop=False), reads=[Umat, logf], writes=[PS[1]], inc=False)
        fw.op(pe, lambda: PE.matmul(PS[1][:, :], lhsT=ones32[0:NT, :], rhs=Rm[:].rearrange("p t h -> p (t h)"), start=False, stop=True), reads=[ones32, Rm], writes=[PS[1]])
        fw.op(dve, lambda: V.tensor_copy(out=cc_[:].rearrange("p t h -> p (t h)"), in_=PS[1][:, :]), reads=[PS[1]], writes=[cc_])
        fw.op(dve, lambda: V.tensor_copy(out=sel0[:], in_=mk[:, 2:3].broadcast_to([128, 128])), reads=[mk], writes=[sel0])
        for s in range(4):
            fw.op(pe, lambda s=s: PE.matmul(PS[2][:, s * 8:(s + 1) * 8], lhsT=sel0[:], rhs=cc_[:, 16 * s + 2, :], start=True, stop=True), reads=[sel0, cc_], writes=[PS[2]], inc=(s == 3))
        fw.op(dve, lambda: V.tensor_copy(out=rbc[:].rearrange("p s h -> p (s h)"), in_=PS[2][:, 0:32]), reads=[PS[2]], writes=[rbc])
        fw.dma(sp, qfirst[:], qfirst_in[0:1, :].partition_broadcast(128), writes=[qfirst])
        for s in range(4):
            fw.op(dve, lambda s=s: V.tensor_scalar(out=visb[:, s, :], in0=kpos_sb[:, 16 * s + 4:16 * s + 16], scalar1=qfirst[:, 0, s:s + 1], scalar2=NEGBIG, op0=ALU.is_gt, op1=ALU.mult),
                  reads=[kpos_sb, qfirst], writes=[visb])
            nk = 16 * (s + 1)
            fw.op(dve, lambda s=s, nk=nk: V.tensor_tensor(out=bfox[:, s, 0:nk, :], in0=rbc[:, s, :].unsqueeze(1).broadcast_to([128, nk, 8]), in1=cc_[:, 0:nk, :], op=ALU.subtract),
                  reads=[rbc, cc_], writes=[bfox])
            fw.op(dve, lambda s=s: V.tensor_tensor(out=bfox[:, s, 16 * s + 4:16 * s + 16, :], in0=bfox[:, s, 16 * s + 4:16 * s + 16, :],
                                                   in1=visb[:, s, :].unsqueeze(2).broadcast_to([128, 12, 8]), op=ALU.add), reads=[bfox, visb], writes=[bfox])

        dmask = fw.sb("dmask", [128, 4, 512], BF16)
        fw.dma(pool, dmask[:].rearrange("p a b -> p (a b)"), c_dm[:, :], writes=[dmask])
        KTg = [fw.sb(f"KTg{i}", [128, SEQ], BF16) for i in range(2)]
        Vg = [fw.sb(f"Vg{i}", [128, NT, 130], BF16) for i in range(2)]
        PT = [fw.sb(f"PT{i}", [128, 512], BF16) for i in range(6)]
        qm = [fw.sb(f"qm{i}", [128, 2, 512], BF16) for i in range(2)]
        Ast = [fw.sb(f"Ast{i}", [128, 4, 128], BF16) for i in range(2)]
        od1 = [fw.sb(f"od1_{i}", [128, 4, 64], F32) for i in range(2)]
        od2 = fw.sb("od2", [128, 4, 64], F32)
        sqt = fw.sb("sqt", [128, 4, 64], F32)
        rl4 = [fw.sb(f"rl4_{i}", [128, 4], F32) for i in range(2)]
        ssq4 = fw.sb("ssq4", [128, 4], F32)
        gain_bc = fw.sb("gain_bc", [128, 1, 64], F32)
        fw.dma(sp, gain_bc[:], subln.rearrange("e o -> o e").partition_broadcast(128), writes=[gain_bc])
        fw.op(dve, lambda: V.tensor_scalar(out=gain_bc[:], in0=gain_bc[:], scalar1=1.0 - LAM_INIT, scalar2=None, op0=ALU.mult), reads=[gain_bc], writes=[gain_bc])
        attn_b = Buf("attn_scr")
        PSS = [PS[0], PS[1], PS[2], PS[3]]
        PSO = [PS[4], PS[5], PS[6], PS[7]]
        DSCALE = 32.0 ** -0.5
        FSCALE = 64.0 ** -0.5
        it_i = 0
        pair_i = 0
        qm_i = 0
        ast_i = 0

        def load_group(g):
            fw.dma(sp, KTg[g % 2][:], kT_scr[g, :, :], reads=kT_b, writes=[KTg[g % 2]])
            fw.dma(sp, Vg[g % 2][:].rearrange("p t c -> p (t c)"), v_scr[g, :, :, :].rearrange("p t c -> p (t c)"), reads=v_b, writes=[Vg[g % 2]])

        load_group(0)
        for g in range(8):
            if g + 1 < 8:
                load_group(g + 1)
            kt = KTg[g % 2]
            vg = Vg[g % 2]
            isdiff = g < 4
            for s in range(4):
                nkb = 16 * (s + 1)
                a_t = Ast[ast_i % 2]
                ast_i += 1
                if isdiff:
                    qmt = qm[qm_i % 2]
                    qm_i += 1
                    for m_ in range(2):
                        fw.op(dve, lambda m_=m_, qmt=qmt: V.tensor_scalar(out=qmt[:, m_, :], in0=qT[:, g, s * 512:(s + 1) * 512], scalar1=mk[:, m_:m_ + 1], scalar2=None, op0=ALU.mult),
                              reads=[qT, mk], writes=[qmt])
                for m in ((0, 1) if isdiff else (None,)):
                    pos_ = [PSO[(2 * pair_i) % 4], PSO[(2 * pair_i + 1) % 4]]
                    pair_i += 1
                    if isdiff:
                        qsl = [qmt[0:64, m, :], qmt[64:128, m, :]]
                        qsrc = qmt
                    else:
                        qsl = [qT[0:64, g, s * 512:(s + 1) * 512], qT[64:128, g, s * 512:(s + 1) * 512]]
                        qsrc = qT
                    pend = None

                    def pv(kb, pts, pos_=pos_):
                        for hh in range(2):
                            for i4 in range(4):
                                fw.op(pe, lambda hh=hh, i4=i4: PE.matmul(pos_[hh][:, i4 * 65:(i4 + 1) * 65], lhsT=pts[hh][:, i4 * 128:(i4 + 1) * 128], rhs=vg[:, kb, hh * 65:(hh + 1) * 65],
                                                                       start=(kb == 0 and i4 == 0), stop=(kb == nkb - 1 and i4 == 3)),
                                      reads=[vg, pts[hh]], writes=[pos_[hh]], inc=(i4 == 3))

                    for kb in range(nkb):
                        pss = [PSS[(2 * it_i) % 4], PSS[(2 * it_i + 1) % 4]]
                        pts = [PT[(2 * it_i) % 6], PT[(2 * it_i + 1) % 6]]
                        it_i += 1
                        for hh in range(2):
                            fw.op(pe, lambda hh=hh: PE.matmul(pss[hh][:, :], lhsT=kt[hh * 64:(hh + 1) * 64, kb * 128:(kb + 1) * 128], rhs=qsl[hh], start=True, stop=True),
                                  reads=[kt, qsrc], writes=[pss[hh]])
                        if pend is not None:
                            pv(*pend)
                        band = kb - 16 * s
                        for hh in range(2):
                            if isdiff:
                                if band >= 4:
                                    fw.op(act, lambda hh=hh: A.activation(out=pts[hh][:], in_=pss[hh][:, :], func=AF.Exp, scale=DSCALE, bias=visb[:, s, band - 4:band - 3]),
                                          reads=[pss[hh], visb], writes=[pts[hh]])
                                else:
                                    fw.op(act, lambda hh=hh: A.activation(out=pts[hh][:], in_=pss[hh][:, :], func=AF.Exp, scale=DSCALE), reads=[pss[hh]], writes=[pts[hh]])
                            else:
                                fh = 2 * (g - 4) + hh
                                fw.op(act, lambda hh=hh, fh=fh: A.activation(out=pts[hh][:], in_=pss[hh][:, :], func=AF.Exp, scale=FSCALE, bias=bfox[:, s, kb, fh:fh + 1]),
                                      reads=[pss[hh], bfox], writes=[pts[hh]])
                            if 0 <= band < 4:
                                fw.op(pool, lambda hh=hh: G.tensor_tensor(out=pts[hh][:], in0=pts[hh][:], in1=dmask[:, band, :], op=ALU.mult), reads=[pts[hh], dmask], writes=[pts[hh]])
                        pend = (kb, pts)
                    pv(*pend)
                    for hh in range(2):
                        o3 = pos_[hh][:, 0:260].rearrange("p (i c) -> p i c", c=65)
                        rlt = rl4[hh]
                        fw.op(dve, lambda o3=o3, rlt=rlt: V.reciprocal(out=rlt[:], in_=o3[:, :, 64]), reads=[pos_[hh]], writes=[rlt])
                        rb = rlt[:].unsqueeze(2).broadcast_to([128, 4, 64])
                        if not isdiff:
                            fw.op(dve, lambda o3=o3, rb=rb, hh=hh: V.tensor_tensor(out=a_t[:, :, hh * 64:(hh + 1) * 64], in0=o3[:, :, 0:64], in1=rb, op=ALU.mult), reads=[pos_[hh], rlt], writes=[a_t])
                        elif m == 0:
                            fw.op(dve, lambda o3=o3, rb=rb, hh=hh: V.tensor_tensor(out=od1[hh][:], in0=o3[:, :, 0:64], in1=rb, op=ALU.mult), reads=[pos_[hh], rlt], writes=[od1[hh]])
                        else:
                            fw.op(dve, lambda o3=o3, rb=rb: V.tensor_tensor(out=od2[:], in0=o3[:, :, 0:64], in1=rb, op=ALU.mult), reads=[pos_[hh], rlt], writes=[od2])
                            fw.op(dve, lambda hh=hh: V.scalar_tensor_tensor(out=od2[:].rearrange("p a b -> p (a b)"), in0=od2[:].rearrange("p a b -> p (a b)"), scalar=lamt[:, 0:1],
                                                                         in1=od1[hh][:].rearrange("p a b -> p (a b)"), op0=ALU.mult, op1=ALU.add), reads=[od2, od1[hh], lamt], writes=[od2])
                            fw.op(dve, lambda: V.tensor_tensor(out=sqt[:], in0=od2[:], in1=od2[:], op=ALU.mult), reads=[od2], writes=[sqt])
                            fw.op(dve, lambda: V.tensor_reduce(out=ssq4[:], in_=sqt[:], axis=AX.X, op=ALU.add), reads=[sqt], writes=[ssq4])
                            fw.op(act, lambda: A.activation(out=ssq4[:], in_=ssq4[:], func=AF.Ln, scale=1.0 / 64.0, bias=RMS_EPS), reads=[ssq4], writes=[ssq4])
                            fw.op(act, lambda: A.activation(out=ssq4[:], in_=ssq4[:], func=AF.Exp, scale=-0.5), reads=[ssq4], writes=[ssq4])
                            fw.op(dve, lambda: V.tensor_tensor(out=od2[:], in0=od2[:], in1=ssq4[:].unsqueeze(2).broadcast_to([128, 4, 64]), op=ALU.mult), reads=[od2, ssq4], writes=[od2])
                            fw.op(dve, lambda hh=hh: V.tensor_tensor(out=a_t[:, :, hh * 64:(hh + 1) * 64], in0=od2[:], in1=gain_bc[:].broadcast_to([128, 4, 64]), op=ALU.mult), reads=[od2, gain_bc], writes=[a_t])
                c0 = g * 128
                fw.dma(sp, attn_scr[s * 512:(s + 1) * 512, c0:c0 + 128].rearrange("(i p) c -> p i c", p=128), a_t[:], reads=[a_t], writes=[attn_b], sembuf=a_t)

        fw.pop(S2)
        fw.pop(S12)
        S345 = fw.push()
        NTOK = 2048 + 16
        hT = fw.sb("hT", [128, 8, NTOK], BF16)
        yacc = fw.sb("yacc", [128, 17, D], F32)
        gates = fw.sb("gates", [128, 17, NE + 1], F32)
        fw.op(dve, lambda: V.memset(gates[:], 1.0), writes=[gates])
        stats = fw.sb("stats", [128, 2, 6], F32)
        mv = fw.sb("mv", [128, 2], F32)
        rs = fw.sb("rs", [128, 1], F32)
        S3 = fw.push()
        wob = fw.sb("wob", [128, 8, D], BF16)
        fw.dma(pool, wob[:], w_o.rearrange("(k p) d -> p k d", p=128), writes=[wob])
        AT = [fw.sb(f"AT{i}", [128, 8, 128], BF16) for i in range(2)]
        g1 = fw.sb("g1", [128, 1, D], F32)
        b1 = fw.sb("b1", [128, 1, D], F32)
        fw.dma(sp, g1[:], ln1[0:1, :].partition_broadcast(128), writes=[g1])
        fw.dma(sp, b1[:], ln1[1:2, :].partition_broadcast(128), writes=[b1])
        wr32 = fw.sb("wr32", [128, 8, NE], F32)
        fw.dma(sp, wr32[:], w_router.rearrange("(k p) e -> p k e", p=128), writes=[wr32])
        rb_bc = fw.sb("rb_bc", [128, 1, NE], F32)
        fw.dma(sp, rb_bc[:], r_bias[0:1, :].partition_broadcast(128), writes=[rb_bc])
        att = [fw.sb(f"att{i}", [128, D], BF16) for i in range(2)]
        xres = [fw.sb(f"xres{i}", [128, D], F32) for i in range(2)]
        pre = fw.sb("pre", [128, D], F32)
        hT32 = fw.sb("hT32", [128, 8, 128], F32)
        sc = fw.sb("sc", [128, NE], F32)
        chs = fw.sb("chs", [128, NE], F32)
        ch2 = fw.sb("ch2", [128, NE], F32)
        eqm = fw.sb("eqm", [128, NE], F32)
        m1 = fw.sb("m1", [128, 8], F32)
        m2 = fw.sb("m2", [128, 8], F32)
        gs = fw.sb("gs", [128, 8], F32)
        top8 = fw.sb("top8", [128, 8], F32)
        gmask = fw.sb("gmask", [128, 8], F32)
        den = fw.sb("den", [128, 1], F32)

        def layer_norm(src, gam, bet, dst, n):
            for hf in range(2):
                fw.op(dve, lambda hf=hf: V.bn_stats(out=stats[0:n, hf, :], in_=src[0:n, hf * 512:(hf + 1) * 512]), reads=[src], writes=[stats])
            fw.op(dve, lambda: V.bn_aggr(out=mv[0:n, :], in_=stats[0:n, :, :].rearrange("p a b -> p (a b)")), reads=[stats], writes=[mv])
            fw.op(act, lambda: A.activation(out=rs[0:n, :], in_=mv[0:n, 1:2], func=AF.Sqrt, bias=LN_EPS), reads=[mv], writes=[rs])
            fw.op(dve, lambda: V.reciprocal(out=rs[0:n, :], in_=rs[0:n, :]), reads=[rs], writes=[rs])
            fw.op(dve, lambda: V.tensor_scalar(out=dst[0:n, :], in0=src[0:n, :], scalar1=mv[0:n, 0:1], scalar2=rs[0:n, 0:1], op0=ALU.subtract, op1=ALU.mult),
                  reads=[src, mv, rs], writes=[dst])
            fw.op(dve, lambda: V.tensor_tensor(out=dst[0:n, :], in0=dst[0:n, :], in1=gam[0:n, 0, :], op=ALU.mult), reads=[dst, gam], writes=[dst])
            fw.op(dve, lambda: V.tensor_tensor(out=dst[0:n, :], in0=dst[0:n, :], in1=bet[0:n, 0, :], op=ALU.add), reads=[dst, bet], writes=[dst])

        hbuf = fw.sb("hbuf", [128, D], F32)
        pre2 = [pre, fw.sb("pre_b", [128, D], F32)]

        def stageA(ti):
            n = 128 if ti < 16 else 16
            t0 = ti * 128
            pre = pre2[ti % 2]
            xr = xres[ti % 2]
            at_t = att[ti % 2]
            if ti < 16:
                fw.dma(sp, at_t[:], attn_scr[t0:t0 + 128, :], reads=[attn_b], writes=[at_t])
            else:
                fw.dma(pool, at_t[0:16, :], attn_s_in[:, :], writes=[at_t])
            att_T = AT[ti % 2]
            pab = PS[6 + ti % 2][:].bitcast(BF16)
            for kc in range(8):
                fw.op(pe, lambda kc=kc: PE.transpose(out=pab[:, kc * 128:kc * 128 + n], in_=at_t[0:n, kc * 128:(kc + 1) * 128], identity=identb[0:n, 0:n]),
                      reads=[at_t, identb], writes=[PS[6 + ti % 2]], inc=(kc == 7))
            fw.op(act, lambda: A.activation(out=att_T[:, :, 0:n], in_=pab[:, 0:1024].rearrange("p (a b) -> p a b", b=128)[:, :, 0:n], func=AF.Copy), reads=[PS[6 + ti % 2]], writes=[att_T])
            if ti < 16:
                so, j = divmod(ti, 4)
                gt = (so * 16 + j)
                fw.dma(sp, xr[:], x_perm[gt * 128:(gt + 1) * 128, :], writes=[xr])
            else:
                fw.dma(sp, xr[0:16, :], x_s[:, :], writes=[xr])
            for hf in range(2):
                py = PS[hf]
                for h in range(8):
                    fw.op(pe, lambda h=h, py=py, hf=hf: PE.matmul(py[0:n, :], lhsT=att_T[:, h, 0:n], rhs=wob[:, h, hf * 512:(hf + 1) * 512], start=(h == 0), stop=(h == 7)),
                          reads=[att_T, wob], writes=[py], inc=(h == 7))
                fw.op(dve, lambda py=py, hf=hf: V.scalar_tensor_tensor(out=pre[0:n, hf * 512:(hf + 1) * 512], in0=xr[0:n, hf * 512:(hf + 1) * 512], scalar=DEEP_ALPHA,
                                                                       in1=py[0:n, :], op0=ALU.mult, op1=ALU.add), reads=[xr, py], writes=[pre])
        def stageB(ti):
            n = 128 if ti < 16 else 16
            t0 = ti * 128
            pre = pre2[ti % 2]
            layer_norm(pre, g1, b1, hbuf, n)
            fw.op(act, lambda: A.mul(out=yacc[0:n, ti, :], in_=hbuf[0:n, :], mul=DEEP_ALPHA), reads=[hbuf], writes=[yacc])
            for kc in range(8):
                pt_ = PS[2 + kc // 4]
                fw.op(pe, lambda kc=kc, pt_=pt_: PE.transpose(out=pt_[:, (kc % 4) * 128:(kc % 4) * 128 + n], in_=hbuf[0:n, kc * 128:(kc + 1) * 128], identity=ident[0:n, 0:n]),
                      reads=[hbuf, ident], writes=[pt_], inc=(kc % 4 == 3))
            for hb in range(2):
                pt_ = PS[2 + hb]
                fw.op(act, lambda pt_=pt_, hb=hb: A.activation(out=hT32[:, hb * 4:hb * 4 + 4, 0:n], in_=pt_[:, :].rearrange("p (a b) -> p a b", b=128)[:, :, 0:n], func=AF.Copy),
                      reads=[pt_], writes=[hT32])
            fw.op(pool, lambda: G.tensor_copy(out=hT[:, :, t0:t0 + n], in_=hT32[:, :, 0:n]), reads=[hT32], writes=[hT])
            pr = PS[4]
            for kc in range(8):
                fw.op(pe, lambda kc=kc: PE.matmul(pr[0:n, 0:NE], lhsT=hT32[:, kc, 0:n], rhs=wr32[:, kc, :], start=(kc == 0), stop=(kc == 7)), reads=[hT32, wr32], writes=[pr], inc=(kc == 7))
            fw.op(act, lambda: A.activation(out=sc[0:n, :], in_=pr[0:n, 0:NE], func=AF.Sigmoid), reads=[pr], writes=[sc])
            fw.op(dve, lambda: V.tensor_tensor(out=chs[0:n, :], in0=sc[0:n, :], in1=rb_bc[0:n, 0, :], op=ALU.add), reads=[sc, rb_bc], writes=[chs])
            c3 = chs[0:n, :].rearrange("p (g k) -> p g k", k=8)
            fw.op(dve, lambda: V.tensor_reduce(out=m1[0:n, :], in_=c3, axis=AX.X, op=ALU.max), reads=[chs], writes=[m1])
            fw.op(dve, lambda: V.tensor_tensor(out=eqm[0:n, :].rearrange("p (g k) -> p g k", k=8), in0=c3, in1=m1[0:n, :].unsqueeze(2).broadcast_to([n, 8, 8]), op=ALU.is_ge), reads=[chs, m1], writes=[eqm])
            fw.op(dve, lambda: V.scalar_tensor_tensor(out=ch2[0:n, :], in0=eqm[0:n, :], scalar=-1e30, in1=chs[0:n, :], op0=ALU.mult, op1=ALU.add), reads=[eqm, chs], writes=[ch2])
            fw.op(dve, lambda: V.tensor_reduce(out=m2[0:n, :], in_=ch2[0:n, :].rearrange("p (g k) -> p g k", k=8), axis=AX.X, op=ALU.max), reads=[ch2], writes=[m2])
            fw.op(dve, lambda: V.tensor_tensor(out=gs[0:n, :], in0=m1[0:n, :], in1=m2[0:n, :], op=ALU.add), reads=[m1, m2], writes=[gs])
            fw.op(dve, lambda: V.max(out=top8[0:n, :], in_=gs[0:n, :]), reads=[gs], writes=[top8])
            fw.op(dve, lambda: V.tensor_scalar(out=gmask[0:n, :], in0=gs[0:n, :], scalar1=top8[0:n, 3:4], scalar2=None, op0=ALU.is_ge), reads=[gs, top8], writes=[gmask])
            fw.op(dve, lambda: V.tensor_tensor(out=ch2[0:n, :].rearrange("p (g k) -> p g k", k=8), in0=c3, in1=gmask[0:n, :].unsqueeze(2).broadcast_to([n, 8, 8]), op=ALU.mult), reads=[chs, gmask], writes=[ch2])
            fw.op(dve, lambda: V.tensor_scalar(out=eqm[0:n, 0:8], in0=gmask[0:n, :], scalar1=-1.0, scalar2=1e30, op0=ALU.add, op1=ALU.mult), reads=[gmask], writes=[eqm])
            fw.op(dve, lambda: V.tensor_tensor(out=ch2[0:n, :].rearrange("p (g k) -> p g k", k=8), in0=ch2[0:n, :].rearrange("p (g k) -> p g k", k=8),
                                               in1=eqm[0:n, 0:8].unsqueeze(2).broadcast_to([n, 8, 8]), op=ALU.add), reads=[ch2, eqm], writes=[ch2])
            fw.op(dve, lambda: V.max(out=top8[0:n, :], in_=ch2[0:n, :]), reads=[ch2], writes=[top8])
            fw.op(dve, lambda: V.tensor_scalar(out=eqm[0:n, :], in0=ch2[0:n, :], scalar1=top8[0:n, 7:8], scalar2=None, op0=ALU.is_ge), reads=[ch2, top8], writes=[eqm])
            fw.op(dve, lambda: V.tensor_tensor(out=ch2[0:n, :], in0=eqm[0:n, :], in1=sc[0:n, :], op=ALU.mult), reads=[eqm, sc], writes=[ch2])
            fw.op(dve, lambda: V.tensor_reduce(out=den[0:n, :], in_=ch2[0:n, :], axis=AX.X, op=ALU.add), reads=[ch2], writes=[den])
            fw.op(dve, lambda: V.tensor_scalar(out=den[0:n, :], in0=den[0:n, :], scalar1=1e-20, scalar2=None, op0=ALU.add), reads=[den], writes=[den])
            fw.op(dve, lambda: V.reciprocal(out=den[0:n, :], in_=den[0:n, :]), reads=[den], writes=[den])
            fw.op(dve, lambda: V.tensor_scalar(out=gates[0:n, ti, 0:NE], in0=ch2[0:n, :], scalar1=den[0:n, 0:1], scalar2=2.5, op0=ALU.mult, op1=ALU.mult), reads=[ch2, den], writes=[gates])

        stageA(0)
        for ti in range(17):
            if ti + 1 < 17:
                stageA(ti + 1)
            stageB(ti)
        fw.pop(S3)
        S4 = fw.push()
        wgb = [fw.sb(f"wgb{i}", [128, 8, 256], BF16) for i in range(2)]
        wub = [fw.sb(f"wub{i}", [128, 8, 256], BF16) for i in range(2)]
        wdb = [fw.sb(f"wdb{i}", [128, 2, D], BF16) for i in range(2)]
        sa = [fw.sb(f"sa{i}", [128, 512], F32) for i in range(2)]
        actb = [fw.sb(f"actb{i}", [128, 2, 512], BF16) for i in range(2)]
        PA = [PS[0], PS[1]]
        PU = [PS[2], PS[3]]
        PY = [[PS[4], PS[5]], [PS[6], PS[7]]]

        wg32 = [fw.sb(f"wg32_{i}", [128, 8, 256], F32) for i in range(2)]
        wu32 = [fw.sb(f"wu32_{i}", [128, 8, 256], F32) for i in range(2)]
        wd32 = [fw.sb(f"wd32_{i}", [128, 2, D], F32) for i in range(2)]

        def load_expert(e):
            i = e % 2
            fw.dma(sp, wg32[i][:], w_g[e].rearrange("(k p) f -> p k f", p=128), writes=[wg32[i]])
            fw.dma(sp, wu32[i][:], w_u[e].rearrange("(k p) f -> p k f", p=128), writes=[wu32[i]])
            fw.dma(sp, wd32[i][:], w_d[e].rearrange("(k p) d -> p k d", p=128), writes=[wd32[i]])

        def cast_expert(e):
            i = e % 2
            fw.op(pool, lambda: G.tensor_copy(out=wgb[i][:].rearrange("p a b -> p (a b)"), in_=wg32[i][:].rearrange("p a b -> p (a b)")), reads=[wg32[i]], writes=[wgb[i]])
            fw.op(act, lambda: A.activation(out=wub[i][:].rearrange("p a b -> p (a b)"), in_=wu32[i][:].rearrange("p a b -> p (a b)"), func=AF.Copy), reads=[wu32[i]], writes=[wub[i]])
            fw.op(act, lambda: A.activation(out=wdb[i][:].rearrange("p a b -> p (a b)"), in_=wd32[i][:].rearrange("p a b -> p (a b)"), func=AF.Copy), reads=[wd32[i]], writes=[wdb[i]])

        load_expert(0)
        cast_expert(0)
        load_expert(1)
        chunks = [(c * 512, 512) for c in range(4)] + [(2048, 16)]
        cnt = {"au": 0, "y": 0, "ck": 0}

        def gate_up(e, c0, cn):
            i = e % 2
            ab = actb[cnt["ck"] % 2]
            cnt["ck"] += 1
            for fc in range(2):
                pa = PA[cnt["au"] % 2]
                pu = PU[cnt["au"] % 2]
                sat = sa[cnt["au"] % 2]
                cnt["au"] += 1
                for kc in range(8):
                    fw.op(pe, lambda kc=kc: PE.matmul(pa[:, 0:cn], lhsT=wgb[i][:, kc, fc * 128:(fc + 1) * 128], rhs=hT[:, kc, c0:c0 + cn], start=(kc == 0), stop=(kc == 7)),
                          reads=[wgb[i], hT], writes=[pa], inc=(kc == 7))
                for kc in range(8):
                    fw.op(pe, lambda kc=kc: PE.matmul(pu[:, 0:cn], lhsT=wub[i][:, kc, fc * 128:(fc + 1) * 128], rhs=hT[:, kc, c0:c0 + cn], start=(kc == 0), stop=(kc == 7)),
                          reads=[wub[i], hT], writes=[pu], inc=(kc == 7))
                fw.op(act, lambda: A.activation(out=sat[:, 0:cn], in_=pa[:, 0:cn], func=AF.Silu), reads=[pa], writes=[sat])
                fw.op(dve, lambda: V.tensor_tensor(out=ab[:, fc, 0:cn], in0=sat[:, 0:cn], in1=pu[:, 0:cn], op=ALU.mult), reads=[sat, pu], writes=[ab])
            return ab

        def down(e, c0, cn, ab):
            i = e % 2
            nsub = max(1, cn // 128)
            for sb_ in range(nsub):
                n = min(128, cn)
                ti = c0 // 128 + sb_
                py = PY[cnt["y"] % 2]
                cnt["y"] += 1
                for hf in range(2):
                    for fc in range(2):
                        fw.op(pe, lambda fc=fc: PE.matmul(py[hf][0:n, :], lhsT=ab[:, fc, sb_ * 128:sb_ * 128 + n], rhs=wdb[i][:, fc, hf * 512:(hf + 1) * 512], start=(fc == 0), stop=(fc == 1)),
                              reads=[ab, wdb[i]], writes=[py[hf]], inc=(fc == 1))
                    fw.op(dve, lambda: V.scalar_tensor_tensor(out=yacc[0:n, ti, hf * 512:(hf + 1) * 512], in0=py[hf][0:n, :], scalar=gates[0:n, ti, e:e + 1],
                                                              in1=yacc[0:n, ti, hf * 512:(hf + 1) * 512], op0=ALU.mult, op1=ALU.add),
                          reads=[py[hf], gates, yacc], writes=[yacc])

        prev = None
        for e in range(NE + 1):
            for (c0, cn) in chunks:
                ab = gate_up(e, c0, cn)
                if prev is not None:
                    down(*prev)
                prev = (e, c0, cn, ab)
                if c0 == 0 and e + 1 <= NE:
                    cast_expert(e + 1)
                    if e + 2 <= NE:
                        load_expert(e + 2)
        down(*prev)


        fw.pop(S4)
        S5 = fw.push()
        g2 = fw.sb("g2", [128, 1, D], F32)
        b2 = fw.sb("b2", [128, 1, D], F32)
        fw.dma(sp, g2[:], ln2[0:1, :].partition_broadcast(128), writes=[g2])
        fw.dma(sp, b2[:], ln2[1:2, :].partition_broadcast(128), writes=[b2])
        yo = [fw.sb(f"yo{i}", [128, D], F32) for i in range(2)]
        ysrc = [fw.sb(f"ysrc{i}", [128, D], F32) for i in range(2)]
        for ti in range(17):
            n = 128 if ti < 16 else 16
            o_t = yo[ti % 2]
            s_t = ysrc[ti % 2]
            fw.op(act, lambda s_t=s_t, ti=ti, n=n: A.activation(out=s_t[0:n, :], in_=yacc[0:n, ti, :], func=AF.Copy), reads=[yacc], writes=[s_t])
            layer_norm(s_t, g2, b2, o_t, n)
            if ti < 16:
                fw.dma(sp, y_own[ti * 128:(ti + 1) * 128, :], o_t[:], reads=[o_t], writes=[Buf("o")], sembuf=o_t, final=True)
            else:
                fw.dma(sp, y_s[:, :], o_t[0:16, :], reads=[o_t], writes=[Buf("o")], sembuf=o_t, final=True)
        fw.finish()
        fw.pop(S5)
        fw.pop(S345)
        print("main program: instructions", fw.ninst, "semaphores", fw.nsem)
    return nc


NPOOL = 2560
_NC_CACHE = {}


def host_consts_A():
    p = np.arange(128)
    SU = (p[:, None] > p[None, :]).astype(np.float32)
    j = np.arange(64)
    SUP = (j[:, None] > j[None, :]).astype(np.float32)
    rm = np.zeros((128, 4), np.float32)
    rm[:, 3] = p
    rm[:, 0] = (p < 32)
    rm[:, 1] = (p >= 32) & (p < 64)
    rm[:, 2] = (p >= 64)
    mN = np.zeros((128, 32, 12), np.float32)
    for sq in range(32):
        for i in range(4):
            for ip in range(i + 1):
                mN[sq * 4 + ip, sq, [i, 4 + i, 8 + i]] = 1.0
    E = np.zeros((3, 12, 32, 128), np.float32)
    for sq in range(32):
        for i in range(4):
            E[0, i, sq, sq * 4 + i] = 1.0
            E[1, 4 + i, sq, sq * 4 + i] = 1.0
            E[2, 8 + i, sq, sq * 4 + i] = 1.0
    t = np.arange(128)
    PN = ((t[:, None] // 4 == t[None, :] // 4) & (t[:, None] <= t[None, :])).astype(np.float32)
    return SU, SUP, rm, mN.reshape(128, 384), E.reshape(3, 12, 4096), PN


def build_sample():
    nc = bass.Bass("TRN2", target_bir_lowering=False)

    def din(name, shape, dt=F32):
        return nc.dram_tensor(name, list(shape), dt, kind="ExternalInput").ap()

    def dout(name, shape, dt=F32):
        return nc.dram_tensor(name, list(shape), dt, kind="ExternalOutput").ap()

    xs = din("xs", [128, D])
    w_s = din("w_s", [D, 385])
    bf1 = din("bf1", [1, 1])
    lam4 = din("lam4", [4, 32])
    subln = din("subln", [1, 64])
    spos = din("spos", [128, 1])
    ptb_in = din("ptb", [1, 2048], I32)
    ptP_in = din("ptP", [128, 16], I32)
    kv_pool = din("kv_pool", [NPOOL * 128, 256])
    lf_pool = din("lf_pool", [NPOOL, 128])
    c_ident = din("c_ident", [128, 128])
    c_SU = din("c_SU", [128, 128])
    c_SUP = din("c_SUP", [64, 64])
    c_rm = din("c_rm", [128, 4])
    c_mN = din("c_mN", [128, 384])
    c_E = din("c_E", [3, 12, 4096])
    c_PN = din("c_PN", [128, 128])
    c_invf = din("c_invf", [128, 4])

    attn_c = dout("attn_c", [128, 128])
    nkd = dout("nkd", [128, 64])
    nvd = dout("nvd", [128, 64])
    nkf = dout("nkf", [128, 64])
    nvf = dout("nvf", [128, 64])
    nlf = dout("nlf", [128, 1])

    with ExitStack() as st:
        fw = FW(nc, st)
        pe, act, dve, pool, sp = fw.pe, fw.act, fw.dve, fw.pool, fw.sp
        V = nc.vector
        A = nc.scalar
        G = nc.gpsimd
        PE = nc.tensor
        PS = [fw.ps(f"ps{i}", [128, 512], F32) for i in range(8)]
        ld = lambda t, src, q=sp: fw.dma(q, t[:], src, writes=[t])

        ident = fw.sb("ident", [128, 128], F32)
        identb = fw.sb("identb", [128, 128], BF16)
        SU = fw.sb("SU", [128, 128], F32)
        SUP = fw.sb("SUP", [64, 64], F32)
        rm = fw.sb("rm", [128, 4], F32)
        mN = fw.sb("mN", [128, 32, 12], F32)
        PN = fw.sb("PN", [128, 128], F32)
        invf = fw.sb("invf", [128, 4], F32)
        ones32 = fw.sb("ones32", [128, 128], F32)
        ld(ident, c_ident[:, :])
        ld(SU, c_SU[:, :])
        ld(SUP, c_SUP[:, :])
        ld(rm, c_rm[:, :])
        ld(PN, c_PN[:, :])
        ld(invf, c_invf[:, :])
        fw.dma(sp, mN[:].rearrange("p a b -> p (a b)"), c_mN[:, :], writes=[mN])
        fw.op(dve, lambda: V.tensor_copy(out=identb[:], in_=ident[:]), reads=[ident], writes=[identb])
        fw.op(dve, lambda: V.memset(ones32[:], 1.0), writes=[ones32])

        lamv = fw.sb("lamv", [128, 4, 32], F32)
        fw.dma(sp, lamv[:].rearrange("p a b -> p (a b)"), lam4.rearrange("(o a) b -> o (a b)", o=1).partition_broadcast(128).squeeze(1), writes=[lamv])
        lprod = fw.sb("lprod", [128, 2, 32], F32)
        lsum = fw.sb("lsum", [128, 2], F32)
        lexp = fw.sb("lexp", [128, 2], F32)
        lamt = fw.sb("lamt", [128, 1], F32)
        fw.op(dve, lambda: V.tensor_tensor(out=lprod[:, 0, :], in0=lamv[:, 0, :], in1=lamv[:, 1, :], op=ALU.mult), reads=[lamv], writes=[lprod])
        fw.op(dve, lambda: V.tensor_tensor(out=lprod[:, 1, :], in0=lamv[:, 2, :], in1=lamv[:, 3, :], op=ALU.mult), reads=[lamv], writes=[lprod])
        fw.op(dve, lambda: V.tensor_reduce(out=lsum[:], in_=lprod[:], axis=AX.X, op=ALU.add), reads=[lprod], writes=[lsum])
        fw.op(act, lambda: A.activation(out=lexp[:], in_=lsum[:], func=AF.Exp), reads=[lsum], writes=[lexp])
        fw.op(dve, lambda: V.tensor_tensor(out=lamt[:], in0=lexp[:, 1:2], in1=lexp[:, 0:1], op=ALU.subtract), reads=[lexp], writes=[lamt])
        fw.op(dve, lambda: V.tensor_scalar(out=lamt[:], in0=lamt[:], scalar1=-LAM_INIT, scalar2=None, op0=ALU.add), reads=[lamt], writes=[lamt])
        E0 = fw.sb("E0", [12, 4096], F32)
        E1 = fw.sb("E1", [12, 4096], F32)
        SelF = fw.sb("SelF", [12, 4096], F32)
        SelD = fw.sb("SelD", [12, 4096], F32)
        ld(E0, c_E[0, :, :])
        ld(E1, c_E[1, :, :])
        ld(SelF, c_E[2, :, :])
        fw.op(dve, lambda: V.scalar_tensor_tensor(out=SelD[:], in0=E1[:], scalar=lamt[0:12, 0:1], in1=E0[:], op0=ALU.mult, op1=ALU.add), reads=[E1, E0, lamt], writes=[SelD])

        xsb = fw.sb("xsb", [128, D], BF16)
        xsT = fw.sb("xsT", [128, 8, 128], BF16)
        wsb = fw.sb("wsb", [128, 8, 385], BF16)
        fw.dma(pool, xsb[:], xs[:, :], writes=[xsb])
        fw.dma(pool, wsb[:], w_s.rearrange("(k p) c -> p k c", p=128), writes=[wsb])
        pxb = PS[0][:].bitcast(BF16)
        for kc in range(8):
            fw.op(pe, lambda kc=kc: PE.transpose(out=pxb[:, kc * 128:(kc + 1) * 128], in_=xsb[:, kc * 128:(kc + 1) * 128], identity=identb[:]), reads=[xsb, identb], writes=[PS[0]], inc=(kc == 7))
        fw.op(dve, lambda: V.tensor_copy(out=xsT[:].rearrange("p a b -> p (a b)"), in_=pxb[:, 0:1024]), reads=[PS[0]], writes=[xsT])
        for kc in range(8):
            fw.op(pe, lambda kc=kc: PE.matmul(PS[1][:, 0:385], lhsT=xsT[:, kc, :], rhs=wsb[:, kc, :], start=(kc == 0), stop=(kc == 7)), reads=[xsT, wsb], writes=[PS[1]], inc=(kc == 7))
        z = fw.sb("z", [128, 385], F32)
        fw.op(act, lambda: A.activation(out=z[:], in_=PS[1][:, 0:385], func=AF.Copy), reads=[PS[1]], writes=[z])
        pos = fw.sb("pos", [128, 1], F32)
        ld(pos, spos[:, :])
        ang = fw.sb("ang", [128, 4], F32)
        angk = fw.sb("angk", [128, 4], F32)
        angi = fw.sb("angi", [128, 4], I32)
        angr = fw.sb("angr", [128, 4], F32)
        angc = fw.sb("angc", [128, 4], F32)
        cosS = fw.sb("cosS", [128, 4], F32)
        sinS = fw.sb("sinS", [128, 4], F32)
        TWO_PI = 2.0 * math.pi
        fw.op(dve, lambda: V.tensor_scalar(out=ang[:], in0=invf[:], scalar1=pos[:, 0:1], scalar2=None, op0=ALU.mult), reads=[invf, pos], writes=[ang])

        def reduce_sin(dst, shift):
            fw.op(dve, lambda: V.tensor_scalar(out=angk[:], in0=ang[:], scalar1=shift, scalar2=1.0 / TWO_PI, op0=ALU.add, op1=ALU.mult), reads=[ang], writes=[angk])
            fw.op(dve, lambda: V.tensor_copy(out=angi[:], in_=angk[:]), reads=[angk], writes=[angi])
            fw.op(dve, lambda: V.tensor_copy(out=angk[:], in_=angi[:]), reads=[angi], writes=[angk])
            fw.op(dve, lambda: V.tensor_scalar(out=angr[:], in0=ang[:], scalar1=shift, scalar2=None, op0=ALU.add), reads=[ang], writes=[angr])
            fw.op(dve, lambda: V.scalar_tensor_tensor(out=angr[:], in0=angk[:], scalar=-TWO_PI, in1=angr[:], op0=ALU.mult, op1=ALU.add), reads=[angk, angr], writes=[angr])
            fw.op(dve, lambda: V.tensor_scalar(out=angc[:], in0=angr[:], scalar1=math.pi, scalar2=-TWO_PI, op0=ALU.is_gt, op1=ALU.mult), reads=[angr], writes=[angc])
            fw.op(dve, lambda: V.tensor_tensor(out=angr[:], in0=angr[:], in1=angc[:], op=ALU.add), reads=[angr, angc], writes=[angr])
            fw.op(dve, lambda: V.tensor_scalar(out=angc[:], in0=angr[:], scalar1=-math.pi, scalar2=TWO_PI, op0=ALU.is_lt, op1=ALU.mult), reads=[angr], writes=[angc])
            fw.op(dve, lambda: V.tensor_tensor(out=angr[:], in0=angr[:], in1=angc[:], op=ALU.add), reads=[angr, angc], writes=[angr])
            fw.op(dve, lambda: V.tensor_scalar(out=angr[:], in0=angr[:], scalar1=-3.14159, scalar2=3.14159, op0=ALU.max, op1=ALU.min), reads=[angr], writes=[angr])
            fw.op(act, lambda: A.activation(out=dst[:], in_=angr[:], func=AF.Sin), reads=[angr], writes=[dst])

        reduce_sin(sinS, 0.0)
        reduce_sin(cosS, math.pi / 2.0)
        rt = [fw.sb(f"rt{i}", [128, 8], F32) for i in range(4)]

        def rope64(c0):
            v3 = z[:, c0:c0 + 64].rearrange("p (g d) -> p g d", d=32)
            x1 = v3[:, :, 0:4]
            x2 = v3[:, :, 4:8]
            cb = cosS[:].unsqueeze(1).broadcast_to([128, 2, 4])
            sbb = sinS[:].unsqueeze(1).broadcast_to([128, 2, 4])
            r = [x[:].rearrange("p (g d) -> p g d", d=4) for x in rt]
            fw.op(dve, lambda: V.tensor_tensor(out=r[0], in0=x1, in1=cb, op=ALU.mult), reads=[z, cosS], writes=[rt[0]])
            fw.op(dve, lambda: V.tensor_tensor(out=r[1], in0=x2, in1=sbb, op=ALU.mult), reads=[z, sinS], writes=[rt[1]])
            fw.op(dve, lambda: V.tensor_tensor(out=r[2], in0=x2, in1=cb, op=ALU.mult), reads=[z, cosS], writes=[rt[2]])
            fw.op(dve, lambda: V.tensor_tensor(out=r[3], in0=x1, in1=sbb, op=ALU.mult), reads=[z, sinS], writes=[rt[3]])
            fw.op(dve, lambda: V.tensor_tensor(out=x1, in0=r[0], in1=r[1], op=ALU.subtract), reads=[rt[0], rt[1]], writes=[z])
            fw.op(dve, lambda: V.tensor_tensor(out=x2, in0=r[2], in1=r[3], op=ALU.add), reads=[rt[2], rt[3]], writes=[z])

        rope64(0)
        rope64(64)
        bfc = fw.sb("bfc", [128, 1, 1], F32)
        fw.dma(sp, bfc[:], bf1[0:1, :].partition_broadcast(128), writes=[bfc])
        slf = fw.sb("slf", [128, 1], F32)
        e1 = fw.sb("e1", [128, 1], F32)
        fw.op(dve, lambda: V.tensor_tensor(out=slf[:], in0=z[:, 384:385], in1=bfc[:, 0, :], op=ALU.add), reads=[z, bfc], writes=[slf])
        fw.op(act, lambda: A.activation(out=e1[:], in_=slf[:], func=AF.Exp, scale=-1.0), reads=[slf], writes=[e1])
        fw.op(act, lambda: A.activation(out=slf[:], in_=e1[:], func=AF.Ln, bias=1.0), reads=[e1], writes=[slf])
        fw.op(dve, lambda: V.tensor_scalar(out=slf[:], in0=slf[:], scalar1=-1.0, scalar2=None, op0=ALU.mult), reads=[slf], writes=[slf])
        fw.dma(sp, nkd[:, :], z[:, 64:128], reads=[z], writes=[Buf("o")], sembuf=z, final=True)
        fw.dma(sp, nvd[:, :], z[:, 128:192], reads=[z], writes=[Buf("o")], sembuf=z, final=True)
        fw.dma(sp, nkf[:, :], z[:, 256:320], reads=[z], writes=[Buf("o")], sembuf=z, final=True)
        fw.dma(sp, nvf[:, :], z[:, 320:384], reads=[z], writes=[Buf("o")], sembuf=z, final=True)
        fw.dma(sp, nlf[:, :], slf[:], reads=[slf], writes=[Buf("o")], sembuf=slf, final=True)

        qk = fw.sb("qk", [128, 2, 128], F32)
        fw.op(dve, lambda: V.tensor_scalar(out=qk[:, 0, 0:64], in0=z[:, 0:64], scalar1=32.0 ** -0.5, scalar2=None, op0=ALU.mult), reads=[z], writes=[qk])
        fw.op(dve, lambda: V.tensor_scalar(out=qk[:, 0, 64:128], in0=z[:, 192:256], scalar1=0.125, scalar2=None, op0=ALU.mult), reads=[z], writes=[qk])
        fw.op(dve, lambda: V.tensor_copy(out=qk[:, 1, 0:64], in_=z[:, 64:128]), reads=[z], writes=[qk])
        fw.op(dve, lambda: V.tensor_copy(out=qk[:, 1, 64:128], in_=z[:, 256:320]), reads=[z], writes=[qk])
        pqb = PS[2][:]
        for a in range(2):
            fw.op(pe, lambda a=a: PE.transpose(out=pqb[:, a * 128:(a + 1) * 128], in_=qk[:, a, :], identity=ident[:]), reads=[qk, ident], writes=[PS[2]], inc=(a == 1))
        Qblk = fw.sb("Qblk", [128, 32, 12], F32)
        KTn = fw.sb("KTn", [128, 128], F32)
        for jb in range(3):
            fw.op(dve, lambda jb=jb: V.tensor_scalar(out=Qblk[:, :, jb * 4:(jb + 1) * 4], in0=pqb[:, 0:128].rearrange("p (s i) -> p s i", i=4), scalar1=rm[:, jb:jb + 1], scalar2=None, op0=ALU.mult),
                  reads=[PS[2], rm], writes=[Qblk])
        fw.op(dve, lambda: V.tensor_copy(out=KTn[:], in_=pqb[:, 128:256]), reads=[PS[2]], writes=[KTn])
        Vn = fw.sb("Vn", [128, 129], F32)
        fw.op(dve, lambda: V.memset(Vn[:], 1.0), writes=[Vn])
        fw.op(dve, lambda: V.tensor_copy(out=Vn[:, 0:64], in_=z[:, 128:192]), reads=[z], writes=[Vn])
        fw.op(dve, lambda: V.tensor_copy(out=Vn[:, 64:128], in_=z[:, 320:384]), reads=[z], writes=[Vn])
        biasN = fw.sb("biasN", [128, 1], F32)
        fw.op(pe, lambda: PE.matmul(PS[3][:, 0:1], lhsT=PN[:], rhs=slf[:], start=True, stop=True), reads=[PN, slf], writes=[PS[3]])
        fw.op(dve, lambda: V.tensor_scalar(out=biasN[:], in0=PS[3][:, 0:1], scalar1=-1.0, scalar2=None, op0=ALU.mult), reads=[PS[3]], writes=[biasN])

        ptb = fw.sb("ptb_sb", [128, 1, 2048], I32)
        fw.dma(sp, ptb[:], ptb_in[0:1, :].partition_broadcast(128), writes=[ptb])
        idxf = fw.sb("idxf", [128, 2048], F32)
        idx = fw.sb("idx", [128, 2048], I32)
        fw.op(dve, lambda: V.tensor_copy(out=idxf[:], in_=ptb[:, 0, :]), reads=[ptb], writes=[idxf])
        fw.op(dve, lambda: V.tensor_scalar(out=idxf[:], in0=idxf[:], scalar1=128.0, scalar2=rm[:, 3:4], op0=ALU.mult, op1=ALU.add), reads=[idxf, rm], writes=[idxf])
        fw.op(dve, lambda: V.tensor_copy(out=idx[:], in_=idxf[:]), reads=[idxf], writes=[idx])
        ptP = fw.sb("ptP_sb", [128, 16], I32)
        ld(ptP, ptP_in[:, :])
        LT = [fw.sb(f"LT{i}", [128, 128], F32) for i in range(2)]
        Lall = fw.sb("Lall", [128, 32, 64], F32)
        for i in range(16):
            lt = LT[i % 2]
            fw.dma(pool, lt[:], lf_pool[:, :], reads=[ptP], writes=[lt], indirect=ptP[:, i:i + 1].bitcast(U32))
            pt_ = PS[4 + (i % 2)]
            fw.op(pe, lambda lt=lt, pt_=pt_: PE.transpose(out=pt_[:, 0:128], in_=lt[:], identity=ident[:]), reads=[lt, ident], writes=[pt_])
            fw.op(dve, lambda i=i, pt_=pt_: V.tensor_copy(out=Lall[:, 2 * i:2 * i + 2, :].rearrange("p a b -> p (a b)"), in_=pt_[:, 0:128]), reads=[pt_], writes=[Lall])
        Tcol = fw.sb("Tcol", [64, 32], F32)
        Rm = fw.sb("Rm", [64, 32, 64], F32)
        biasP = fw.sb("biasP", [128, 32, 64], F32)
        for sq in range(32):
            fw.op(pe, lambda sq=sq: PE.matmul(PS[6][0:64, sq:sq + 1], lhsT=Lall[:, sq, :], rhs=ones32[:, 0:1], start=True, stop=True), reads=[Lall, ones32], writes=[PS[6]], inc=(sq == 31))
        fw.op(dve, lambda: V.tensor_copy(out=Tcol[:], in_=PS[6][0:64, 0:32]), reads=[PS[6]], writes=[Tcol])
        fw.op(dve, lambda: V.tensor_tensor(out=Rm[:], in0=Tcol[:].unsqueeze(2).broadcast_to([64, 32, 64]), in1=SUP[:].unsqueeze(1).broadcast_to([64, 32, 64]), op=ALU.mult), reads=[Tcol, SUP], writes=[Rm])
        for q4 in range(4):
            pb = PS[q4 % 2]
            fw.op(pe, lambda q4=q4, pb=pb: PE.matmul(pb[:, :], lhsT=SU[:], rhs=Lall[:, q4 * 8:(q4 + 1) * 8, :].rearrange("p a b -> p (a b)"), start=True, stop=False), reads=[SU, Lall], writes=[pb], inc=False)
            fw.op(pe, lambda q4=q4, pb=pb: PE.matmul(pb[:, :], lhsT=ones32[0:64, :], rhs=Rm[:, q4 * 8:(q4 + 1) * 8, :].rearrange("p a b -> p (a b)"), start=False, stop=True), reads=[ones32, Rm], writes=[pb])
            fw.op(dve, lambda q4=q4, pb=pb: V.tensor_copy(out=biasP[:, q4 * 8:(q4 + 1) * 8, :].rearrange("p a b -> p (a b)"), in_=pb[:, :]), reads=[pb], writes=[biasP])

        NKV = 48
        kv = [fw.sb(f"kv{i}", [128, 257], F32) for i in range(NKV)]
        for t_ in kv:
            fw.op(dve, lambda t_=t_: V.memset(t_[:, 256:257], 1.0), writes=[t_])
        PTt = [fw.sb(f"PTt{i}", [128, 4, 12], F32) for i in range(2)]
        sx = [fw.sb(f"sx{i}", [128, 4, 4], F32) for i in range(2)]
        PTn = fw.sb("PTn", [128, 12], F32)
        sxn = fw.sb("sxn", [128, 4], F32)
        On = [fw.sb(f"On{i}", [12, 129], F32) for i in range(2)]
        rl = [fw.sb(f"rl{i}", [12, 1], F32) for i in range(2)]
        PSS = [PS[0], PS[1]]
        PSO = [PS[2], PS[3]]
        PFD = PS[4]
        PFF = PS[5]
        kv_i = 0
        grp_i = 0
        for sq in range(32):
            po = PSO[sq % 2]
            pend = []

            def flush(pend=pend, po=po):
                for (first, last, lhs, rhs_t, rd) in pend:
                    fw.op(pe, lambda: PE.matmul(po[0:12, 0:129], lhsT=lhs, rhs=rhs_t[:, 128:257] if rhs_t is not Vn else rhs_t[:, :], start=first, stop=last), reads=rd, writes=[po])
                del pend[:]

            for g4 in range(16):
                pss = PSS[grp_i % 2]
                ptt = PTt[grp_i % 2]
                sxt = sx[grp_i % 2]
                grp_i += 1
                tiles = []
                for jj in range(4):
                    j = g4 * 4 + jj
                    t_ = kv[kv_i % NKV]
                    kv_i += 1
                    fw.dma(pool, t_[:, 0:256], kv_pool[:, :], reads=[idx], writes=[t_], indirect=idx[:, sq * 64 + j:sq * 64 + j + 1].bitcast(U32))
                    fw.op(pe, lambda jj=jj, t_=t_, pss=pss: PE.matmul(pss[:, jj * 12:(jj + 1) * 12], lhsT=t_[:, 0:128], rhs=Qblk[:, sq, :], start=True, stop=True), reads=[t_, Qblk], writes=[pss], inc=(jj == 3))
                    tiles.append(t_)
                flush()
                p3 = pss[:, 0:48].rearrange("p (a b) -> p a b", b=12)
                fw.op(dve, lambda: V.tensor_tensor(out=sxt[:], in0=p3[:, :, 8:12], in1=biasP[:, sq, g4 * 4:g4 * 4 + 4].unsqueeze(2).broadcast_to([128, 4, 4]), op=ALU.add), reads=[pss, biasP], writes=[sxt])
                fw.op(act, lambda: A.activation(out=ptt[:, :, 0:8], in_=p3[:, :, 0:8], func=AF.Exp), reads=[pss], writes=[ptt])
                fw.op(act, lambda: A.activation(out=ptt[:, :, 8:12], in_=sxt[:], func=AF.Exp), reads=[sxt], writes=[ptt])
                for jj in range(4):
                    pend.append((g4 == 0 and jj == 0, False, ptt[:, jj, :], tiles[jj], [ptt, tiles[jj]]))
            pss = PSS[grp_i % 2]
            grp_i += 1
            fw.op(pe, lambda: PE.matmul(pss[:, 0:12], lhsT=KTn[:], rhs=Qblk[:, sq, :], start=True, stop=True), reads=[KTn, Qblk], writes=[pss])
            flush()
            fw.op(dve, lambda: V.tensor_scalar(out=sxn[:], in0=pss[:, 8:12], scalar1=biasN[:, 0:1], scalar2=None, op0=ALU.add), reads=[pss, biasN], writes=[sxn])
            fw.op(act, lambda: A.activation(out=PTn[:, 0:8], in_=pss[:, 0:8], func=AF.Exp), reads=[pss], writes=[PTn])
            fw.op(act, lambda: A.activation(out=PTn[:, 8:12], in_=sxn[:], func=AF.Exp), reads=[sxn], writes=[PTn])
            fw.op(dve, lambda: V.tensor_tensor(out=PTn[:], in0=PTn[:], in1=mN[:, sq, :], op=ALU.mult), reads=[PTn, mN], writes=[PTn])
            pend.append((False, True, PTn[:, :], Vn, [PTn, Vn]))
            flush()
            on = On[sq % 2]
            rlt = rl[sq % 2]
            fw.op(dve, lambda: V.reciprocal(out=rlt[:], in_=po[0:12, 128:129]), reads=[po], writes=[rlt])
            fw.op(dve, lambda: V.tensor_scalar(out=on[:], in0=po[0:12, 0:129], scalar1=rlt[:, 0:1], scalar2=None, op0=ALU.mult), reads=[po, rlt], writes=[on])
            fw.op(pe, lambda: PE.matmul(PFD[:, 0:64], lhsT=SelD[:, sq * 128:(sq + 1) * 128], rhs=on[:, 0:64], start=(sq == 0), stop=(sq == 31)), reads=[SelD, on], writes=[PFD])
            fw.op(pe, lambda: PE.matmul(PFF[:, 0:64], lhsT=SelF[:, sq * 128:(sq + 1) * 128], rhs=on[:, 64:128], start=(sq == 0), stop=(sq == 31)), reads=[SelF, on], writes=[PFF])

        res = fw.sb("res", [128, 128], F32)
        od = fw.sb("od", [128, 64], F32)
        sqv = fw.sb("sqv", [128, 64], F32)
        ssq = fw.sb("ssq", [128, 1], F32)
        gb = fw.sb("gb", [128, 1, 64], F32)
        fw.dma(sp, gb[:], subln[0:1, :].partition_broadcast(128), writes=[gb])
        fw.op(dve, lambda: V.tensor_copy(out=od[:], in_=PFD[:, 0:64]), reads=[PFD], writes=[od])
        fw.op(dve, lambda: V.tensor_tensor(out=sqv[:], in0=od[:], in1=od[:], op=ALU.mult), reads=[od], writes=[sqv])
        fw.op(dve, lambda: V.tensor_reduce(out=ssq[:], in_=sqv[:], axis=AX.X, op=ALU.add), reads=[sqv], writes=[ssq])
        fw.op(act, lambda: A.activation(out=ssq[:], in_=ssq[:], func=AF.Sqrt, scale=1.0 / 64.0, bias=RMS_EPS), reads=[ssq], writes=[ssq])
        fw.op(dve, lambda: V.reciprocal(out=ssq[:], in_=ssq[:]), reads=[ssq], writes=[ssq])
        fw.op(dve, lambda: V.tensor_scalar(out=od[:], in0=od[:], scalar1=ssq[:, 0:1], scalar2=1.0 - LAM_INIT, op0=ALU.mult, op1=ALU.mult), reads=[od, ssq], writes=[od])
        fw.op(dve, lambda: V.tensor_tensor(out=res[:, 0:64], in0=od[:], in1=gb[:, 0, :], op=ALU.mult), reads=[od, gb], writes=[res])
        fw.op(dve, lambda: V.tensor_copy(out=res[:, 64:128], in_=PFF[:, 0:64]), reads=[PFF], writes=[res])
        fw.dma(sp, attn_c[:, :], res[:], reads=[res], writes=[Buf("o")], sembuf=res, final=True)
        fw.finish()
        print("sample program: instructions", fw.ninst, "semaphores", fw.nsem)
    return nc


def run_sample(inputs):
    f32 = np.float32
    if "sample" not in _NC_CACHE:
        _NC_CACHE["sample"] = build_sample()
    nc = _NC_CACHE["sample"]
    ident, U, dm, mk, invf = host_consts()
    SU, SUP, rm, mN, E, PN = host_consts_A()
    xs = np.ascontiguousarray(np.asarray(inputs["x_sample"], f32).reshape(128, D))
    w_in = np.asarray(inputs["w_in"][0], f32)
    pt = np.asarray(inputs["page_table"]).astype(np.int32)
    lam4 = np.stack([inputs["lambda_q1"][0], inputs["lambda_k1"][0], inputs["lambda_q2"][0], inputs["lambda_k2"][0]]).astype(f32)
    spos = (PAST + (np.arange(128) % 4)).astype(f32).reshape(128, 1)
    ptb = np.ascontiguousarray(pt.reshape(1, 2048))
    ptP = np.ascontiguousarray(pt.reshape(16, 128).T)
    ck = np.asarray(inputs["cache_diff_k"][0], f32).reshape(NPOOL, 128, 8, 64)
    cv = np.asarray(inputs["cache_diff_v"][0], f32)
    fk = np.asarray(inputs["cache_fox_k"][0], f32)
    fv = np.asarray(inputs["cache_fox_v"][0], f32)
    fl = np.asarray(inputs["cache_fox_logf"][0], f32)
    shared = {"xs": xs, "lam4": lam4, "subln": np.asarray(inputs["subln_gain"], f32).reshape(1, 64), "spos": spos, "ptb": ptb, "ptP": ptP,
              "c_ident": ident, "c_SU": SU, "c_SUP": SUP, "c_rm": rm, "c_mN": mN, "c_E": E, "c_PN": PN, "c_invf": invf}
    in_maps = []
    for c in range(8):
        m = dict(shared)
        cols = np.concatenate([C_QD + c * 64 + np.arange(64), C_KD + c * 64 + np.arange(64), C_VD + c * 64 + np.arange(64),
                               C_QF + c * 64 + np.arange(64), C_KF + c * 64 + np.arange(64), C_VF + c * 64 + np.arange(64), [C_FL + c]])
        m["w_s"] = np.ascontiguousarray(w_in[:, cols])
        m["bf1"] = np.asarray(inputs["b_forget"], f32).reshape(8)[c].reshape(1, 1)
        kvp = np.empty((NPOOL, 128, 256), f32)
        kvp[:, 0:64, 0:128] = ck[:, :, c, :].transpose(0, 2, 1)
        kvp[:, 64:128, 0:128] = fk[:, :, c, :].transpose(0, 2, 1)
        kvp[:, :, 128:192] = cv[:, :, c, :]
        kvp[:, :, 192:256] = fv[:, :, c, :]
        m["kv_pool"] = kvp.reshape(NPOOL * 128, 256)
        m["lf_pool"] = np.ascontiguousarray(fl[:, :, c])
        in_maps.append(m)
    res = run_bass_kernel_spmd(nc, in_maps, core_ids=list(range(8)))
    attn = np.zeros((128, 16, 64), f32)
    nkd = np.zeros((128, 8, 64), f32)
    nvd = np.zeros((128, 8, 64), f32)
    nkf = np.zeros((128, 8, 64), f32)
    nvf = np.zeros((128, 8, 64), f32)
    nlf = np.zeros((128, 8), f32)
    for c in range(8):
        r = res.results[c]
        attn[:, c, :] = r["attn_c"][:, 0:64]
        attn[:, 8 + c, :] = r["attn_c"][:, 64:128]
        nkd[:, c] = r["nkd"]
        nvd[:, c] = r["nvd"]
        nkf[:, c] = r["nkf"]
        nvf[:, c] = r["nvf"]
        nlf[:, c] = r["nlf"][:, 0]
    return (attn.reshape(128, 1024), nkd.reshape(1, 32, 4, 8, 2, 32), nvd.reshape(1, 32, 4, 8, 64), nkf.reshape(1, 32, 4, 8, 64),
            nvf.reshape(1, 32, 4, 8, 64), nlf.reshape(1, 32, 4, 8))


def kernel(**inputs):
    attn_s, nkd, nvd, nkf, nvf, nlf = run_sample(inputs)
    y_p, y_s, kd, vd, kf, vf, lf = run_main(inputs, attn_s)
    return (y_p, y_s, kd, vd, kf, vf, lf, nkd, nvd, nkf, nvf, nlf)


def chunk_order(cc):
    order = []
    for s in range(4):
        order += [4 * s + cc] + [4 * s + j for j in range(4) if j != cc]
    return order


def run_main(inputs, attn_s):
    f32 = np.float32
    if "main" not in _NC_CACHE:
        _NC_CACHE["main"] = build_main()
    nc = _NC_CACHE["main"]
    ident, U, dm, mk, invf = host_consts()
    xp = np.asarray(inputs["x_prompt"], f32)
    xs = np.asarray(inputs["x_sample"], f32).reshape(128, D)
    w_g = np.concatenate([np.asarray(inputs["w_exp_gate"][0], f32), np.asarray(inputs["w_sh_gate"][0], f32)[None]], axis=0)
    w_u = np.concatenate([np.asarray(inputs["w_exp_up"][0], f32), np.asarray(inputs["w_sh_up"][0], f32)[None]], axis=0)
    w_d = np.concatenate([np.asarray(inputs["w_exp_down"][0], f32), np.asarray(inputs["w_sh_down"][0], f32)[None]], axis=0)
    lam4 = np.stack([inputs["lambda_q1"][0], inputs["lambda_k1"][0], inputs["lambda_q2"][0], inputs["lambda_k2"][0]]).astype(f32)
    shared = {
        "w_in": np.ascontiguousarray(inputs["w_in"][0], f32), "b_forget": np.asarray(inputs["b_forget"], f32).reshape(1, 8),
        "lam4": lam4, "subln": np.asarray(inputs["subln_gain"], f32).reshape(64, 1), "w_o": np.ascontiguousarray(inputs["w_o"][0], f32),
        "ln1": np.stack([inputs["ln1_g"][0], inputs["ln1_b"][0]]).astype(f32), "ln2": np.stack([inputs["ln2_g"][0], inputs["ln2_b"][0]]).astype(f32),
        "w_router": np.ascontiguousarray(inputs["w_router"][0], f32), "r_bias": np.asarray(inputs["router_bias"], f32).reshape(1, NE),
        "w_g": w_g, "w_u": w_u, "w_d": w_d,
        "c_ident": ident, "c_U": U, "c_dm": dm, "c_mk": mk, "c_invf": invf,
    }
    in_maps = []
    toks = []
    for c in range(8):
        b, cc = divmod(c, 4)
        tok = np.concatenate([np.arange(ch * 512, (ch + 1) * 512) for ch in chunk_order(cc)])
        toks.append(tok)
        tt = tok.reshape(NT, 128)
        m = dict(shared)
        m["x_perm"] = np.ascontiguousarray(xp[b][tok])
        m["kpos"] = np.ascontiguousarray(tt.T.astype(f32))
        m["tpos"] = np.ascontiguousarray(tt[:, 0:1].astype(f32))
        m["qfirst"] = np.array([[tok[s * 2048] for s in range(4)]], f32)
        m["x_s"] = np.ascontiguousarray(xs[16 * c:16 * c + 16])
        m["attn_s"] = np.ascontiguousarray(attn_s[16 * c:16 * c + 16])
        in_maps.append(m)
    res = run_bass_kernel_spmd(nc, in_maps, core_ids=list(range(8)))
    y_p = np.zeros((2, SEQ, D), f32)
    y_s = np.zeros((128, D), f32)
    kd = np.zeros((2, SEQ, 512), f32)
    vd = np.zeros((2, SEQ, 512), f32)
    kf = np.zeros((2, SEQ, 512), f32)
    vf = np.zeros((2, SEQ, 512), f32)
    lf = np.zeros((2, SEQ, 8), f32)
    for c in range(8):
        b, cc = divmod(c, 4)
        r = res.results[c]
        own = np.concatenate([toks[c][s * 2048:s * 2048 + 512] for s in range(4)])
        y_p[b, own] = r["y_own"]
        kd[b, own] = r["kd_own"]
        vd[b, own] = r["vd_own"]
        kf[b, own] = r["kf_own"]
        vf[b, own] = r["vf_own"]
        lf[b, own] = r["lf_own"]
        y_s[16 * c:16 * c + 16] = r["y_s"]
    return (y_p, y_s.reshape(32, 4, D), kd.reshape(1, 2, SEQ, 8, 2, 32), vd.reshape(1, 2, SEQ, 8, 64),
            kf.reshape(1, 2, SEQ, 8, 64), vf.reshape(1, 2, SEQ, 8, 64), lf.reshape(1, 2, SEQ, 8))


if __name__ == "__main__":
    build_sample()
    build_main()
```
